# Optimizing a Trainium2 kernel written in Bass

```python
import math
import jax, jax.numpy as jnp
from jax import lax
import numpy as np

D_MODEL = 1024
BATCH = 4
SEQ = 8192
DEPTH = 2

GRID_W = 64
CTX_LEN = 256
N_MOD = 6
RMS_EPS = 1e-6
GN_EPS = 64e-5
F32 = jnp.float32
GDN_HEADS = 8
GDN_DK = 64
GDN_DV = 64
GDN_CONV = 3
GDN_CHUNK = 64
RWKV_HEADS = 8
RWKV_N = 64
RWKV_DECAY_LORA = 64
RWKV_AAA_LORA = 64
RWKV_GATE_LORA = 128
ATTN_HEADS = 8
ATTN_KV_HEADS = 2
ATTN_GROUP = ATTN_HEADS // ATTN_KV_HEADS
ATTN_DH = 64
WINDOW = 128
ATTN_BLOCK = 128
ROPE_THETA = 10000.0
HYENA_WIDTH = 512
HYENA_ORDER = 2
HYENA_SHORT = 3
HYENA_BANDS = 16
HYENA_EMB = 1 + 2 * HYENA_BANDS
HYENA_HIDDEN = 64
HYENA_FAST_DECAY = 0.3
HYENA_SLOW_DECAY = 1.5
HYENA_TARGET = 1e-2
PEER_HEADS = 8
PEER_NKEYS = 128
PEER_EXPERTS = PEER_NKEYS * PEER_NKEYS
PEER_QDIM = 256
PEER_TOPK = 16
PEER_BLOCK = 64

GDN_QK = GDN_HEADS * GDN_DK
GDN_VW = GDN_HEADS * GDN_DV
RWKV_W = RWKV_HEADS * RWKV_N
RWKV_SPLITS = (RWKV_W, RWKV_W, RWKV_W, RWKV_DECAY_LORA, RWKV_AAA_LORA, RWKV_GATE_LORA)
RWKV_IN = 3 * RWKV_W + RWKV_DECAY_LORA + RWKV_AAA_LORA + RWKV_GATE_LORA
EVEN_SPLITS = (2 * GDN_QK + GDN_VW, GDN_VW, 4 * GDN_HEADS, RWKV_IN)
EVEN_IN = 2 * GDN_QK + 2 * GDN_VW + 4 * GDN_HEADS + RWKV_IN
EVEN_OUT = GDN_VW + RWKV_W
ATTN_QW = ATTN_HEADS * ATTN_DH
ATTN_KVW = ATTN_KV_HEADS * ATTN_DH
HYENA_IN = (HYENA_ORDER + 1) * HYENA_WIDTH
ODD_SPLITS = (ATTN_QW, ATTN_KVW, ATTN_KVW, HYENA_IN)
ODD_IN = ATTN_QW + 2 * ATTN_KVW + HYENA_IN
ODD_OUT = ATTN_QW + HYENA_WIDTH

kernel_name = 'hybrid_gdn_rwkv7_swa_hyena_peer_flow'


def _split(t, sizes):
    return jnp.split(t, [int(s) for s in np.cumsum(sizes)[:-1]], axis=-1)


def _orient(t, d):
    return jnp.flip(t, axis=1) if d == 1 else t


def rmsnorm(x, w):
    xf = x.astype(F32)
    y = xf * lax.rsqrt(jnp.mean(xf * xf, axis=-1, keepdims=True) + RMS_EPS)
    return (y * w.astype(F32)).astype(x.dtype)


def l2norm(x):
    return x * lax.rsqrt(jnp.sum(x * x, axis=-1, keepdims=True) + 1e-6)


def group_norm_heads(y, w, b):
    h, n = y.shape[-2:]
    yc = y - jnp.mean(y, axis=-1, keepdims=True)
    var = jnp.mean(yc * yc, axis=-1, keepdims=True)
    return yc * lax.rsqrt(var + GN_EPS) * w.astype(F32).reshape(h, n) + b.astype(F32).reshape(h, n)


def adaln(cond, mod_w, mod_b):
    return jnp.split(jax.nn.silu(cond) @ mod_w + mod_b, N_MOD, axis=-1)


def pre(x, norm_w, shift, scale):
    return rmsnorm(x, norm_w) * (1.0 + scale) + shift


def dwconv_centered(x, w):
    width, ch = w.shape
    return lax.conv_general_dilated(x, w[:, None, :].astype(x.dtype), window_strides=(1,),
                                    padding=[(width // 2, width // 2)],
                                    dimension_numbers=('NWC', 'WIO', 'NWC'), feature_group_count=ch)


def token_shift_bidir(p, mu):
    prev = jnp.pad(p[:, :-1], ((0, 0), (1, 0), (0, 0)))
    nxt = jnp.pad(p[:, 1:], ((0, 0), (0, 1), (0, 0)))
    return p + mu[0] * (prev - p) + mu[1] * (nxt - p)


def gdn_chunk_scan(q, k, v, g, beta, s0):
    B, L, H, _ = q.shape
    dv = v.shape[-1]
    C = GDN_CHUNK
    n = L // C

    def chunks(t):
        return jnp.moveaxis(t.reshape((B, n, C, H) + t.shape[3:]), 3, 1)

    q, k, v, g, beta = (chunks(t) for t in (q, k, v, g, beta))
    G = jnp.cumsum(g, axis=-1)
    causal = jnp.tril(jnp.ones((C, C), bool))
    strict = jnp.tril(jnp.ones((C, C), bool), -1)
    decay = jnp.exp(jnp.where(causal, G[..., :, None] - G[..., None, :], -jnp.inf))
    kb = k * beta[..., None]
    m = jnp.where(strict, jnp.einsum('bhnik,bhnjk->bhnij', kb, k) * decay, 0.0)
    lower = m + jnp.eye(C, dtype=m.dtype)
    rhs = jnp.concatenate([v * beta[..., None], kb * jnp.exp(G)[..., None]], axis=-1)
    sol = lax.linalg.triangular_solve(lower, rhs, left_side=True, lower=True, unit_diagonal=True)
    u, w = sol[..., :dv], sol[..., dv:]
    attn = jnp.einsum('bhnik,bhnjk->bhnij', q, k) * decay
    qg = q * jnp.exp(G)[..., None]
    kd = k * jnp.exp(G[..., -1:] - G)[..., None]
    gl = jnp.exp(G[..., -1])

    def step(s, inp):
        attn_i, qg_i, kd_i, u_i, w_i, gl_i = inp
        v_new = u_i - jnp.einsum('bhck,bhkv->bhcv', w_i, s)
        o = jnp.einsum('bhck,bhkv->bhcv', qg_i, s) + jnp.einsum('bhij,bhjv->bhiv', attn_i, v_new)
        s = s * gl_i[..., None, None] + jnp.einsum('bhck,bhcv->bhkv', kd_i, v_new)
        return s, o

    xs = tuple(jnp.moveaxis(t, 2, 0) for t in (attn, qg, kd, u, w, gl))
    s, o = lax.scan(step, s0, xs)
    return jnp.transpose(o, (1, 0, 3, 2, 4)).reshape(B, L, H, dv), s


def rwkv_scan(r, decay, k, v, kk, a, s0):
    def step(s, inp):
        r_t, w_t, k_t, v_t, kk_t, a_t = inp
        sa = jnp.einsum('bhvk,bhk->bhv', s, -kk_t)
        s = (s * w_t[:, :, None, :] + sa[..., None] * (kk_t * a_t)[:, :, None, :]
             + v_t[..., None] * k_t[:, :, None, :])
        return s, jnp.einsum('bhvk,bhk->bhv', s, r_t)

    xs = tuple(jnp.moveaxis(t, 1, 0) for t in (r, decay, k, v, kk, a))
    s, y = lax.scan(step, s0, xs)
    return jnp.moveaxis(y, 0, 1), s


def even_mixer(p, states, gdn_conv, gdn_A_log, gdn_dt_bias, gdn_norm, rwkv_mu, rwkv_w0, rwkv_w2,
               rwkv_a0, rwkv_a2, rwkv_g2, rwkv_k_k, rwkv_k_a, rwkv_r_k, rwkv_ln_w, rwkv_ln_b):
    B, L, _ = p.shape
    qkv, z, ab, pr = _split(p, EVEN_SPLITS)
    new_states = []
    qkv = jax.nn.silu(dwconv_centered(qkv, gdn_conv)).astype(F32)
    q, k, v = _split(qkv, (GDN_QK, GDN_QK, GDN_VW))
    q = l2norm(q.reshape(B, L, GDN_HEADS, GDN_DK)) * GDN_DK ** -0.5
    k = l2norm(k.reshape(B, L, GDN_HEADS, GDN_DK))
    v = v.reshape(B, L, GDN_HEADS, GDN_DV)
    ab = ab.astype(F32).reshape(B, L, 4, GDN_HEADS)
    o_gdn = 0.0
    for d in range(2):
        g = -jnp.exp(gdn_A_log[d].astype(F32)) * jax.nn.softplus(ab[:, :, d] + gdn_dt_bias[d].astype(F32))
        beta = jax.nn.sigmoid(ab[:, :, 2 + d])
        o_d, s_d = gdn_chunk_scan(*(_orient(t, d) for t in (q, k, v, g, beta)), states[d])
        o_gdn = o_gdn + _orient(o_d, d)
        new_states.append(s_d)
    zg = jax.nn.silu(z.astype(F32).reshape(B, L, GDN_HEADS, GDN_DV))
    o_gdn = (rmsnorm(o_gdn, gdn_norm) * zg).reshape(B, L, GDN_VW)
    pr = token_shift_bidir(pr, rwkv_mu).astype(F32)
    r, kr, vr, xw, xa, xg = _split(pr, RWKV_SPLITS)

    def hd(t):
        return t.reshape(B, L, RWKV_HEADS, RWKV_N)

    kk = l2norm(hd(kr * rwkv_k_k))
    y_sum, bonus = 0.0, 0.0
    for d in range(2):
        w_log = -jax.nn.softplus(-(rwkv_w0[d] + jnp.tanh(xw) @ rwkv_w2[d])) - 0.5
        decay = jnp.exp(-jnp.exp(w_log))
        a = jax.nn.sigmoid(rwkv_a0[d] + xa @ rwkv_a2)
        k_d = kr * (1.0 + (a - 1.0) * rwkv_k_a)
        y_d, s_d = rwkv_scan(*(_orient(t, d) for t in (hd(r), hd(decay), hd(k_d), hd(vr), kk, hd(a))),
                             states[2 + d])
        y_sum = y_sum + _orient(y_d, d)
        bonus = bonus + jnp.sum(hd(r) * hd(k_d) * rwkv_r_k, axis=-1, keepdims=True) * hd(vr)
        new_states.append(s_d)
    y = (group_norm_heads(y_sum, rwkv_ln_w, rwkv_ln_b) + bonus).reshape(B, L, RWKV_W)
    y = y * (jax.nn.sigmoid(xg) @ rwkv_g2)
    out = jnp.concatenate([o_gdn, y], axis=-1).astype(p.dtype)
    return out, tuple(new_states)


def rope_axial(x, row, col):
    half = x.shape[-1] // 2
    nf = half // 2
    inv = ROPE_THETA ** (-jnp.arange(nf, dtype=F32) / nf)

    def rot(xa, pos):
        ang = pos.astype(F32)[:, None] * inv[None, :]
        cos, sin = jnp.cos(ang)[None, :, None, :], jnp.sin(ang)[None, :, None, :]
        x1, x2 = xa[..., :nf], xa[..., nf:]
        return jnp.concatenate([x1 * cos - x2 * sin, x1 * sin + x2 * cos], axis=-1)

    xf = x.astype(F32)
    return jnp.concatenate([rot(xf[..., :half], row), rot(xf[..., half:], col)], axis=-1).astype(x.dtype)


def sink_softmax(s, sink):
    sk = sink.astype(F32).reshape(1, ATTN_KV_HEADS, ATTN_GROUP, 1)
    m = jnp.maximum(jnp.max(s, axis=-1), sk)
    e = jnp.exp(s - m[..., None])
    return e / (jnp.sum(e, axis=-1) + jnp.exp(sk - m))[..., None]


def window_attention(q, k, v, k_ctx, v_ctx, sink):
    B, L = q.shape[:2]
    nb = L // ATTN_BLOCK
    scale = ATTN_DH ** -0.5
    q5 = q.reshape(B, L, ATTN_KV_HEADS, ATTN_GROUP, ATTN_DH)
    pad = ((0, 0), (ATTN_BLOCK, ATTN_BLOCK), (0, 0), (0, 0))
    kp, vp = jnp.pad(k, pad), jnp.pad(v, pad)
    qi = jnp.arange(ATTN_BLOCK)
    kj = jnp.arange(3 * ATTN_BLOCK) - ATTN_BLOCK
    in_window = jnp.abs(kj[None, :] - qi[:, None]) <= WINDOW

    def block(b):
        start = b * ATTN_BLOCK
        qb = lax.dynamic_slice_in_dim(q5, start, ATTN_BLOCK, axis=1)
        kb = lax.dynamic_slice_in_dim(kp, start, 3 * ATTN_BLOCK, axis=1)
        vb = lax.dynamic_slice_in_dim(vp, start, 3 * ATTN_BLOCK, axis=1)
        key_pos = start + kj
        valid = in_window & ((key_pos >= 0) & (key_pos < L))[None, :]
        s_loc = jnp.einsum('bqhgd,bkhd->bhgqk', qb, kb).astype(F32) * scale
        s_loc = jnp.where(valid, s_loc, -jnp.inf)
        s_ctx = jnp.einsum('bqhgd,bkhd->bhgqk', qb, k_ctx).astype(F32) * scale
        pr = sink_softmax(jnp.concatenate([s_loc, s_ctx], axis=-1), sink).astype(v.dtype)
        return (jnp.einsum('bhgqk,bkhd->bqhgd', pr[..., :3 * ATTN_BLOCK], vb)
                + jnp.einsum('bhgqk,bkhd->bqhgd', pr[..., 3 * ATTN_BLOCK:], v_ctx))

    o = lax.map(block, jnp.arange(nb))
    return jnp.moveaxis(o, 0, 1).reshape(B, L, ATTN_QW)


def hyena_filters(L, w1, b1, w2, b2, freq, w3):
    t = jnp.arange(L, dtype=F32)
    t_norm = jnp.linspace(0.0, 1.0, L, dtype=F32)[:, None]
    bands = jnp.linspace(1e-4, HYENA_BANDS - 1, HYENA_BANDS, dtype=F32)
    ang = (2.0 * math.pi / L) * t[:, None] * bands[None, :]
    z = jnp.concatenate([t_norm, jnp.cos(ang), jnp.sin(ang)], axis=-1)
    fr = freq.astype(F32)
    h = jnp.sin(fr * (z @ w1.astype(F32) + b1.astype(F32)))
    h = jnp.sin(fr * (h @ w2.astype(F32) + b2.astype(F32)))
    h = h @ w3.astype(F32)
    deltas = jnp.abs(jnp.linspace(math.log(HYENA_TARGET) / HYENA_SLOW_DECAY,
                                  math.log(HYENA_TARGET) / HYENA_FAST_DECAY, HYENA_WIDTH, dtype=F32))
    window = jnp.exp(-t_norm * deltas[None, :])
    return h.reshape(L, HYENA_ORDER, 2, HYENA_WIDTH) * window[:, None, None, :]


def fft_longconv(u, h_f, h_b, bias):
    L = u.shape[1]
    l1 = jnp.sum(jnp.abs(h_f), axis=0) + jnp.sum(jnp.abs(h_b), axis=0)
    h_f, h_b = h_f / l1, h_b / l1
    kern = jnp.concatenate([h_f[:1] + h_b[:1], h_f[1:], jnp.zeros_like(h_f[:1]), h_b[:0:-1]], axis=0)
    n = 2 * L
    y = jnp.fft.irfft(jnp.fft.rfft(u, n=n, axis=1) * jnp.fft.rfft(kern, n=n, axis=0)[None], n=n, axis=1)[:, :L]
    return y + u * bias


def hyena_operator(ph, conv_w, conv_b, w1, b1, w2, b2, freq, w3, bias):
    L = ph.shape[1]
    u = (dwconv_centered(ph, conv_w) + conv_b).astype(F32)
    v, x1, x2 = _split(u, (HYENA_WIDTH, HYENA_WIDTH, HYENA_WIDTH))
    filt = hyena_filters(L, w1, b1, w2, b2, freq, w3)
    z = v
    for n, gate in enumerate((x1, x2)):
        z = gate * fft_longconv(z, filt[:, n, 0], filt[:, n, 1], bias[n].astype(F32))
    return z


def peer(h, peer_wq, peer_k1, peer_k2, peer_u, peer_v):
    B, L, D = h.shape
    nb = L // PEER_BLOCK
    half = PEER_QDIM // 2
    hb = jnp.moveaxis(h.reshape(B, nb, PEER_BLOCK, D), 1, 0)

    def block(xb):
        q = (xb @ peer_wq).astype(F32).reshape(B, PEER_BLOCK, PEER_HEADS, 2, half)
        s1 = jnp.einsum('bthk,hnk->bthn', q[..., 0, :], peer_k1.astype(F32))
        s2 = jnp.einsum('bthk,hnk->bthn', q[..., 1, :], peer_k2.astype(F32))
        v1, i1 = lax.top_k(s1, PEER_TOPK)
        v2, i2 = lax.top_k(s2, PEER_TOPK)
        cand = (v1[..., :, None] + v2[..., None, :]).reshape(B, PEER_BLOCK, PEER_HEADS, PEER_TOPK * PEER_TOPK)
        cid = (i1[..., :, None] * PEER_NKEYS + i2[..., None, :]).reshape(cand.shape)
        best, pos = lax.top_k(cand, PEER_TOPK)
        eid = jnp.take_along_axis(cid, pos, axis=-1)
        gate = jax.nn.softmax(best, axis=-1)
        u = jnp.take(peer_u, eid, axis=0)
        act = jax.nn.gelu(jnp.einsum('bthkd,btd->bthk', u, xb).astype(F32))
        vsel = jnp.take(peer_v, eid, axis=0)
        return jnp.einsum('bthk,bthkd->btd', (gate * act).astype(xb.dtype), vsel)

    return jnp.moveaxis(lax.map(block, hb), 0, 1).reshape(B, L, D)


def ffn_residual(x, norm_w, shift, scale, gate, peer_wq, peer_k1, peer_k2, peer_u, peer_v):
    return x + gate * peer(pre(x, norm_w, shift, scale), peer_wq, peer_k1, peer_k2, peer_u, peer_v)


def even_layer(x_ctx, x_lat, c, c_ctx, mod_w, mod_b, norm1, norm2, w_in, w_out,
               peer_wq, peer_k1, peer_k2, peer_u, peer_v, **mix):
    ml = [m[:, None, :] for m in adaln(c, mod_w, mod_b)]
    mc = adaln(c_ctx, mod_w, mod_b)
    B = x_lat.shape[0]
    z_gdn = jnp.zeros((B, GDN_HEADS, GDN_DK, GDN_DV), F32)
    z_rwkv = jnp.zeros((B, RWKV_HEADS, RWKV_N, RWKV_N), F32)
    y_ctx, ctx_states = even_mixer(pre(x_ctx, norm1, mc[0], mc[1]) @ w_in, (z_gdn, z_gdn, z_rwkv, z_rwkv), **mix)
    y_lat, _ = even_mixer(pre(x_lat, norm1, ml[0], ml[1]) @ w_in, ctx_states, **mix)
    x_lat = ffn_residual(x_lat + ml[2] * (y_lat @ w_out), norm2, ml[3], ml[4], ml[5],
                         peer_wq, peer_k1, peer_k2, peer_u, peer_v)
    x_ctx = ffn_residual(x_ctx + mc[2] * (y_ctx @ w_out), norm2, mc[3], mc[4], mc[5],
                         peer_wq, peer_k1, peer_k2, peer_u, peer_v)
    return x_ctx, x_lat


def final_odd_layer(x_ctx, x_lat, c, c_ctx, row, col, mod_w, mod_b, norm1, norm2, w_in, w_out, attn_sink,
                    hy_conv_w, hy_conv_b, hy_w1, hy_b1, hy_w2, hy_b2, hy_freq, hy_w3, hy_bias,
                    peer_wq, peer_k1, peer_k2, peer_u, peer_v):
    ml = [m[:, None, :] for m in adaln(c, mod_w, mod_b)]
    mc = adaln(c_ctx, mod_w, mod_b)
    B, C, _ = x_ctx.shape
    L = x_lat.shape[1]
    kv_c = pre(x_ctx, norm1, mc[0], mc[1]) @ w_in[:, ATTN_QW:ATTN_QW + 2 * ATTN_KVW]
    k_c, v_c = (t.reshape(B, C, ATTN_KV_HEADS, ATTN_DH) for t in _split(kv_c, (ATTN_KVW, ATTN_KVW)))
    q, k, v, ph = _split(pre(x_lat, norm1, ml[0], ml[1]) @ w_in, ODD_SPLITS)
    q = rope_axial(q.reshape(B, L, ATTN_HEADS, ATTN_DH), row, col)
    k = rope_axial(k.reshape(B, L, ATTN_KV_HEADS, ATTN_DH), row, col)
    v = v.reshape(B, L, ATTN_KV_HEADS, ATTN_DH)
    y_attn = window_attention(q, k, v, k_c, v_c, attn_sink)
    y_hy = hyena_operator(ph, hy_conv_w, hy_conv_b, hy_w1, hy_b1, hy_w2, hy_b2, hy_freq, hy_w3,
                          hy_bias).astype(x_lat.dtype)
    y = jnp.concatenate([y_attn, y_hy], axis=-1) @ w_out
    return ffn_residual(x_lat + ml[2] * y, norm2, ml[3], ml[4], ml[5], peer_wq, peer_k1, peer_k2, peer_u, peer_v)


def setup_inputs(seed: int = 0) -> dict:
    keys = iter(jax.random.split(jax.random.key(seed), 64))
    D = D_MODEL

    def nrm(shape, scale):
        return scale * jax.random.normal(next(keys), shape, F32)

    def gain(shape):
        return 1.0 + nrm(shape, 0.02)

    def unif(shape, lo, hi):
        return jax.random.uniform(next(keys), shape, F32, lo, hi)

    inp = {}
    inp['x'] = nrm((BATCH, SEQ, D), 1.0)
    inp['c'] = nrm((BATCH, D), 1.0)
    inp['ctx'] = nrm((BATCH, CTX_LEN, D), 1.0)
    inp['c_ctx'] = nrm((D,), 1.0)

    def add_common(pfx, n_in, n_out):
        inp[pfx + 'mod_w'] = nrm((D, N_MOD * D), 0.5 * D ** -0.5)
        inp[pfx + 'mod_b'] = nrm((N_MOD * D,), 0.02)
        inp[pfx + 'norm1'] = gain((D,))
        inp[pfx + 'norm2'] = gain((D,))
        inp[pfx + 'w_in'] = nrm((D, n_in), D ** -0.5)
        inp[pfx + 'w_out'] = nrm((n_out, D), n_out ** -0.5)

    def add_peer(pfx):
        inp[pfx + 'peer_wq'] = nrm((D, PEER_HEADS * PEER_QDIM), D ** -0.5)
        inp[pfx + 'peer_k1'] = nrm((PEER_HEADS, PEER_NKEYS, PEER_QDIM // 2), (PEER_QDIM // 2) ** -0.5)
        inp[pfx + 'peer_k2'] = nrm((PEER_HEADS, PEER_NKEYS, PEER_QDIM // 2), (PEER_QDIM // 2) ** -0.5)
        inp[pfx + 'peer_u'] = nrm((PEER_EXPERTS, D), D ** -0.5)
        inp[pfx + 'peer_v'] = nrm((PEER_EXPERTS, D), PEER_HEADS ** -0.5)

    add_common('l0_', EVEN_IN, EVEN_OUT)
    inp['l0_gdn_conv'] = nrm((GDN_CONV, 2 * GDN_QK + GDN_VW), GDN_CONV ** -0.5)
    inp['l0_gdn_A_log'] = jnp.log(unif((2, GDN_HEADS), 1.0, 16.0))
    dt = jnp.exp(unif((2, GDN_HEADS), math.log(1e-3), math.log(1e-1)))
    inp['l0_gdn_dt_bias'] = dt + jnp.log(-jnp.expm1(-dt))
    inp['l0_gdn_norm'] = gain((GDN_DV,))
    inp['l0_rwkv_mu'] = unif((2, RWKV_IN), 0.0, 0.5)
    ratio = jnp.arange(RWKV_W, dtype=F32) / (RWKV_W - 1)
    inp['l0_rwkv_w0'] = (-6.0 + 5.0 * ratio ** 0.9)[None, :] + nrm((2, RWKV_W), 0.1)
    inp['l0_rwkv_w2'] = nrm((2, RWKV_DECAY_LORA, RWKV_W), 0.5 * RWKV_DECAY_LORA ** -0.5)
    inp['l0_rwkv_a0'] = nrm((2, RWKV_W), 0.1)
    inp['l0_rwkv_a2'] = nrm((RWKV_AAA_LORA, RWKV_W), 0.5 * RWKV_AAA_LORA ** -0.5)
    inp['l0_rwkv_g2'] = nrm((RWKV_GATE_LORA, RWKV_W), RWKV_GATE_LORA ** -0.5)
    inp['l0_rwkv_k_k'] = 0.85 + nrm((RWKV_W,), 0.02)
    inp['l0_rwkv_k_a'] = 1.0 + nrm((RWKV_W,), 0.02)
    inp['l0_rwkv_r_k'] = -0.04 + nrm((RWKV_HEADS, RWKV_N), 0.01)
    inp['l0_rwkv_ln_w'] = gain((RWKV_W,))
    inp['l0_rwkv_ln_b'] = nrm((RWKV_W,), 0.02)
    add_peer('l0_')
    add_common('l1_', ODD_IN, ODD_OUT)
    inp['l1_attn_sink'] = nrm((ATTN_HEADS,), 0.5)
    inp['l1_hy_conv_w'] = nrm((HYENA_SHORT, HYENA_IN), HYENA_SHORT ** -0.5)
    inp['l1_hy_conv_b'] = nrm((HYENA_IN,), 0.02)
    inp['l1_hy_w1'] = nrm((HYENA_EMB, HYENA_HIDDEN), HYENA_EMB ** -0.5)
    inp['l1_hy_b1'] = nrm((HYENA_HIDDEN,), 0.02)
    inp['l1_hy_w2'] = nrm((HYENA_HIDDEN, HYENA_HIDDEN), HYENA_HIDDEN ** -0.5)
    inp['l1_hy_b2'] = nrm((HYENA_HIDDEN,), 0.02)
    inp['l1_hy_freq'] = 1.0 + nrm((HYENA_HIDDEN,), 0.02)
    inp['l1_hy_w3'] = nrm((HYENA_HIDDEN, HYENA_ORDER * 2 * HYENA_WIDTH), HYENA_HIDDEN ** -0.5)
    inp['l1_hy_bias'] = nrm((HYENA_ORDER, HYENA_WIDTH), 0.5)
    add_peer('l1_')
    inp['final_norm'] = gain((D,))
    return inp


def reference(x, c, ctx, c_ctx,
              l0_mod_w, l0_mod_b, l0_norm1, l0_norm2, l0_w_in, l0_w_out,
              l0_gdn_conv, l0_gdn_A_log, l0_gdn_dt_bias, l0_gdn_norm,
              l0_rwkv_mu, l0_rwkv_w0, l0_rwkv_w2, l0_rwkv_a0, l0_rwkv_a2, l0_rwkv_g2,
              l0_rwkv_k_k, l0_rwkv_k_a, l0_rwkv_r_k, l0_rwkv_ln_w, l0_rwkv_ln_b,
              l0_peer_wq, l0_peer_k1, l0_peer_k2, l0_peer_u, l0_peer_v,
              l1_mod_w, l1_mod_b, l1_norm1, l1_norm2, l1_w_in, l1_w_out,
              l1_attn_sink, l1_hy_conv_w, l1_hy_conv_b, l1_hy_w1, l1_hy_b1, l1_hy_w2, l1_hy_b2,
              l1_hy_freq, l1_hy_w3, l1_hy_bias,
              l1_peer_wq, l1_peer_k1, l1_peer_k2, l1_peer_u, l1_peer_v,
              final_norm):
    L = x.shape[1]
    ROWS = L // GRID_W
    row = jnp.repeat(jnp.arange(ROWS, dtype=jnp.int32), GRID_W)
    col = jnp.tile(jnp.arange(GRID_W, dtype=jnp.int32), ROWS)
    layer0 = dict(mod_w=l0_mod_w, mod_b=l0_mod_b, norm1=l0_norm1, norm2=l0_norm2, w_in=l0_w_in, w_out=l0_w_out,
                  gdn_conv=l0_gdn_conv, gdn_A_log=l0_gdn_A_log, gdn_dt_bias=l0_gdn_dt_bias, gdn_norm=l0_gdn_norm,
                  rwkv_mu=l0_rwkv_mu, rwkv_w0=l0_rwkv_w0, rwkv_w2=l0_rwkv_w2, rwkv_a0=l0_rwkv_a0,
                  rwkv_a2=l0_rwkv_a2, rwkv_g2=l0_rwkv_g2, rwkv_k_k=l0_rwkv_k_k, rwkv_k_a=l0_rwkv_k_a,
                  rwkv_r_k=l0_rwkv_r_k, rwkv_ln_w=l0_rwkv_ln_w, rwkv_ln_b=l0_rwkv_ln_b,
                  peer_wq=l0_peer_wq, peer_k1=l0_peer_k1, peer_k2=l0_peer_k2, peer_u=l0_peer_u, peer_v=l0_peer_v)
    layer1 = dict(mod_w=l1_mod_w, mod_b=l1_mod_b, norm1=l1_norm1, norm2=l1_norm2, w_in=l1_w_in, w_out=l1_w_out,
                  attn_sink=l1_attn_sink, hy_conv_w=l1_hy_conv_w, hy_conv_b=l1_hy_conv_b, hy_w1=l1_hy_w1,
                  hy_b1=l1_hy_b1, hy_w2=l1_hy_w2, hy_b2=l1_hy_b2, hy_freq=l1_hy_freq, hy_w3=l1_hy_w3,
                  hy_bias=l1_hy_bias, peer_wq=l1_peer_wq, peer_k1=l1_peer_k1, peer_k2=l1_peer_k2,
                  peer_u=l1_peer_u, peer_v=l1_peer_v)
    layers = (layer0, layer1)
    x_ctx, x_lat = ctx, x
    for i in range(DEPTH):
        if i % 2 == 0:
            x_ctx, x_lat = even_layer(x_ctx, x_lat, c, c_ctx, **layers[i])
        else:
            x_lat = final_odd_layer(x_ctx, x_lat, c, c_ctx, row, col, **layers[i])
    return rmsnorm(x_lat, final_norm)
```

```python
import numpy as np
from contextlib import ExitStack
import concourse.bass as bass
import concourse.mybir as mybir
from concourse.bass_utils import run_bass_kernel_spmd

F32 = mybir.dt.float32
BF16 = mybir.dt.bfloat16
ALU = mybir.AluOpType
AF = mybir.ActivationFunctionType
AX = mybir.AxisListType

SAME_ENGINE_SYNC = True
N_DMA_SEM = 8
D = 1024


class Buf:
    __slots__ = ("w", "r", "name")

    def __init__(self, name=""):
        self.w = None
        self.r = {}
        self.name = name


class Prog:
    def __init__(self, nc):
        self.nc = nc
        self.es = ExitStack()
        self.eng = {"pe": nc.tensor, "dve": nc.vector, "act": nc.scalar, "pool": nc.gpsimd, "sp": nc.sync}
        self.sem = {}
        self.cnt = {}
        for e in ("pe", "dve", "act", "pool"):
            self.sem[e] = self.es.enter_context(nc.semaphore("s_" + e))
            self.cnt[e] = 0
        self.dsem = []
        for i in range(N_DMA_SEM):
            k = "d%d" % i
            self.sem[k] = self.es.enter_context(nc.semaphore("s_" + k))
            self.cnt[k] = 0
            self.dsem.append(k)
        self.dnext = 0
        self.waited = {e: {} for e in self.eng}
        self.ninst = 0

    def sb(self, name, shape, dt=F32):
        return self.es.enter_context(self.nc.sbuf_tensor(name, list(shape), dt))

    def ps(self, name, shape, dt=F32):
        return self.es.enter_context(self.nc.psum_tensor(name, list(shape), dt))

    def dram(self, name, shape, dt=F32, kind="Internal"):
        return self.nc.dram_tensor(name, list(shape), dt, kind=kind).ap()

    def _wait(self, e, key, val):
        if key == e and (e == "pe" or not SAME_ENGINE_SYNC):
            return
        if self.waited[e].get(key, 0) >= val:
            return
        self.eng[e].wait_ge(self.sem[key], val)
        self.ninst += 1
        self.waited[e][key] = val

    def _deps(self, e, reads, writes):
        deps = {}
        for b in reads:
            if b.w is not None:
                k, v = b.w
                deps[k] = max(deps.get(k, 0), v)
        for b in writes:
            if b.w is not None:
                k, v = b.w
                deps[k] = max(deps.get(k, 0), v)
            for k, v in b.r.items():
                deps[k] = max(deps.get(k, 0), v)
        for k, v in deps.items():
            self._wait(e, k, v)

    def _mark(self, key, val, reads, writes):
        for b in reads:
            if b.r.get(key, 0) < val:
                b.r[key] = val
        for b in writes:
            b.w = (key, val)
            b.r = {}

    def op(self, e, fn, reads=(), writes=()):
        self._deps(e, reads, writes)
        ins = fn(self.eng[e])
        self.cnt[e] += 1
        ins.then_inc(self.sem[e], 1)
        self.ninst += 1
        self._mark(e, self.cnt[e], reads, writes)
        return ins

    def dma(self, q, out, in_, reads=(), writes=(), **kw):
        self._deps(q, reads, writes)
        k = self.dsem[self.dnext]
        self.dnext = (self.dnext + 1) % len(self.dsem)
        if self.cnt[k] > 0:
            self._wait(q, k, 16 * self.cnt[k])
        ins = self.eng[q].dma_start(out=out, in_=in_, **kw)
        self.cnt[k] += 1
        ins.then_inc(self.sem[k], 16)
        self.ninst += 1
        self._mark(k, 16 * self.cnt[k], reads, writes)
        return ins

    def finish(self, e="sp"):
        for k in self.dsem:
            if self.cnt[k] > 0:
                self._wait(e, k, 16 * self.cnt[k])
        for k in ("pe", "dve", "act", "pool"):
            if self.cnt[k] > 0 and k != e:
                self._wait(e, k, self.cnt[k])

    def barrier(self):
        for e in self.eng:
            for k in self.dsem:
                if self.cnt[k] > 0:
                    self._wait(e, k, 16 * self.cnt[k])
            for k in ("pe", "dve", "act", "pool"):
                if self.cnt[k] > 0 and k != e:
                    self._wait(e, k, self.cnt[k])

    def push_scope(self):
        self._outer = self.es
        self.es = ExitStack()

    def pop_scope(self):
        self.barrier()
        self.es.close()
        self.es = self._outer

    def close(self):
        self.es.close()


class Stream:
    def __init__(self, p, name, shape, nbuf, q):
        self.p = p
        self.q = q
        self.tiles = [p.sb("%s%d" % (name, i), shape) for i in range(nbuf)]
        self.bufs = [Buf() for _ in range(nbuf)]
        self.srcs = []
        self.issued = 0
        self.used = 0

    def plan(self, src):
        self.srcs.append(src)

    def _issue(self):
        k = self.issued
        if k >= len(self.srcs):
            return
        i = k % len(self.tiles)
        src, view = self.srcs[k]
        self.p.dma(self.q, view(self.tiles[i]), src, writes=[self.bufs[i]])
        self.issued += 1

    def get(self):
        k = self.used
        while self.issued <= min(k + len(self.tiles) - 1, len(self.srcs) - 1):
            if self.issued - k >= len(self.tiles):
                break
            self._issue()
        self.used += 1
        i = k % len(self.tiles)
        return self.tiles[i], self.bufs[i]


def bc_last(ap, n):
    sh = list(ap.shape)
    return ap.unsqueeze(len(sh)).to_broadcast(sh + [n])


def build_B(n_lat, n_ctx, final):
    NT = n_lat + n_ctx
    nc = bass.Bass("TRN2", target_bir_lowering=False)
    def ein(name, shape):
        return nc.dram_tensor(name, list(shape), F32, kind="ExternalInput").ap()
    xs = ein("xs", [NT * 128, D])
    fs = ein("fs", [NT * 128, D])
    cond2 = ein("cond2", [2, D])
    mod_w = ein("mod_w", [D, 6 * D])
    mod_b = ein("mod_b", [6 * D])
    norm2 = ein("norm2", [D])
    w_out = ein("w_out", [D, D])
    wq = ein("wq", [D, 2048])
    k1T = ein("k1T", [8, 128, 128])
    k2T = ein("k2T", [8, 128, 128])
    UT = ein("UT", [D, 16384])
    V = ein("V", [16384, D])
    fnorm = ein("fnorm", [D])
    ident_d = ein("ident_in", [128, 128])
    xo = nc.dram_tensor("xo", [NT * 128, D], F32, kind="ExternalOutput").ap()

    p = Prog(nc)
    ident = p.sb("ident", [128, 128]); b_id = Buf()
    p.dma("sp", ident[:], ident_d[:, :], writes=[b_id])

    ps_acc = p.ps("ps_acc", [128, 1024]); b_acc = Buf()
    ps_m = p.ps("ps_m", [128, 1024]); b_m = Buf()
    ps_a = [p.ps("ps_a%d" % i, [128, 512]) for i in range(2)]; b_a = [Buf(), Buf()]
    ps_t = [p.ps("ps_t%d" % i, [128, 512]) for i in range(2)]; b_t = [Buf(), Buf()]

    wst = Stream(p, "wst", [128, 8, 512], 2, "sp")
    vst = Stream(p, "vst", [128, 4, 1024], 2, "pool")
    kcv = lambda t: t[:]
    def wsrc(w, c0):
        return (w[:, c0:c0 + 512].rearrange("(kc p) n -> p kc n", p=128), kcv)

    csil = p.sb("csil", [128, 2, 8]); b_cs = Buf()
    p.dma("sp", csil[:], cond2.rearrange("s (kc p) -> p s kc", p=128), writes=[b_cs], allow_slow_non_contiguous=True)
    p.op("act", lambda e: e.activation(csil[:], csil[:], AF.Silu), reads=[b_cs], writes=[b_cs])
    CB = p.sb("CB", [128, 2, 8, 128]); b_cb = Buf()
    p.op("dve", lambda e: e.tensor_copy(CB[:], bc_last(csil[:], 128)), reads=[b_cs], writes=[b_cb])
    MOD = p.sb("MOD", [128, 2, 4, 1024]); b_mod = Buf()
    tmpb = p.sb("tmpb", [128, 1024]); b_tmpb = Buf()
    for j in range(4):
        p.dma("sp", tmpb[:], mod_b[(2 + j) * D:(3 + j) * D].partition_broadcast(128), writes=[b_tmpb])
        for nb in range(2):
            c0 = (2 + j) * D + nb * 512
            wt = p.sb("mw_%d_%d" % (j, nb), [128, 8, 512]) if False else None
            wst.plan(wsrc(mod_w, c0))
            blk, bb = wst.get()
            for s in range(2):
                for kc in range(8):
                    p.op("pe", lambda e, s=s, kc=kc: e.matmul(ps_a[s][:], lhsT=CB[:, s, kc, :], rhs=blk[:, kc, :],
                                                             start=(kc == 0), stop=(kc == 7)),
                         reads=[b_cb, bb], writes=[b_a[s]])
                p.op("dve", lambda e, s=s: e.tensor_tensor(MOD[:, s, j, nb * 512:(nb + 1) * 512], ps_a[s][:],
                                                           tmpb[:, nb * 512:(nb + 1) * 512], ALU.add),
                     reads=[b_a[s], b_tmpb], writes=[b_mod])
    n2b = p.sb("n2b", [128, 1024]); b_n2 = Buf()
    p.dma("sp", n2b[:], norm2.partition_broadcast(128), writes=[b_n2])
    for s in range(2):
        p.op("dve", lambda e, s=s: e.scalar_tensor_tensor(MOD[:, s, 2, :], in0=MOD[:, s, 2, :], scalar=1.0, in1=n2b[:],
                                                          op0=ALU.add, op1=ALU.mult),
             reads=[b_mod, b_n2], writes=[b_mod])
    if final:
        p.dma("sp", n2b[:], fnorm.partition_broadcast(128), reads=[b_mod], writes=[b_n2])
    k1s = p.sb("k1s", [128, 8, 128]); k2s = p.sb("k2s", [128, 8, 128]); b_k = Buf()
    p.dma("sp", k1s[:], k1T.rearrange("h k i -> k h i"), writes=[b_k])
    p.dma("sp", k2s[:], k2T.rearrange("h k i -> k h i"), writes=[b_k])

    A1 = p.sb("A1", [128, 1024]); b_A1 = Buf()
    A2 = p.sb("A2", [128, 1024]); b_A2 = Buf()
    A3 = p.sb("A3", [128, 1024]); b_A3 = Buf()
    A4 = p.sb("A4", [128, 2048]); b_A4 = Buf()
    A5 = p.sb("A5", [128, 2048]); b_A5 = Buf()
    s1 = p.sb("s1", [128, 1024]); b_s1 = Buf()
    s2 = p.sb("s2", [128, 1024]); b_s2 = Buf()
    e1 = p.sb("e1", [128, 1024]); b_e1 = Buf()
    e2 = p.sb("e2", [128, 1024]); b_e2 = Buf()
    wk = p.sb("wk", [128, 256]); b_wk = Buf()
    v1 = p.sb("v1", [128, 8, 16]); v2 = p.sb("v2", [128, 8, 16]); b_v = Buf()
    c24 = p.sb("c24", [128, 8, 24]); b_c = Buf()
    sm = p.sb("sm", [128, 64]); b_sm = Buf()
    Mw = [p.sb("Mw%d" % i, [128, 1024]) for i in range(2)]; b_Mw = [Buf(), Buf()]
    Wacc = [p.sb("Wacc%d" % i, [128, 512]) for i in range(2)]; b_W = [Buf(), Buf()]
    actt = [p.sb("actt%d" % i, [128, 512]) for i in range(2)]; b_act = [Buf(), Buf()]
    ATt = [p.sb("ATt%d" % i, [128, 512]) for i in range(2)]; b_AT = [Buf(), Buf()]

    for ti in range(NT):
        for nb in range(2):
            wst.plan(wsrc(w_out, nb * 512))
        for nb in range(4):
            wst.plan(wsrc(wq, nb * 512))
        for g in range(32):
            wst.plan(wsrc(UT, g * 512))
            vst.plan((V[g * 512:(g + 1) * 512, :].rearrange("(ib p) d -> p ib d", p=128), kcv))

    def transpose8(src, bsrc, dst, bdst, nblk=8):
        for c in range(nblk):
            p.op("pe", lambda e, c=c: e.transpose(ps_m[:, c * 128:(c + 1) * 128], src[:, c * 128:(c + 1) * 128], ident[:]),
                 reads=[bsrc, b_id], writes=[b_m])
        h = nblk * 64
        p.op("act", lambda e: e.copy(dst[:, 0:h], ps_m[:, 0:h]), reads=[b_m], writes=[bdst])
        p.op("dve", lambda e: e.tensor_copy(dst[:, h:2 * h], ps_m[:, h:2 * h]), reads=[b_m], writes=[bdst])

    for ti in range(NT):
        s = 0 if ti < n_lat else 1
        r0 = ti * 128
        p.dma("sp", A1[:], xs[r0:r0 + 128, :], writes=[b_A1])
        p.dma("sp", A2[:], fs[r0:r0 + 128, :], writes=[b_A2])
        transpose8(A2, b_A2, A3, b_A3)
        for nb in range(2):
            blk, bb = wst.get()
            for kc in range(8):
                p.op("pe", lambda e, kc=kc: e.matmul(ps_acc[:, nb * 512:(nb + 1) * 512], lhsT=A3[:, kc * 128:(kc + 1) * 128],
                                                     rhs=blk[:, kc, :], start=(kc == 0), stop=(kc == 7)),
                     reads=[b_A3, bb], writes=[b_acc])
        p.op("dve", lambda e: e.tensor_tensor(A2[:], ps_acc[:], MOD[:, s, 0, :], ALU.mult), reads=[b_acc, b_mod], writes=[b_A2])
        p.op("dve", lambda e: e.tensor_tensor(A1[:], A1[:], A2[:], ALU.add), reads=[b_A1, b_A2], writes=[b_A1])
        p.op("act", lambda e: e.activation(A2[:], A1[:], AF.Square, accum_out=sm[:, 0:1]), reads=[b_A1], writes=[b_A2, b_sm])
        p.op("dve", lambda e: e.tensor_scalar(sm[:, 1:2], sm[:, 0:1], 1.0 / D, 1e-6, ALU.mult, ALU.add), reads=[b_sm], writes=[b_sm])
        p.op("act", lambda e: e.sqrt(sm[:, 2:3], sm[:, 1:2]), reads=[b_sm], writes=[b_sm])
        p.op("dve", lambda e: e.reciprocal(sm[:, 3:4], sm[:, 2:3]), reads=[b_sm], writes=[b_sm])
        p.op("dve", lambda e: e.scalar_tensor_tensor(A2[:], in0=A1[:], scalar=sm[:, 3:4], in1=MOD[:, s, 2, :],
                                                     op0=ALU.mult, op1=ALU.mult), reads=[b_A1, b_sm, b_mod], writes=[b_A2])
        p.op("dve", lambda e: e.tensor_tensor(A2[:], A2[:], MOD[:, s, 1, :], ALU.add), reads=[b_A2, b_mod], writes=[b_A2])
        transpose8(A2, b_A2, A3, b_A3)
        for nb in range(4):
            blk, bb = wst.get()
            for kc in range(8):
                p.op("pe", lambda e, kc=kc: e.matmul(ps_a[nb % 2][:], lhsT=A3[:, kc * 128:(kc + 1) * 128], rhs=blk[:, kc, :],
                                                     start=(kc == 0), stop=(kc == 7)),
                     reads=[b_A3, bb], writes=[b_a[nb % 2]])
            p.op("act", lambda e: e.copy(A4[:, nb * 512:(nb + 1) * 512], ps_a[nb % 2][:]), reads=[b_a[nb % 2]], writes=[b_A4])
        for half in range(2):
            transpose8(A4[:, half * 1024:(half + 1) * 1024], b_A4, A5[:, half * 1024:(half + 1) * 1024], b_A5)
        for (ks, sd, bsd, part) in ((k1s, s1, b_s1, 0), (k2s, s2, b_s2, 1)):
            for h in range(8):
                c = 2 * h + part
                p.op("pe", lambda e, h=h, c=c: e.matmul(ps_acc[:, h * 128:(h + 1) * 128], lhsT=A5[:, c * 128:(c + 1) * 128],
                                                        rhs=ks[:, h, :], start=True, stop=True),
                     reads=[b_A5, b_k], writes=[b_acc])
            p.op("act", lambda e: e.copy(sd[:], ps_acc[:]), reads=[b_acc], writes=[bsd])
        for (sd, bsd, vv) in ((s1, b_s1, v1), (s2, b_s2, v2)):
            for h in range(8):
                sl = sd[:, h * 128:(h + 1) * 128]
                p.op("dve", lambda e: e.max(out=vv[:, h, 0:8], in_=sl), reads=[bsd], writes=[b_v])
                p.op("dve", lambda e: e.match_replace(out=wk[:, 0:128], in_to_replace=vv[:, h, 0:8], in_values=sl, imm_value=-1e30),
                     reads=[bsd, b_v], writes=[b_wk])
                p.op("dve", lambda e: e.max(out=vv[:, h, 8:16], in_=wk[:, 0:128]), reads=[b_wk], writes=[b_v])
        cand = A5[:, 0:2048].rearrange("p (h a b) -> p h a b", h=8, a=16)
        p.op("dve", lambda e: e.tensor_tensor(cand, v1[:].unsqueeze(3).to_broadcast([128, 8, 16, 16]),
                                              v2[:].unsqueeze(2).to_broadcast([128, 8, 16, 16]), ALU.add),
             reads=[b_v, b_A5], writes=[b_A5])
        for h in range(8):
            cs = A5[:, h * 256:(h + 1) * 256]
            p.op("dve", lambda e: e.max(out=c24[:, h, 0:8], in_=cs), reads=[b_A5], writes=[b_c])
            p.op("dve", lambda e: e.match_replace(out=wk[:], in_to_replace=c24[:, h, 0:8], in_values=cs, imm_value=-1e30),
                 reads=[b_A5, b_c], writes=[b_wk])
            p.op("dve", lambda e: e.max(out=c24[:, h, 8:16], in_=wk[:]), reads=[b_wk], writes=[b_c])
            p.op("dve", lambda e: e.match_replace(out=wk[:], in_to_replace=c24[:, h, 8:16], in_values=wk[:], imm_value=-1e30),
                 reads=[b_wk, b_c], writes=[b_wk])
            p.op("dve", lambda e: e.max(out=c24[:, h, 16:24], in_=wk[:]), reads=[b_wk], writes=[b_c])
        tau = sm[:, 8:16]; zs = sm[:, 16:24]; rz = sm[:, 24:32]
        p.op("dve", lambda e: e.tensor_tensor(tau, c24[:, :, 15], c24[:, :, 16], ALU.add), reads=[b_c], writes=[b_sm])
        p.op("dve", lambda e: e.tensor_scalar(tau, tau, 0.5, None, ALU.mult), reads=[b_sm], writes=[b_sm])
        ex = Mw[0][:, 0:128].rearrange("p (h a) -> p h a", h=8)
        p.op("dve", lambda e: e.tensor_tensor(ex, c24[:, :, 0:16], bc_last(c24[:, :, 0], 16), ALU.subtract),
             reads=[b_c], writes=[b_Mw[0]])
        p.op("act", lambda e: e.activation(ex, ex, AF.Exp), reads=[b_Mw[0]], writes=[b_Mw[0]])
        p.op("dve", lambda e: e.tensor_reduce(zs, ex, AX.X, ALU.add), reads=[b_Mw[0]], writes=[b_sm])
        p.op("dve", lambda e: e.reciprocal(rz, zs), reads=[b_sm], writes=[b_sm])
        s1v = s1[:].rearrange("p (h i) -> p h i", h=8); s2v = s2[:].rearrange("p (h i) -> p h i", h=8)
        e1v = e1[:].rearrange("p (h i) -> p h i", h=8); e2v = e2[:].rearrange("p (h i) -> p h i", h=8)
        p.op("dve", lambda e: e.tensor_tensor(e1v, s1v, bc_last(v1[:, :, 0], 128), ALU.subtract), reads=[b_s1, b_v], writes=[b_e1])
        p.op("act", lambda e: e.activation(e1[:], e1[:], AF.Exp), reads=[b_e1], writes=[b_e1])
        p.op("dve", lambda e: e.tensor_tensor(e1v, e1v, bc_last(rz, 128), ALU.mult), reads=[b_e1, b_sm], writes=[b_e1])
        p.op("dve", lambda e: e.tensor_tensor(e2v, s2v, bc_last(v2[:, :, 0], 128), ALU.subtract), reads=[b_s2, b_v], writes=[b_e2])
        p.op("act", lambda e: e.activation(e2[:], e2[:], AF.Exp), reads=[b_e2], writes=[b_e2])
        p.op("dve", lambda e: e.scalar_tensor_tensor(s1v, in0=s1v, scalar=-1.0, in1=bc_last(tau, 128), op0=ALU.mult, op1=ALU.add),
             reads=[b_s1, b_sm], writes=[b_s1])
        for g in range(32):
            gi = g % 2
            ublk, ub = wst.get()
            for kc in range(8):
                p.op("pe", lambda e, kc=kc: e.matmul(ps_a[gi][:], lhsT=A3[:, kc * 128:(kc + 1) * 128], rhs=ublk[:, kc, :],
                                                     start=(kc == 0), stop=(kc == 7)),
                     reads=[b_A3, ub], writes=[b_a[gi]])
            p.op("act", lambda e: e.activation(actt[gi][:], ps_a[gi][:], AF.Gelu_apprx_tanh), reads=[b_a[gi]], writes=[b_act[gi]])
            for ib in range(4):
                i = g * 4 + ib
                mi = i % 2
                M = Mw[mi][:].rearrange("p (h j) -> p h j", h=8)
                p.op("dve", lambda e: e.tensor_tensor(M, s2v, bc_last(s1v[:, :, i], 128), ALU.is_ge), reads=[b_s2, b_s1], writes=[b_Mw[mi]])
                p.op("dve", lambda e: e.tensor_tensor(M, M, e2v, ALU.mult), reads=[b_Mw[mi], b_e2], writes=[b_Mw[mi]])
                p.op("dve", lambda e: e.tensor_tensor(M, M, bc_last(e1v[:, :, i], 128), ALU.mult), reads=[b_Mw[mi], b_e1], writes=[b_Mw[mi]])
                p.op("dve", lambda e: e.tensor_reduce(Wacc[gi][:, ib * 128:(ib + 1) * 128], Mw[mi][:].rearrange("p (h j) -> p j h", h=8),
                                                      AX.X, ALU.add), reads=[b_Mw[mi]], writes=[b_W[gi]])
            p.op("dve", lambda e: e.tensor_tensor(actt[gi][:], actt[gi][:], Wacc[gi][:], ALU.mult), reads=[b_act[gi], b_W[gi]], writes=[b_act[gi]])
            for ib in range(4):
                p.op("pe", lambda e, ib=ib: e.transpose(ps_t[gi][:, ib * 128:(ib + 1) * 128], actt[gi][:, ib * 128:(ib + 1) * 128], ident[:]),
                     reads=[b_act[gi], b_id], writes=[b_t[gi]])
            p.op("act", lambda e: e.copy(ATt[gi][:], ps_t[gi][:]), reads=[b_t[gi]], writes=[b_AT[gi]])
            vblk, vb = vst.get()
            for ib in range(4):
                for nb in range(2):
                    p.op("pe", lambda e, ib=ib, nb=nb: e.matmul(ps_acc[:, nb * 512:(nb + 1) * 512], lhsT=ATt[gi][:, ib * 128:(ib + 1) * 128],
                                                                rhs=vblk[:, ib, nb * 512:(nb + 1) * 512],
                                                                start=(g == 0 and ib == 0), stop=(g == 31 and ib == 3)),
                         reads=[b_AT[gi], vb], writes=[b_acc])
        p.op("dve", lambda e: e.tensor_tensor(A2[:], ps_acc[:], MOD[:, s, 3, :], ALU.mult), reads=[b_acc, b_mod], writes=[b_A2])
        p.op("dve", lambda e: e.tensor_tensor(A1[:], A1[:], A2[:], ALU.add), reads=[b_A1, b_A2], writes=[b_A1])
        if final:
            p.op("act", lambda e: e.activation(A2[:], A1[:], AF.Square, accum_out=sm[:, 0:1]), reads=[b_A1], writes=[b_A2, b_sm])
            p.op("dve", lambda e: e.tensor_scalar(sm[:, 1:2], sm[:, 0:1], 1.0 / D, 1e-6, ALU.mult, ALU.add), reads=[b_sm], writes=[b_sm])
            p.op("act", lambda e: e.sqrt(sm[:, 2:3], sm[:, 1:2]), reads=[b_sm], writes=[b_sm])
            p.op("dve", lambda e: e.reciprocal(sm[:, 3:4], sm[:, 2:3]), reads=[b_sm], writes=[b_sm])
            p.op("dve", lambda e: e.scalar_tensor_tensor(A1[:], in0=A1[:], scalar=sm[:, 3:4], in1=n2b[:], op0=ALU.mult, op1=ALU.mult),
                 reads=[b_A1, b_sm, b_n2], writes=[b_A1])
        p.dma("sp", xo[r0:r0 + 128, :], A1[:], reads=[b_A1])
    p.finish("sp")
    p.close()
    return nc


class Tl:
    def __init__(self, t):
        self.t = t
        self.b = Buf()

    def __getitem__(self, k):
        return self.t[k]


def mk(p, name, shape, dt=F32):
    return Tl(p.sb(name, shape, dt))


def mkps(p, name, shape):
    return Tl(p.ps(name, shape))


def O(p, e, fn, outs, ins):
    return p.op(e, fn, reads=[i.b for i in ins], writes=[o.b for o in outs])


def adaln_chunks(p, cond2, mod_w, mod_b, chunks, MOD, psA, name):
    csil = mk(p, name + "csil", [128, 2, 8])
    p.dma("sp", csil[:], cond2.rearrange("s (kc p) -> p s kc", p=128), writes=[csil.b], allow_slow_non_contiguous=True)
    O(p, "act", lambda e: e.activation(csil[:], csil[:], AF.Silu), [csil], [csil])
    CB = mk(p, name + "CB", [128, 2, 8, 128])
    O(p, "dve", lambda e: e.tensor_copy(CB[:], bc_last(csil[:], 128)), [CB], [csil])
    tmpb = mk(p, name + "tmpb", [128, 1024])
    wb = [mk(p, name + "mwb%d" % i, [128, 8, 512]) for i in range(2)]
    k = 0
    for j, ch in enumerate(chunks):
        p.dma("sp", tmpb[:], mod_b[ch * D:(ch + 1) * D].partition_broadcast(128), reads=[tmpb.b], writes=[tmpb.b])
        for nb in range(2):
            c0 = ch * D + nb * 512
            blk = wb[k % 2]; k += 1
            p.dma("sp", blk[:], mod_w[:, c0:c0 + 512].rearrange("(kc p) n -> p kc n", p=128), writes=[blk.b])
            for s in range(2):
                for kc in range(8):
                    O(p, "pe", lambda e, s=s, kc=kc: e.matmul(psA[s][:], lhsT=CB[:, s, kc, :], rhs=blk[:, kc, :],
                                                             start=(kc == 0), stop=(kc == 7)), [psA[s]], [CB, blk])
                O(p, "dve", lambda e, s=s: e.tensor_tensor(MOD[:, s, j, nb * 512:(nb + 1) * 512], psA[s][:],
                                                           tmpb[:, nb * 512:(nb + 1) * 512], ALU.add), [MOD], [psA[s], tmpb])


def rms_mod(p, X, H, sm, G, SH, s):
    (Gt, gj), (St, sj) = G, SH
    O(p, "act", lambda e: e.activation(H[:], X[:], AF.Square, accum_out=sm[:, 0:1]), [H, sm], [X])
    O(p, "dve", lambda e: e.tensor_scalar(sm[:, 1:2], sm[:, 0:1], 1.0 / D, 1e-6, ALU.mult, ALU.add), [sm], [sm])
    O(p, "act", lambda e: e.sqrt(sm[:, 2:3], sm[:, 1:2]), [sm], [sm])
    O(p, "dve", lambda e: e.reciprocal(sm[:, 3:4], sm[:, 2:3]), [sm], [sm])
    O(p, "dve", lambda e: e.scalar_tensor_tensor(H[:], in0=X[:], scalar=sm[:, 3:4], in1=Gt[:, s, gj, :], op0=ALU.mult, op1=ALU.mult),
      [H], [X, sm, Gt])
    O(p, "dve", lambda e: e.tensor_tensor(H[:], H[:], St[:, s, sj, :], ALU.add), [H], [H, St])


def transpose_blocks(p, src, dst, psM, ident, nblk):
    for c in range(nblk):
        O(p, "pe", lambda e, c=c: e.transpose(psM[:, c * 128:(c + 1) * 128], src[:, c * 128:(c + 1) * 128], ident[:]), [psM], [src, ident])
    h = nblk * 64
    O(p, "act", lambda e: e.copy(dst[:, 0:h], psM[:, 0:h]), [dst], [psM])
    O(p, "dve", lambda e: e.tensor_copy(dst[:, h:2 * h], psM[:, h:2 * h]), [dst], [psM])


NW = 2064
NO1 = 21


def build_A1(n_ctx, n_lat):
    NT = n_ctx + n_lat
    nc = bass.Bass("TRN2", target_bir_lowering=False)
    def ein(name, shape):
        return nc.dram_tensor(name, list(shape), F32, kind="ExternalInput").ap()
    xs = ein("xs", [NT * 128, D])
    cond2 = ein("cond2", [2, D])
    mod_w = ein("mod_w", [D, 6 * D]); mod_b = ein("mod_b", [6 * D]); norm1 = ein("norm1", [D])
    Wg = ein("Wg", [D, NW])
    convw = ein("convw", [3 * 768]); mu = ein("mu", [2 * 1024])
    alog = ein("alog", [8]); dtb = ein("dtb", [8])
    w0 = ein("w0", [512]); a0 = ein("a0", [512]); k_k = ein("k_k", [256]); k_a = ein("k_a", [256]); r_k = ein("r_k", [256])
    w2 = ein("w2", [64, 512]); a2 = ein("a2", [64, 256]); g2 = ein("g2", [128, 256])
    ident_d = ein("ident_in", [128, 128])
    out = nc.dram_tensor("o1", [NT * 128, NO1 * 256], F32, kind="ExternalOutput").ap()
    p = Prog(nc)
    Pd = p.dram("Pd", [NT * 128 + 4, NW]); b_Pd = Buf()
    ident = mk(p, "ident", [128, 128])
    p.dma("sp", ident[:], ident_d[:, :], writes=[ident.b])
    psA = [mkps(p, "psA%d" % i, [128, 512]) for i in range(4)]
    psM = mkps(p, "psM", [128, 1024])
    psL = mkps(p, "psL", [128, 1024])
    MOD = mk(p, "MOD", [128, 2, 2, 1024])
    p.push_scope()
    adaln_chunks(p, cond2, mod_w, mod_b, [0, 1], MOD, psA, "ad")
    n1b = mk(p, "n1b", [128, 1024])
    p.dma("sp", n1b[:], norm1.partition_broadcast(128), writes=[n1b.b])
    for s in range(2):
        O(p, "dve", lambda e, s=s: e.scalar_tensor_tensor(MOD[:, s, 1, :], in0=MOD[:, s, 1, :], scalar=1.0, in1=n1b[:],
                                                          op0=ALU.add, op1=ALU.mult), [MOD], [MOD, n1b])
    Ws = mk(p, "Ws", [128, 8, NW])
    for kc in range(8):
        p.dma("sp" if kc % 2 == 0 else "pool", Ws[:, kc, :], Wg[kc * 128:(kc + 1) * 128, :], writes=[Ws.b])
    X = mk(p, "X", [128, 1024]); H = mk(p, "H", [128, 1024]); HT = mk(p, "HT", [128, 1024]); sm = mk(p, "sm", [128, 64])
    Pt = [mk(p, "Pt%d" % i, [128, NW]) for i in range(1)]
    zrow = mk(p, "zrow", [1, NW])
    O(p, "dve", lambda e: e.memset(zrow[:], 0.0), [zrow], [])
    rowof = lambda ti: (1 + ti * 128) if ti < n_ctx else (3 + ti * 128)
    for r in (0, n_ctx * 128 + 1, n_ctx * 128 + 2, NT * 128 + 3):
        p.dma("sp", Pd[r:r + 1, :], zrow[:], reads=[zrow.b], writes=[b_Pd])
    for ti in range(NT):
        s = 1 if ti < n_ctx else 0
        p.dma("sp", X[:], xs[ti * 128:(ti + 1) * 128, :], writes=[X.b])
        rms_mod(p, X, H, sm, (MOD, 1), (MOD, 0), s)
        transpose_blocks(p, H, HT, psM, ident, 8)
        PT = Pt[0]
        for nb in range(5):
            c0 = nb * 512; cw = min(512, NW - c0)
            ps = psA[nb % 4]
            for kc in range(8):
                O(p, "pe", lambda e, kc=kc: e.matmul(ps[:, 0:cw], lhsT=HT[:, kc * 128:(kc + 1) * 128], rhs=Ws[:, kc, c0:c0 + cw],
                                                     start=(kc == 0), stop=(kc == 7)), [ps], [HT, Ws])
            O(p, "act" if nb % 2 == 0 else "dve",
              (lambda e: e.copy(PT[:, c0:c0 + cw], ps[:, 0:cw])) if nb % 2 == 0 else (lambda e: e.tensor_copy(PT[:, c0:c0 + cw], ps[:, 0:cw])),
              [PT], [ps])
        r0 = rowof(ti)
        p.dma("sp", Pd[r0:r0 + 128, :], PT[:], reads=[PT.b], writes=[b_Pd])
    p.pop_scope()
    def bload(name, src, n):
        t = mk(p, name, [128, n])
        p.dma("sp", t[:], src.partition_broadcast(128), writes=[t.b])
        return t
    cw_t = bload("cw_t", convw, 3 * 768); mu_t = bload("mu_t", mu, 2048)
    al_t = bload("al_t", alog, 8); dtb_t = bload("dtb_t", dtb, 8)
    w0_t = bload("w0_t", w0, 512); a0_t = bload("a0_t", a0, 512)
    kk_t = bload("kk_t", k_k, 256); ka_t = bload("ka_t", k_a, 256); rk_t = bload("rk_t", r_k, 256)
    O(p, "act", lambda e: e.activation(al_t[:], al_t[:], AF.Exp), [al_t], [al_t])
    O(p, "dve", lambda e: e.tensor_scalar(al_t[:], al_t[:], -1.0, None, ALU.mult), [al_t], [al_t])
    lw = mk(p, "lw", [128, 768])
    p.dma("sp", lw[0:64, 0:512], w2[:, :], writes=[lw.b])
    p.dma("sp", lw[64:128, 0:256], a2[:, :], writes=[lw.b])
    p.dma("sp", lw[:, 512:768], g2[:, :], writes=[lw.b])
    Pc = mk(p, "Pc", [128, NW]); Pp = mk(p, "Pp", [128, 2048]); Pn = mk(p, "Pn", [128, 2048])
    T1 = mk(p, "T1", [128, 1024]); T2 = mk(p, "T2", [128, 1024]); Q = mk(p, "Q", [128, 768]); PR = mk(p, "PR", [128, 1024])
    OUT = [mk(p, "OUT%d" % i, [128, NO1 * 256]) for i in range(1)]
    L = mk(p, "L", [128, 256]); LT = mk(p, "LT", [128, 256])
    s8 = mk(p, "s8", [128, 64])
    def v3(ap):
        return ap.rearrange("p (h k) -> p h k", k=64)
    for ti in range(NT):
        r0 = rowof(ti)
        Ot = OUT[0]
        oc = lambda j: Ot[:, j * 256:(j + 1) * 256]
        p.dma("sp", Pc[:], Pd[r0:r0 + 128, :], reads=[b_Pd], writes=[Pc.b])
        p.dma("pool", Pp[:], Pd[r0 - 1:r0 + 127, 0:2048], reads=[b_Pd], writes=[Pp.b])
        p.dma("pool", Pn[:], Pd[r0 + 1:r0 + 129, 0:2048], reads=[b_Pd], writes=[Pn.b])
        O(p, "dve", lambda e: e.tensor_tensor(Q[:], Pp[:, 0:768], cw_t[:, 0:768], ALU.mult), [Q], [Pp, cw_t])
        O(p, "dve", lambda e: e.tensor_tensor(T1[:, 0:768], Pc[:, 0:768], cw_t[:, 768:1536], ALU.mult), [T1], [Pc, cw_t])
        O(p, "dve", lambda e: e.tensor_tensor(Q[:], Q[:], T1[:, 0:768], ALU.add), [Q], [Q, T1])
        O(p, "dve", lambda e: e.tensor_tensor(T1[:, 0:768], Pn[:, 0:768], cw_t[:, 1536:2304], ALU.mult), [T1], [Pn, cw_t])
        O(p, "dve", lambda e: e.tensor_tensor(Q[:], Q[:], T1[:, 0:768], ALU.add), [Q], [Q, T1])
        O(p, "act", lambda e: e.activation(Q[:], Q[:], AF.Silu), [Q], [Q])
        O(p, "act", lambda e: e.activation(T1[:, 0:512], Q[:, 0:512], AF.Square), [T1], [Q])
        O(p, "dve", lambda e: e.tensor_reduce(s8[:, 0:8], v3(T1[:, 0:512]), AX.X, ALU.add), [s8], [T1])
        O(p, "dve", lambda e: e.tensor_scalar(s8[:, 0:8], s8[:, 0:8], 1e-6, None, ALU.add), [s8], [s8])
        O(p, "act", lambda e: e.sqrt(s8[:, 0:8], s8[:, 0:8]), [s8], [s8])
        O(p, "dve", lambda e: e.reciprocal(s8[:, 0:8], s8[:, 0:8]), [s8], [s8])
        O(p, "dve", lambda e: e.scalar_tensor_tensor(v3(oc(1)), in0=v3(Q[:, 0:256]), scalar=0.125, in1=bc_last(s8[:, 0:4], 64),
                                                     op0=ALU.mult, op1=ALU.mult), [Ot], [Q, s8])
        O(p, "dve", lambda e: e.tensor_tensor(v3(T2[:, 0:256]), v3(Q[:, 256:512]), bc_last(s8[:, 4:8], 64), ALU.mult), [T2], [Q, s8])
        O(p, "dve", lambda e: e.tensor_scalar(oc(0), T2[:, 0:256], -1.0, None, ALU.mult), [Ot], [T2])
        O(p, "act", lambda e: e.copy(oc(2), Q[:, 512:768]), [Ot], [Q])
        O(p, "dve", lambda e: e.tensor_tensor(s8[:, 8:16], Pc[:, 2048:2056], dtb_t[:], ALU.add), [s8], [Pc, dtb_t])
        O(p, "act", lambda e: e.activation(s8[:, 8:16], s8[:, 8:16], AF.Exp), [s8], [s8])
        O(p, "act", lambda e: e.activation(s8[:, 8:16], s8[:, 8:16], AF.Ln, bias=1.0), [s8], [s8])
        O(p, "dve", lambda e: e.tensor_tensor(s8[:, 8:16], s8[:, 8:16], al_t[:], ALU.mult), [s8], [s8, al_t])
        O(p, "act", lambda e: e.activation(s8[:, 8:16], s8[:, 8:16], AF.Exp), [s8], [s8])
        O(p, "act", lambda e: e.activation(s8[:, 16:24], Pc[:, 2056:2064], AF.Sigmoid), [s8], [Pc])
        for d in range(2):
            O(p, "dve", lambda e, d=d: e.tensor_copy(v3(oc(3 + 3 * d)), bc_last(s8[:, 8 + 4 * d:12 + 4 * d], 64)), [Ot], [s8])
            O(p, "dve", lambda e, d=d: e.tensor_tensor(v3(oc(5 + 3 * d)), v3(T2[:, 0:256]), bc_last(s8[:, 16 + 4 * d:20 + 4 * d], 64), ALU.mult),
              [Ot], [T2, s8])
            O(p, "dve", lambda e, d=d: e.tensor_tensor(v3(oc(4 + 3 * d)), v3(oc(5 + 3 * d)), bc_last(s8[:, 8 + 4 * d:12 + 4 * d], 64), ALU.mult),
              [Ot], [Ot, s8])
        O(p, "act", lambda e: e.activation(oc(18), Pc[:, 768:1024], AF.Silu), [Ot], [Pc])
        O(p, "dve", lambda e: e.tensor_tensor(T1[:], Pp[:, 1024:2048], Pc[:, 1024:2048], ALU.subtract), [T1], [Pp, Pc])
        O(p, "dve", lambda e: e.tensor_tensor(T1[:], T1[:], mu_t[:, 0:1024], ALU.mult), [T1], [T1, mu_t])
        O(p, "dve", lambda e: e.tensor_tensor(PR[:], Pc[:, 1024:2048], T1[:], ALU.add), [PR], [Pc, T1])
        O(p, "dve", lambda e: e.tensor_tensor(T1[:], Pn[:, 1024:2048], Pc[:, 1024:2048], ALU.subtract), [T1], [Pn, Pc])
        O(p, "dve", lambda e: e.tensor_tensor(T1[:], T1[:], mu_t[:, 1024:2048], ALU.mult), [T1], [T1, mu_t])
        O(p, "dve", lambda e: e.tensor_tensor(PR[:], PR[:], T1[:], ALU.add), [PR], [PR, T1])
        O(p, "act", lambda e: e.copy(oc(10), PR[:, 0:256]), [Ot], [PR])
        O(p, "act", lambda e: e.copy(oc(11), PR[:, 512:768]), [Ot], [PR])
        O(p, "dve", lambda e: e.tensor_tensor(T2[:, 256:512], PR[:, 256:512], kk_t[:], ALU.mult), [T2], [PR, kk_t])
        O(p, "act", lambda e: e.activation(T1[:, 0:256], T2[:, 256:512], AF.Square), [T1], [T2])
        O(p, "dve", lambda e: e.tensor_reduce(s8[:, 24:28], v3(T1[:, 0:256]), AX.X, ALU.add), [s8], [T1])
        O(p, "dve", lambda e: e.tensor_scalar(s8[:, 24:28], s8[:, 24:28], 1e-6, None, ALU.add), [s8], [s8])
        O(p, "act", lambda e: e.sqrt(s8[:, 24:28], s8[:, 24:28]), [s8], [s8])
        O(p, "dve", lambda e: e.reciprocal(s8[:, 24:28], s8[:, 24:28]), [s8], [s8])
        O(p, "dve", lambda e: e.tensor_tensor(v3(T2[:, 256:512]), v3(T2[:, 256:512]), bc_last(s8[:, 24:28], 64), ALU.mult), [T2], [T2, s8])
        O(p, "dve", lambda e: e.tensor_scalar(oc(9), T2[:, 256:512], -1.0, None, ALU.mult), [Ot], [T2])
        O(p, "act", lambda e: e.activation(L[:, 0:64], PR[:, 768:832], AF.Tanh), [L], [PR])
        O(p, "act", lambda e: e.copy(L[:, 64:128], PR[:, 832:896]), [L], [PR])
        O(p, "act", lambda e: e.activation(L[:, 128:256], PR[:, 896:1024], AF.Sigmoid), [L], [PR])
        transpose_blocks(p, L, LT, psM, ident, 2)
        for d in range(2):
            O(p, "pe", lambda e, d=d: e.matmul(psL[:, d * 256:(d + 1) * 256], lhsT=LT[0:64, 0:128], rhs=lw[0:64, d * 256:(d + 1) * 256],
                                               start=True, stop=True), [psL], [LT, lw])
        O(p, "pe", lambda e: e.matmul(psL[:, 512:768], lhsT=LT[64:128, 0:128], rhs=lw[64:128, 0:256], start=True, stop=True), [psL], [LT, lw])
        O(p, "pe", lambda e: e.matmul(psL[:, 768:1024], lhsT=LT[:, 128:256], rhs=lw[:, 512:768], start=True, stop=True), [psL], [LT, lw])
        O(p, "act", lambda e: e.copy(oc(20), psL[:, 768:1024]), [Ot], [psL])
        O(p, "dve", lambda e: e.tensor_tensor(T1[:, 512:768], PR[:, 0:256], rk_t[:], ALU.mult), [T1], [PR, rk_t])
        for d in range(2):
            O(p, "dve", lambda e, d=d: e.tensor_tensor(T1[:, 0:256], psL[:, d * 256:(d + 1) * 256], w0_t[:, d * 256:(d + 1) * 256], ALU.add),
              [T1], [psL, w0_t])
            O(p, "act", lambda e: e.activation(T1[:, 0:256], T1[:, 0:256], AF.Exp, scale=-1.0), [T1], [T1])
            O(p, "dve", lambda e: e.tensor_scalar(T1[:, 0:256], T1[:, 0:256], 1.0, None, ALU.add), [T1], [T1])
            O(p, "dve", lambda e: e.reciprocal(T1[:, 0:256], T1[:, 0:256]), [T1], [T1])
            O(p, "act", lambda e, d=d: e.activation(oc(12 + 3 * d), T1[:, 0:256], AF.Exp, scale=-0.6065306597126334), [Ot], [T1])
            O(p, "dve", lambda e, d=d: e.tensor_tensor(T1[:, 256:512], psL[:, 512:768], a0_t[:, d * 256:(d + 1) * 256], ALU.add), [T1], [psL, a0_t])
            O(p, "act", lambda e: e.activation(T1[:, 256:512], T1[:, 256:512], AF.Sigmoid), [T1], [T1])
            O(p, "dve", lambda e, d=d: e.tensor_tensor(oc(13 + 3 * d), T2[:, 256:512], T1[:, 256:512], ALU.mult), [Ot], [T2, T1])
            O(p, "dve", lambda e: e.scalar_tensor_tensor(T1[:, 256:512], in0=T1[:, 256:512], scalar=-1.0, in1=ka_t[:], op0=ALU.add, op1=ALU.mult),
              [T1], [T1, ka_t])
            O(p, "dve", lambda e, d=d: e.scalar_tensor_tensor(oc(14 + 3 * d), in0=T1[:, 256:512], scalar=1.0, in1=PR[:, 256:512], op0=ALU.add, op1=ALU.mult),
              [Ot], [T1, PR])
            O(p, "dve", lambda e, d=d: e.tensor_tensor(T1[:, 768:1024], T1[:, 512:768], oc(14 + 3 * d), ALU.mult), [T1], [T1, Ot])
            O(p, "dve", lambda e, d=d: e.tensor_reduce(s8[:, 32 + 4 * d:36 + 4 * d], v3(T1[:, 768:1024]), AX.X, ALU.add), [s8], [T1])
        O(p, "dve", lambda e: e.tensor_tensor(s8[:, 32:36], s8[:, 32:36], s8[:, 36:40], ALU.add), [s8], [s8])
        O(p, "dve", lambda e: e.tensor_tensor(v3(oc(19)), v3(PR[:, 512:768]), bc_last(s8[:, 32:36], 64), ALU.mult), [Ot], [PR, s8])
        p.dma("sp", out[ti * 128:(ti + 1) * 128, :], Ot[:], reads=[Ot.b])
    p.finish("sp")
    p.close()
    return nc


TB = 4
TBV = 128


def build_A2(S_steps):
    nc = bass.Bass("TRN2", target_bir_lowering=False)
    names = ["sc_nk", "sc_w", "sc_bb", "sc_kt", "sc_r"]
    sc = [nc.dram_tensor(n, [2, S_steps, 512], F32, kind="ExternalInput").ap() for n in names]
    vt = nc.dram_tensor("vt", [128, S_steps, 8], F32, kind="ExternalInput").ap()
    yo = nc.dram_tensor("yo", [128, S_steps, 8], F32, kind="ExternalOutput").ap()
    p = Prog(nc)
    BR = [[mk(p, "br%d_%d" % (q, i), [128, TB, 512]) for i in range(2)] for q in range(5)]
    Vc = [mk(p, "vc%d" % i, [128, TBV, 8]) for i in range(2)]
    Yb = [mk(p, "yb%d" % i, [128, TBV, 8]) for i in range(2)]
    St = mk(p, "St", [128, 512]); tmp = mk(p, "tmp", [128, 512]); sa = mk(p, "sa", [128, 8])
    O(p, "dve", lambda e: e.memset(St[:], 0.0), [St], [])
    v3 = lambda ap: ap.rearrange("p (g k) -> p g k", k=64)
    nblk = S_steps // TB

    def load_blk(bi):
        for q in range(5):
            t = BR[q][bi % 2]
            for m in range(2):
                src = sc[q][m, bi * TB:(bi + 1) * TB, :].rearrange("s n -> (s n)").partition_broadcast(64)
                p.dma("sp" if m == 0 else "act", t[m * 64:(m + 1) * 64, :, :].rearrange("p s n -> p (s n)"), src, writes=[t.b])

    load_blk(0)
    for bi in range(nblk):
        if bi + 1 < nblk:
            load_blk(bi + 1)
        for j in range(TB):
            s = bi * TB + j
            vb = s // TBV
            if s % TBV == 0:
                p.dma("pool", Vc[vb % 2][:], vt[:, s:s + TBV, :], writes=[Vc[vb % 2].b])
            V_ = Vc[vb % 2]; Y_ = Yb[vb % 2]
            NK, W, BB, KT, R = [BR[q][bi % 2] for q in range(5)]
            O(p, "dve", lambda e: e.tensor_tensor(tmp[:], St[:], NK[:, j, :], ALU.mult), [tmp], [St, NK])
            O(p, "dve", lambda e: e.tensor_reduce(sa[:], v3(tmp[:]), AX.X, ALU.add), [sa], [tmp])
            O(p, "dve", lambda e: e.tensor_tensor(St[:], St[:], W[:, j, :], ALU.mult), [St], [St, W])
            O(p, "dve", lambda e: e.tensor_tensor(v3(tmp[:]), v3(BB[:, j, :]), bc_last(sa[:], 64), ALU.mult), [tmp], [BB, sa])
            O(p, "dve", lambda e: e.tensor_tensor(St[:], St[:], tmp[:], ALU.add), [St], [St, tmp])
            O(p, "dve", lambda e: e.tensor_tensor(v3(tmp[:]), v3(KT[:, j, :]), bc_last(V_[:, s % TBV, :], 64), ALU.mult), [tmp], [KT, V_])
            O(p, "dve", lambda e: e.tensor_tensor(St[:], St[:], tmp[:], ALU.add), [St], [St, tmp])
            O(p, "dve", lambda e: e.tensor_tensor(tmp[:], St[:], R[:, j, :], ALU.mult), [tmp], [St, R])
            O(p, "dve", lambda e: e.tensor_reduce(Y_[:, s % TBV, :], v3(tmp[:]), AX.X, ALU.add), [Y_], [tmp])
            if s % TBV == TBV - 1:
                p.dma("pool", yo[:, s - TBV + 1:s + 1, :], Y_[:], reads=[Y_.b])
    p.finish("sp")
    p.close()
    return nc


def build_A3(NT):
    nc = bass.Bass("TRN2", target_bir_lowering=False)
    def ein(name, shape):
        return nc.dram_tensor(name, list(shape), F32, kind="ExternalInput").ap()
    yin = ein("yin", [NT * 128, 4 * 256])
    aux = ein("aux", [NT * 128, 3 * 256])
    gnw = ein("gnw", [256]); lnw = ein("lnw", [256]); lnb = ein("lnb", [256])
    out = nc.dram_tensor("feat", [NT * 128, 512], F32, kind="ExternalOutput").ap()
    p = Prog(nc)
    def bload(name, src, n):
        t = mk(p, name, [128, n])
        p.dma("sp", t[:], src.partition_broadcast(128), writes=[t.b])
        return t
    gn_t = bload("gn_t", gnw, 256); lw_t = bload("lw_t", lnw, 256); lb_t = bload("lb_t", lnb, 256)
    Y = [mk(p, "Y%d" % i, [128, 1024]) for i in range(2)]; A = [mk(p, "A%d" % i, [128, 768]) for i in range(2)]
    Fo = [mk(p, "Fo%d" % i, [128, 512]) for i in range(2)]
    o = mk(p, "o", [128, 256]); t1 = mk(p, "t1", [128, 256]); s8 = mk(p, "s8", [128, 16])
    v3 = lambda ap: ap.rearrange("p (h k) -> p h k", k=64)
    for ti in range(NT):
        Yt = Y[ti % 2]; At = A[ti % 2]; F = Fo[ti % 2]
        p.dma("sp", Yt[:], yin[ti * 128:(ti + 1) * 128, :], writes=[Yt.b])
        p.dma("pool", At[:], aux[ti * 128:(ti + 1) * 128, :], writes=[At.b])
        O(p, "dve", lambda e: e.tensor_tensor(o[:], Yt[:, 0:256], Yt[:, 256:512], ALU.add), [o], [Yt])
        O(p, "act", lambda e: e.activation(t1[:], o[:], AF.Square), [t1], [o])
        O(p, "dve", lambda e: e.tensor_reduce(s8[:, 0:4], v3(t1[:]), AX.X, ALU.add), [s8], [t1])
        O(p, "dve", lambda e: e.tensor_scalar(s8[:, 0:4], s8[:, 0:4], 1.0 / 64, 1e-6, ALU.mult, ALU.add), [s8], [s8])
        O(p, "act", lambda e: e.sqrt(s8[:, 0:4], s8[:, 0:4]), [s8], [s8])
        O(p, "dve", lambda e: e.reciprocal(s8[:, 0:4], s8[:, 0:4]), [s8], [s8])
        O(p, "dve", lambda e: e.tensor_tensor(v3(o[:]), v3(o[:]), bc_last(s8[:, 0:4], 64), ALU.mult), [o], [o, s8])
        O(p, "dve", lambda e: e.tensor_tensor(o[:], o[:], gn_t[:], ALU.mult), [o], [o, gn_t])
        O(p, "dve", lambda e: e.tensor_tensor(F[:, 0:256], o[:], At[:, 0:256], ALU.mult), [F], [o, At])
        O(p, "dve", lambda e: e.tensor_tensor(o[:], Yt[:, 512:768], Yt[:, 768:1024], ALU.add), [o], [Yt])
        O(p, "dve", lambda e: e.tensor_reduce(s8[:, 4:8], v3(o[:]), AX.X, ALU.add), [s8], [o])
        O(p, "dve", lambda e: e.tensor_scalar(s8[:, 4:8], s8[:, 4:8], -1.0 / 64, None, ALU.mult), [s8], [s8])
        O(p, "dve", lambda e: e.tensor_tensor(v3(o[:]), v3(o[:]), bc_last(s8[:, 4:8], 64), ALU.add), [o], [o, s8])
        O(p, "act", lambda e: e.activation(t1[:], o[:], AF.Square), [t1], [o])
        O(p, "dve", lambda e: e.tensor_reduce(s8[:, 8:12], v3(t1[:]), AX.X, ALU.add), [s8], [t1])
        O(p, "dve", lambda e: e.tensor_scalar(s8[:, 8:12], s8[:, 8:12], 1.0 / 64, 64e-5, ALU.mult, ALU.add), [s8], [s8])
        O(p, "act", lambda e: e.sqrt(s8[:, 8:12], s8[:, 8:12]), [s8], [s8])
        O(p, "dve", lambda e: e.reciprocal(s8[:, 8:12], s8[:, 8:12]), [s8], [s8])
        O(p, "dve", lambda e: e.tensor_tensor(v3(o[:]), v3(o[:]), bc_last(s8[:, 8:12], 64), ALU.mult), [o], [o, s8])
        O(p, "dve", lambda e: e.tensor_tensor(o[:], o[:], lw_t[:], ALU.mult), [o], [o, lw_t])
        O(p, "dve", lambda e: e.tensor_tensor(o[:], o[:], lb_t[:], ALU.add), [o], [o, lb_t])
        O(p, "dve", lambda e: e.tensor_tensor(o[:], o[:], At[:, 256:512], ALU.add), [o], [o, At])
        O(p, "dve", lambda e: e.tensor_tensor(F[:, 256:512], o[:], At[:, 512:768], ALU.mult), [F], [o, At])
        p.dma("sp", out[ti * 128:(ti + 1) * 128, :], F[:], reads=[F.b])
    p.finish("sp")
    p.close()
    return nc


def l0_core_inputs(b, g, inp, x_seq, n_ctx, n_lat):
    hs = slice(g * 256, (g + 1) * 256)
    w_in = inp["l0_w_in"]
    o_qkv, o_z, o_ab, o_r = 0, 1536, 2048, 2080
    cols = np.concatenate([
        o_qkv + np.arange(g * 256, (g + 1) * 256), o_qkv + 512 + np.arange(g * 256, (g + 1) * 256),
        o_qkv + 1024 + np.arange(g * 256, (g + 1) * 256), o_z + np.arange(g * 256, (g + 1) * 256),
        o_r + np.arange(g * 256, (g + 1) * 256), o_r + 512 + np.arange(g * 256, (g + 1) * 256),
        o_r + 1024 + np.arange(g * 256, (g + 1) * 256), o_r + 1536 + np.arange(256),
        np.concatenate([o_ab + j * 8 + np.arange(g * 4, (g + 1) * 4) for j in range(4)])])
    qkv_cols = cols[0:768] - o_qkv
    r_cols = cols[1024:2048] - o_r
    return dict(
        xs=x_seq, cond2=np.stack([inp["c"][b], inp["c_ctx"]]), mod_w=inp["l0_mod_w"], mod_b=inp["l0_mod_b"], norm1=inp["l0_norm1"],
        Wg=np.ascontiguousarray(w_in[:, cols]),
        convw=np.ascontiguousarray(inp["l0_gdn_conv"][:, qkv_cols]).reshape(-1),
        mu=np.ascontiguousarray(inp["l0_rwkv_mu"][:, r_cols]).reshape(-1),
        alog=np.ascontiguousarray(inp["l0_gdn_A_log"][:, g * 4:(g + 1) * 4]).reshape(-1),
        dtb=np.ascontiguousarray(inp["l0_gdn_dt_bias"][:, g * 4:(g + 1) * 4]).reshape(-1),
        w0=np.ascontiguousarray(inp["l0_rwkv_w0"][:, hs]).reshape(-1), a0=np.ascontiguousarray(inp["l0_rwkv_a0"][:, hs]).reshape(-1),
        k_k=np.ascontiguousarray(inp["l0_rwkv_k_k"][hs]), k_a=np.ascontiguousarray(inp["l0_rwkv_k_a"][hs]),
        r_k=np.ascontiguousarray(inp["l0_rwkv_r_k"][g * 4:(g + 1) * 4]).reshape(-1),
        w2=np.ascontiguousarray(inp["l0_rwkv_w2"][:, :, hs].transpose(1, 0, 2)).reshape(64, 512),
        a2=np.ascontiguousarray(inp["l0_rwkv_a2"][:, hs]), g2=np.ascontiguousarray(inp["l0_rwkv_g2"][:, hs]),
        ident_in=np.eye(128, dtype=np.float32))


def _rev(a, nc_rows):
    return np.concatenate([a[:nc_rows][::-1], a[nc_rows:][::-1]], axis=0)


def l0_scan_inputs(o1, nc_rows):
    S = o1.shape[0]
    col = lambda j: o1[:, j * 256:(j + 1) * 256]
    res = {}
    spec = {"sc_nk": ((0, 0), (9, 9)), "sc_w": ((3, 6), (12, 15)), "sc_bb": ((4, 7), (13, 16)), "sc_kt": ((5, 8), (14, 17)), "sc_r": ((1, 1), (10, 10))}
    for name, ms in spec.items():
        arr = np.empty((2, S, 512), np.float32)
        for m, (cf, cb) in enumerate(ms):
            arr[m, :, 0:256] = col(cf)
            arr[m, :, 256:512] = _rev(col(cb), nc_rows)
        res[name] = arr
    vt = np.empty((128, S, 8), np.float32)
    for m, cv in enumerate((2, 11)):
        v = col(cv).reshape(S, 4, 64)
        vt[m * 64:(m + 1) * 64, :, 0:4] = v.transpose(2, 0, 1)
        vt[m * 64:(m + 1) * 64, :, 4:8] = _rev(v, nc_rows).transpose(2, 0, 1)
    res["vt"] = vt
    return res


def l0_post_inputs(yo, o1, nc_rows, inp, g):
    S = yo.shape[1]
    hs = slice(g * 256, (g + 1) * 256)
    yin = np.empty((S, 1024), np.float32)
    for m in range(2):
        y = yo[m * 64:(m + 1) * 64]
        f = y[:, :, 0:4].transpose(1, 2, 0).reshape(S, 256)
        bk = _rev(y[:, :, 4:8].transpose(1, 2, 0).reshape(S, 256), nc_rows)
        yin[:, m * 512:m * 512 + 256] = f
        yin[:, m * 512 + 256:m * 512 + 512] = bk
    return dict(yin=yin, aux=np.ascontiguousarray(o1[:, 18 * 256:21 * 256]),
                gnw=np.tile(inp["l0_gdn_norm"], 4), lnw=np.ascontiguousarray(inp["l0_rwkv_ln_w"][hs]),
                lnb=np.ascontiguousarray(inp["l0_rwkv_ln_b"][hs]))


def build_C(n_ctx, n_lat):
    NT = n_ctx + n_lat
    nc = bass.Bass("TRN2", target_bir_lowering=False)
    def ein(name, shape):
        return nc.dram_tensor(name, list(shape), F32, kind="ExternalInput").ap()
    xs = ein("xs", [NT * 128, D])
    cond2 = ein("cond2", [2, D])
    mod_w = ein("mod_w", [D, 6 * D]); mod_b = ein("mod_b", [6 * D]); norm1 = ein("norm1", [D])
    Wa = ein("Wa", [D, 384])
    sink = ein("sink", [4])
    cos_d = ein("cos_t", [n_lat * 128, 64]); sin_d = ein("sin_t", [n_lat * 128, 64])
    mask_d = ein("amask", [3, 128, 640])
    ident_d = ein("ident_in", [128, 128])
    out = nc.dram_tensor("yattn", [n_lat * 128, 256], F32, kind="ExternalOutput").ap()
    p = Prog(nc)
    ident = mk(p, "ident", [128, 128])
    p.dma("sp", ident[:], ident_d[:, :], writes=[ident.b])
    psA = [mkps(p, "psA%d" % i, [128, 512]) for i in range(4)]
    psM = mkps(p, "psM", [128, 1024])
    psO = mkps(p, "psO", [128, 512])
    Qall = mk(p, "Qall", [128, n_lat, 256]); KT = mk(p, "KT", [64, NT * 128]); Vall = mk(p, "Vall", [128, NT, 64])
    MOD = mk(p, "MOD", [128, 2, 2, 1024])
    p.push_scope()
    adaln_chunks(p, cond2, mod_w, mod_b, [0, 1], MOD, psA, "ad")
    n1b = mk(p, "n1b", [128, 1024])
    p.dma("sp", n1b[:], norm1.partition_broadcast(128), writes=[n1b.b])
    for s in range(2):
        O(p, "dve", lambda e, s=s: e.scalar_tensor_tensor(MOD[:, s, 1, :], in0=MOD[:, s, 1, :], scalar=1.0, in1=n1b[:],
                                                          op0=ALU.add, op1=ALU.mult), [MOD], [MOD, n1b])
    Ws = mk(p, "Ws", [128, 8, 384])
    p.dma("sp", Ws[:], Wa.rearrange("(kc p) n -> p kc n", p=128), writes=[Ws.b])
    X = mk(p, "X", [128, 1024]); H = mk(p, "H", [128, 1024]); HT = mk(p, "HT", [128, 1024]); sm = mk(p, "sm", [128, 64])
    Pq = mk(p, "Pq", [128, 384]); Pr = mk(p, "Pr", [128, 320]); CS = mk(p, "CS", [128, 128]); tq = mk(p, "tq", [128, 320])
    for ti in range(NT):
        s = 1 if ti < n_ctx else 0
        p.dma("sp", X[:], xs[ti * 128:(ti + 1) * 128, :], writes=[X.b])
        rms_mod(p, X, H, sm, (MOD, 1), (MOD, 0), s)
        transpose_blocks(p, H, HT, psM, ident, 8)
        for kc in range(8):
            O(p, "pe", lambda e, kc=kc: e.matmul(psA[0][:, 0:384], lhsT=HT[:, kc * 128:(kc + 1) * 128], rhs=Ws[:, kc, :],
                                                 start=(kc == 0), stop=(kc == 7)), [psA[0]], [HT, Ws])
        O(p, "act", lambda e: e.copy(Pq[:], psA[0][:, 0:384]), [Pq], [psA[0]])
        O(p, "act", lambda e: e.copy(Vall[:, ti, :], Pq[:, 320:384]), [Vall], [Pq])
        if s == 1:
            ksrc = Pq[:, 256:320]; kb = Pq
        else:
            li = ti - n_ctx
            p.dma("pool", CS[:, 0:64], cos_d[li * 128:(li + 1) * 128, :], writes=[CS.b])
            p.dma("pool", CS[:, 64:128], sin_d[li * 128:(li + 1) * 128, :], writes=[CS.b])
            x5 = Pq[:, 0:320].rearrange("p (h f c) -> p h f c", h=5, f=2)
            t5 = tq[:].rearrange("p (h f c) -> p h f c", h=5, f=2)
            O(p, "act", lambda e: e.copy(t5[:, :, :, 0:16], x5[:, :, :, 16:32]), [tq], [Pq])
            O(p, "act", lambda e: e.copy(t5[:, :, :, 16:32], x5[:, :, :, 0:16]), [tq], [Pq])
            v5 = lambda ap: ap.rearrange("p (h d) -> p h d", h=5)
            cosb = CS[:, 0:64].unsqueeze(1).to_broadcast([128, 5, 64]); sinb = CS[:, 64:128].unsqueeze(1).to_broadcast([128, 5, 64])
            O(p, "dve", lambda e: e.tensor_tensor(v5(Pr[:]), v5(Pq[:, 0:320]), cosb, ALU.mult), [Pr], [Pq, CS])
            O(p, "dve", lambda e: e.tensor_tensor(v5(tq[:]), v5(tq[:]), sinb, ALU.mult), [tq], [tq, CS])
            O(p, "dve", lambda e: e.tensor_tensor(Pr[:], Pr[:], tq[:], ALU.add), [Pr], [Pr, tq])
            O(p, "act", lambda e, li=li: e.copy(Qall[:, li, :], Pr[:, 0:256]), [Qall], [Pr])
            ksrc = Pr[:, 256:320]; kb = Pr
        O(p, "pe", lambda e: e.transpose(psO[0:64, 0:128], ksrc, ident[:]), [psO], [kb, ident])
        O(p, "act", lambda e, ti=ti: e.copy(KT[:, ti * 128:(ti + 1) * 128], psO[0:64, 0:128]), [KT], [psO])
    p.pop_scope()
    masks = mk(p, "masks", [128, 3, 640])
    p.dma("sp", masks[:], mask_d.rearrange("m p k -> p m k"), writes=[masks.b])
    snk = mk(p, "snk", [128, 4])
    p.dma("sp", snk[:], sink.partition_broadcast(128), writes=[snk.b])
    qT = mk(p, "qT", [64, 512]); Sc = [mk(p, "Sc%d" % i, [128, 640]) for i in range(2)]; PT = [mk(p, "PT%d" % i, [128, 640]) for i in range(2)]
    s4 = mk(p, "s4", [128, 16]); Ot = [mk(p, "Ot%d" % i, [128, 256]) for i in range(2)]
    nck = n_ctx
    for li in range(n_lat):
        first, last = (li == 0), (li == n_lat - 1)
        lo = li - 1 if not first else li
        hi = li + 1 if not last else li
        chunks = list(range(nck)) + [nck + t for t in range(lo, hi + 1)]
        nk = len(chunks) * 128
        mi = 1 if first else (2 if last else 0)
        kranges = [(0, nck * 128), ((nck + lo) * 128, (nck + hi + 1) * 128)]
        for h in range(4):
            O(p, "pe", lambda e, h=h: e.transpose(psO[0:64, h * 128:(h + 1) * 128], Qall[:, li, h * 64:(h + 1) * 64], ident[:]), [psO], [Qall, ident])
        O(p, "act", lambda e: e.copy(qT[:], psO[0:64, :]), [qT], [psO])
        O_ = Ot[li % 2]
        for h in range(4):
            ps = psA[2 * (h % 2)]; ps2 = psA[2 * (h % 2) + 1]
            S_ = Sc[h % 2]; P_ = PT[h % 2]
            c0 = 0
            for (a, b_) in kranges:
                w = b_ - a
                tgt = ps if c0 + w <= 512 else None
                if c0 == 0:
                    O(p, "pe", lambda e, a=a, w=w: e.matmul(ps[:, 0:w], lhsT=qT[:, h * 128:(h + 1) * 128], rhs=KT[:, a:a + w], start=True, stop=True),
                      [ps], [qT, KT])
                    O(p, "dve", lambda e, w=w: e.scalar_tensor_tensor(S_[:, 0:w], in0=ps[:, 0:w], scalar=0.125, in1=masks[:, mi, 0:w],
                                                                      op0=ALU.mult, op1=ALU.add), [S_], [ps, masks])
                else:
                    O(p, "pe", lambda e, a=a, w=w: e.matmul(ps2[:, 0:w], lhsT=qT[:, h * 128:(h + 1) * 128], rhs=KT[:, a:a + w], start=True, stop=True),
                      [ps2], [qT, KT])
                    O(p, "dve", lambda e, w=w, c0=c0: e.scalar_tensor_tensor(S_[:, c0:c0 + w], in0=ps2[:, 0:w], scalar=0.125,
                                                                             in1=masks[:, mi, c0:c0 + w], op0=ALU.mult, op1=ALU.add), [S_], [ps2, masks])
                c0 += w
            O(p, "dve", lambda e: e.tensor_reduce(s4[:, 0:1], S_[:, 0:nk], AX.X, ALU.max), [s4], [S_])
            O(p, "dve", lambda e, h=h: e.tensor_tensor(s4[:, 0:1], s4[:, 0:1], snk[:, h:h + 1], ALU.max), [s4], [s4, snk])
            O(p, "dve", lambda e: e.tensor_scalar(s4[:, 1:2], s4[:, 0:1], -1.0, None, ALU.mult), [s4], [s4])
            O(p, "act", lambda e: e.activation(S_[:, 0:nk], S_[:, 0:nk], AF.Exp, bias=s4[:, 1:2], accum_out=s4[:, 2:3]), [S_, s4], [S_, s4])
            O(p, "act", lambda e, h=h: e.activation(s4[:, 3:4], snk[:, h:h + 1], AF.Exp, bias=s4[:, 1:2]), [s4], [snk, s4])
            O(p, "dve", lambda e: e.tensor_tensor(s4[:, 4:5], s4[:, 2:3], s4[:, 3:4], ALU.add), [s4], [s4])
            O(p, "dve", lambda e: e.reciprocal(s4[:, 5:6], s4[:, 4:5]), [s4], [s4])
            ncks = len(chunks)
            for c in range(ncks):
                O(p, "pe", lambda e, c=c: e.transpose(psM[:, c * 128:(c + 1) * 128], S_[:, c * 128:(c + 1) * 128], ident[:]), [psM], [S_, ident])
            O(p, "act", lambda e: e.copy(P_[:, 0:nk], psM[:, 0:nk]), [P_], [psM])
            for c, tix in enumerate(chunks):
                O(p, "pe", lambda e, c=c, tix=tix: e.matmul(psO[:, 0:64], lhsT=P_[:, c * 128:(c + 1) * 128], rhs=Vall[:, tix, :],
                                                            start=(c == 0), stop=(c == ncks - 1)), [psO], [P_, Vall])
            O(p, "dve", lambda e, h=h: e.tensor_scalar(O_[:, h * 64:(h + 1) * 64], psO[:, 0:64], s4[:, 5:6], None, ALU.mult), [O_], [psO, s4])
        p.dma("sp", out[li * 128:(li + 1) * 128, :], O_[:], reads=[O_.b])
    p.finish("sp")
    p.close()
    return nc


def rope_tables(L):
    t = np.arange(L)
    row = (t // 64).astype(np.float32); col = (t % 64).astype(np.float32)
    inv = (10000.0 ** (-np.arange(16, dtype=np.float32) / 16)).astype(np.float32)
    ar = row[:, None] * inv[None]; ac = col[:, None] * inv[None]
    cos = np.concatenate([np.cos(ar), np.cos(ar), np.cos(ac), np.cos(ac)], axis=1).astype(np.float32)
    sin = np.concatenate([-np.sin(ar), np.sin(ar), -np.sin(ac), np.sin(ac)], axis=1).astype(np.float32)
    return cos, sin


def attn_masks(n_ctx_rows=256):
    q = np.arange(128)[:, None]; j = np.arange(384)[None]
    loc = np.where((j >= q) & (j <= q + 256), 0.0, -1e30).astype(np.float32)
    m = np.zeros((3, 128, 640), np.float32)
    m[0, :, n_ctx_rows:n_ctx_rows + 384] = loc
    m[1, :, n_ctx_rows:n_ctx_rows + 256] = loc[:, 128:384]
    m[2, :, n_ctx_rows:n_ctx_rows + 256] = loc[:, 0:256]
    return m


TWO_PI = 6.283185307179586
MAGIC = 12582912.0


def build_H1(nJ):
    L = nJ * 128
    nc = bass.Bass("TRN2", target_bir_lowering=False)
    def ein(name, shape):
        return nc.dram_tensor(name, list(shape), F32, kind="ExternalInput").ap()
    xs = ein("xs", [4 * L, D])
    condA = ein("condA", [2, D]); condB = ein("condB", [2, D])
    mod_w = ein("mod_w", [D, 6 * D]); mod_b = ein("mod_b", [6 * D]); norm1 = ein("norm1", [D])
    Wh = ein("Wh", [D, 192]); hcw = ein("hcw", [3 * 192]); hcb = ein("hcb", [192])
    zT = ein("zT", [33, L]); w1 = ein("w1", [33, 64]); b1 = ein("b1", [64]); w2 = ein("w2", [64, 64]); b2 = ein("b2", [64])
    fr = ein("fr", [64]); w3c = ein("w3c", [64, 256]); win = ein("win", [L, 64])
    ident_d = ein("ident_in", [128, 128])
    u_out = nc.dram_tensor("u", [4 * L, 192], F32, kind="ExternalOutput").ap()
    hn_out = nc.dram_tensor("hn", [L, 256], F32, kind="ExternalOutput").ap()
    p = Prog(nc)
    Pd = p.dram("Pd", [4 * (L + 2), 192]); b_Pd = Buf()
    ident = mk(p, "ident", [128, 128])
    p.dma("sp", ident[:], ident_d[:, :], writes=[ident.b])
    psA = [mkps(p, "psA%d" % i, [128, 512]) for i in range(4)]
    psM = mkps(p, "psM", [128, 1024])
    psL = mkps(p, "psL", [128, 512])
    MODS = [mk(p, "MODa", [128, 2, 2, 1024]), mk(p, "MODb", [128, 2, 2, 1024])]
    p.push_scope()
    n1b = mk(p, "n1b", [128, 1024])
    p.dma("sp", n1b[:], norm1.partition_broadcast(128), writes=[n1b.b])
    for i, cd in enumerate((condA, condB)):
        p.push_scope2 = None
        adaln_chunks(p, cd, mod_w, mod_b, [0, 1], MODS[i], psA, "ad%d" % i)
        for s in range(2):
            O(p, "dve", lambda e, s=s, i=i: e.scalar_tensor_tensor(MODS[i][:, s, 1, :], in0=MODS[i][:, s, 1, :], scalar=1.0, in1=n1b[:],
                                                              op0=ALU.add, op1=ALU.mult), [MODS[i]], [MODS[i], n1b])
    p.pop_scope()
    p.push_scope()
    Ws = mk(p, "Ws", [128, 8, 192])
    p.dma("sp", Ws[:], Wh.rearrange("(kc p) n -> p kc n", p=128), writes=[Ws.b])
    X = mk(p, "X", [128, 1024]); H = mk(p, "H", [128, 1024]); HT = mk(p, "HT", [128, 1024]); sm = mk(p, "sm", [128, 64])
    Pt = [mk(p, "Pt%d" % i, [128, 192]) for i in range(2)]
    zrow = mk(p, "zrow", [1, 192])
    O(p, "dve", lambda e: e.memset(zrow[:], 0.0), [zrow], [])
    for b in range(4):
        for r in (b * (L + 2), b * (L + 2) + L + 1):
            p.dma("sp", Pd[r:r + 1, :], zrow[:], reads=[zrow.b], writes=[b_Pd])
    for b in range(4):
        for ti in range(nJ):
            gi = b * nJ + ti
            p.dma("sp", X[:], xs[gi * 128:(gi + 1) * 128, :], writes=[X.b])
            rms_mod(p, X, H, sm, (MODS[b // 2], 1), (MODS[b // 2], 0), b % 2)
            transpose_blocks(p, H, HT, psM, ident, 8)
            PT = Pt[gi % 2]
            for kc in range(8):
                O(p, "pe", lambda e, kc=kc: e.matmul(psA[0][:, 0:192], lhsT=HT[:, kc * 128:(kc + 1) * 128], rhs=Ws[:, kc, :],
                                                     start=(kc == 0), stop=(kc == 7)), [psA[0]], [HT, Ws])
            O(p, "act", lambda e: e.copy(PT[:], psA[0][:, 0:192]), [PT], [psA[0]])
            r0 = b * (L + 2) + 1 + ti * 128
            p.dma("sp", Pd[r0:r0 + 128, :], PT[:], reads=[PT.b], writes=[b_Pd])
    p.pop_scope()
    def bload(name, src, n):
        t = mk(p, name, [128, n])
        p.dma("sp", t[:], src.partition_broadcast(128), writes=[t.b])
        return t
    cw_t = bload("cw_t", hcw, 576); cb_t = bload("cb_t", hcb, 192)
    Pc = [mk(p, "Pc%d" % i, [128, 3, 192]) for i in range(2)]; U = [mk(p, "U%d" % i, [128, 192]) for i in range(2)]; t1 = mk(p, "t1", [128, 192])
    for b in range(4):
        for ti in range(nJ):
            gi = b * nJ + ti
            r0 = b * (L + 2) + 1 + ti * 128
            P3 = Pc[gi % 2]; Ut = U[gi % 2]
            for k in range(3):
                p.dma("sp" if k == 1 else "pool", P3[:, k, :], Pd[r0 - 1 + k:r0 + 127 + k, :], reads=[b_Pd], writes=[P3.b])
            O(p, "dve", lambda e: e.tensor_tensor(Ut[:], P3[:, 0, :], cw_t[:, 0:192], ALU.mult), [Ut], [P3, cw_t])
            O(p, "dve", lambda e: e.tensor_tensor(t1[:], P3[:, 1, :], cw_t[:, 192:384], ALU.mult), [t1], [P3, cw_t])
            O(p, "dve", lambda e: e.tensor_tensor(Ut[:], Ut[:], t1[:], ALU.add), [Ut], [Ut, t1])
            O(p, "dve", lambda e: e.tensor_tensor(t1[:], P3[:, 2, :], cw_t[:, 384:576], ALU.mult), [t1], [P3, cw_t])
            O(p, "dve", lambda e: e.tensor_tensor(Ut[:], Ut[:], t1[:], ALU.add), [Ut], [Ut, t1])
            O(p, "dve", lambda e: e.tensor_tensor(Ut[:], Ut[:], cb_t[:], ALU.add), [Ut], [Ut, cb_t])
            p.dma("sp", u_out[gi * 128:(gi + 1) * 128, :], Ut[:], reads=[Ut.b])
    zTs = mk(p, "zTs", [33, L]); p.dma("sp", zTs[:], zT[:, :], writes=[zTs.b])
    w1s = mk(p, "w1s", [33, 64]); p.dma("sp", w1s[:], w1[:, :], writes=[w1s.b])
    w2s = mk(p, "w2s", [64, 64]); p.dma("sp", w2s[:], w2[:, :], writes=[w2s.b])
    w3s = mk(p, "w3s", [64, 256]); p.dma("sp", w3s[:], w3c[:, :], writes=[w3s.b])
    b1_t = bload("b1_t", b1, 64); b2_t = bload("b2_t", b2, 64); fr_t = bload("fr_t", fr, 64)
    ones = mk(p, "ones", [128, 128]); O(p, "dve", lambda e: e.memset(ones[:], 1.0), [ones], [])
    hraw = mk(p, "hraw", [128, nJ, 256]); habs = mk(p, "habs", [128, 256])
    a = mk(p, "a", [128, 64]); k_ = mk(p, "k_", [128, 64]); hT = mk(p, "hT", [64, 128]); wn = [mk(p, "wn%d" % i, [128, 64]) for i in range(2)]

    def sin_layer(ps, bt):
        O(p, "dve", lambda e: e.tensor_tensor(a[:], ps[:, 0:64], bt[:], ALU.add), [a], [ps, bt])
        O(p, "dve", lambda e: e.tensor_tensor(a[:], a[:], fr_t[:], ALU.mult), [a], [a, fr_t])
        O(p, "dve", lambda e: e.tensor_scalar(k_[:], a[:], 1.0 / TWO_PI, MAGIC, ALU.mult, ALU.add), [k_], [a])
        O(p, "dve", lambda e: e.tensor_scalar(k_[:], k_[:], -MAGIC, None, ALU.add), [k_], [k_])
        O(p, "dve", lambda e: e.scalar_tensor_tensor(a[:], in0=k_[:], scalar=-TWO_PI, in1=a[:], op0=ALU.mult, op1=ALU.add), [a], [k_, a])
        O(p, "act", lambda e: e.activation(a[:], a[:], AF.Sin), [a], [a])
        O(p, "pe", lambda e: e.transpose(psM[0:64, 0:128], a[:], ident[:]), [psM], [a, ident])
        O(p, "act", lambda e: e.copy(hT[:], psM[0:64, 0:128]), [hT], [psM])

    for ti in range(nJ):
        W_ = wn[ti % 2]
        p.dma("pool", W_[:], win[ti * 128:(ti + 1) * 128, :], writes=[W_.b])
        O(p, "pe", lambda e: e.matmul(psA[1][:, 0:64], lhsT=zTs[:, ti * 128:(ti + 1) * 128], rhs=w1s[:], start=True, stop=True), [psA[1]], [zTs, w1s])
        sin_layer(psA[1], b1_t)
        O(p, "pe", lambda e: e.matmul(psA[2][:, 0:64], lhsT=hT[:], rhs=w2s[:], start=True, stop=True), [psA[2]], [hT, w2s])
        sin_layer(psA[2], b2_t)
        O(p, "pe", lambda e: e.matmul(psA[3][:, 0:256], lhsT=hT[:], rhs=w3s[:], start=True, stop=True), [psA[3]], [hT, w3s])
        O(p, "dve", lambda e: e.tensor_tensor(hraw[:, ti, :].rearrange("p (g c) -> p g c", c=64), psA[3][:, 0:256].rearrange("p (g c) -> p g c", c=64),
                                              W_[:].unsqueeze(1).to_broadcast([128, 4, 64]), ALU.mult), [hraw], [psA[3], W_])
        O(p, "act", lambda e: e.activation(habs[:], hraw[:, ti, :], AF.Abs), [habs], [hraw])
        O(p, "pe", lambda e: e.matmul(psL[:, 0:256], lhsT=ones[:], rhs=habs[:], start=(ti == 0), stop=(ti == nJ - 1)), [psL], [ones, habs])
    l1 = mk(p, "l1", [128, 2, 64])
    pl = psL[:, 0:256].rearrange("p (n f c) -> p n f c", n=2, f=2)
    O(p, "dve", lambda e: e.tensor_copy(l1[:], pl[:, :, 0, :]), [l1], [psL])
    O(p, "dve", lambda e: e.tensor_tensor(l1[:], l1[:], pl[:, :, 1, :], ALU.add), [l1], [l1, psL])
    O(p, "dve", lambda e: e.reciprocal(l1[:], l1[:]), [l1], [l1])
    ho = [mk(p, "ho%d" % i, [128, 256]) for i in range(2)]
    for ti in range(nJ):
        Ho = ho[ti % 2]
        O(p, "dve", lambda e: e.tensor_tensor(Ho[:].rearrange("p (n f c) -> p n f c", n=2, f=2), hraw[:, ti, :].rearrange("p (n f c) -> p n f c", n=2, f=2),
                                              l1[:].unsqueeze(2).to_broadcast([128, 2, 2, 64]), ALU.mult), [Ho], [hraw, l1])
        p.dma("sp", hn_out[ti * 128:(ti + 1) * 128, :], Ho[:], reads=[Ho.b])
    p.finish("sp")
    p.close()
    return nc


def build_H2(nJ):
    L = nJ * 128
    NB = nJ * 4
    nc = bass.Bass("TRN2", target_bir_lowering=False)
    def ein(name, shape):
        return nc.dram_tensor(name, list(shape), F32, kind="ExternalInput").ap()
    zrev = ein("zrev", [128, 64, NB]); znat = ein("znat", [128, 64, NB]); xg = ein("xg", [128, 64, NB])
    ka = ein("ka", [64, 2 * L]); bias = ein("bias", [64]); hb0 = ein("hb0", [64])
    zo = nc.dram_tensor("zo", [128, 64, NB], F32, kind="ExternalOutput").ap()
    p = Prog(nc)
    be = mk(p, "be", [128, 64]); hb = mk(p, "hb", [128, 64])
    p.dma("sp", be[:], bias.partition_broadcast(128), writes=[be.b])
    p.dma("sp", hb[:], hb0.partition_broadcast(128), writes=[hb.b])
    O(p, "dve", lambda e: e.tensor_tensor(be[:], be[:], hb[:], ALU.add), [be], [be, hb])
    CH = 8
    ZR = [mk(p, "ZR%d" % i, [128, CH, NB]) for i in range(2)]; ZN = [mk(p, "ZN%d" % i, [128, CH, NB]) for i in range(2)]
    XG = [mk(p, "XG%d" % i, [128, CH, NB]) for i in range(2)]; ZO = [mk(p, "ZO%d" % i, [128, CH, NB]) for i in range(2)]
    GD = 16
    ds_all = [0] + [d for d in range(-(nJ - 1), nJ) if d != 0]
    dlist = list(range(-(nJ - 1), nJ))
    groups = [dlist[i:i + GD] for i in range(0, len(dlist), GD)]
    groups.sort(key=lambda g: 0 if 0 in g else 1)
    Kt = [mk(p, "Kt%d" % i, [128, GD, 128]) for i in range(3)]
    psY = [mkps(p, "psY%d" % i, [128, 512]) for i in range(2)]
    kidx = 0
    for c0 in range(0, 64, CH):
        ci = (c0 // CH) % 2
        p.dma("pool", ZR[ci][:], zrev[:, c0:c0 + CH, :], writes=[ZR[ci].b])
        p.dma("pool", ZN[ci][:], znat[:, c0:c0 + CH, :], writes=[ZN[ci].b])
        p.dma("pool", XG[ci][:], xg[:, c0:c0 + CH, :], writes=[XG[ci].b])
        for cc in range(CH):
            c = c0 + cc
            Y = psY[c % 2]
            first = True
            nmm = 2 * nJ - 1
            done = 0
            for g in groups:
                K_ = Kt[kidx % 3]; kidx += 1
                src = bass.AP(ka.tensor, c * 2 * L + L + g[0] * 128 - 127, [[1, 128], [128, len(g)], [1, 128]])
                p.dma("sp", K_[:, 0:len(g), :], src, writes=[K_.b])
                order = ([0] + [d for d in g if d != 0]) if 0 in g else g
                for d in order:
                    J0, J1 = max(0, -d), min(nJ, nJ - d)
                    done += 1
                    O(p, "pe", lambda e, d=d, J0=J0, J1=J1, gi=d - g[0], first=first, last=(done == nmm):
                      e.matmul(Y[:, (J0 + d) * 4:(J1 + d) * 4], lhsT=K_[:, gi, :], rhs=ZR[ci][:, cc, J0 * 4:J1 * 4], start=first, stop=last),
                      [Y], [K_, ZR[ci]])
                    first = False
            O(p, "dve", lambda e, c=c, cc=cc: e.scalar_tensor_tensor(ZO[ci][:, cc, :], in0=ZN[ci][:, cc, :], scalar=be[:, c:c + 1], in1=Y[:, 0:NB],
                                                                    op0=ALU.mult, op1=ALU.add), [ZO[ci]], [ZN[ci], be, Y])
            O(p, "dve", lambda e, cc=cc: e.tensor_tensor(ZO[ci][:, cc, :], ZO[ci][:, cc, :], XG[ci][:, cc, :], ALU.mult), [ZO[ci]], [ZO[ci], XG[ci]])
        p.dma("sp", zo[:, c0:c0 + CH, :], ZO[ci][:], reads=[ZO[ci].b])
    p.finish("sp")
    p.close()
    return nc


def hyena_consts(L, cg):
    t = np.arange(L, dtype=np.float32)
    t_norm = np.linspace(0.0, 1.0, L, dtype=np.float32)[:, None]
    bands = np.linspace(1e-4, 15, 16, dtype=np.float32)
    ang = (np.float32(2.0 * np.pi / L) * t[:, None] * bands[None, :]).astype(np.float32)
    z = np.concatenate([t_norm, np.cos(ang), np.sin(ang)], axis=-1).astype(np.float32)
    deltas = np.abs(np.linspace(np.log(1e-2) / 1.5, np.log(1e-2) / 0.3, 512, dtype=np.float32))
    window = np.exp(-t_norm * deltas[None, :]).astype(np.float32)
    return np.ascontiguousarray(z.T), np.ascontiguousarray(window[:, cg * 64:(cg + 1) * 64])


def hy_layout(a, nJ, rev):
    x = a.reshape(4, nJ, 128, 64)
    if rev:
        x = x[:, :, ::-1, :]
    return np.ascontiguousarray(x.transpose(2, 3, 1, 0).reshape(128, 64, nJ * 4))


def hy_unlayout(zo, nJ):
    return np.ascontiguousarray(zo.reshape(128, 64, nJ, 4).transpose(3, 2, 0, 1).reshape(4, nJ * 128, 64))


def hy_kernel_array(hn, n, L):
    h = hn.reshape(L, 2, 2, 64)
    ka = np.zeros((64, 2 * L), np.float32)
    ka[:, L:2 * L] = h[:, n, 0, :].T
    ka[:, 1:L] = h[1:, n, 1, :][::-1].T
    return ka, np.ascontiguousarray(h[0, n, 1, :])


def _run(nc, in_maps):
    res = run_bass_kernel_spmd(nc, in_maps, core_ids=list(range(8)))
    return res.results


def _stageB(pf, inp, xs_list, fs_list, cond_list, n_lat, n_ctx, final):
    nc = build_B(n_lat, n_ctx, final)
    common = dict(mod_w=inp[pf + "mod_w"], mod_b=inp[pf + "mod_b"], norm2=inp[pf + "norm2"], w_out=inp[pf + "w_out"], wq=inp[pf + "peer_wq"],
                  k1T=np.ascontiguousarray(inp[pf + "peer_k1"].transpose(0, 2, 1)), k2T=np.ascontiguousarray(inp[pf + "peer_k2"].transpose(0, 2, 1)),
                  UT=np.ascontiguousarray(inp[pf + "peer_u"].T), V=inp[pf + "peer_v"], fnorm=inp["final_norm"],
                  ident_in=np.eye(128, dtype=np.float32))
    maps = [dict(common, xs=xs_list[i], fs=fs_list[i], cond2=cond_list[i]) for i in range(8)]
    return [r["xo"] for r in _run(nc, maps)]


def kernel(**inp):
    inp = {k: np.ascontiguousarray(np.asarray(v, dtype=np.float32)) for k, v in inp.items()}
    x, c, ctx, c_ctx = inp["x"], inp["c"], inp["ctx"], inp["c_ctx"]
    B, L, _ = x.shape
    C = ctx.shape[1]
    n_ctx, n_lat = C // 128, L // 128
    NT = n_ctx + n_lat
    xseq = [np.concatenate([ctx[b], x[b]], axis=0) for b in range(B)]
    nc = build_A1(n_ctx, n_lat)
    r1 = _run(nc, [l0_core_inputs(i // 2, i % 2, inp, xseq[i // 2], n_ctx, n_lat) for i in range(8)])
    o1 = [r["o1"] for r in r1]
    del r1
    nc = build_A2(NT * 128)
    r2 = _run(nc, [l0_scan_inputs(o1[i], C) for i in range(8)])
    yo = [r["yo"] for r in r2]
    del r2
    nc = build_A3(NT)
    r3 = _run(nc, [l0_post_inputs(yo[i], o1[i], C, inp, i % 2) for i in range(8)])
    feat = [r["feat"] for r in r3]
    del r3, o1, yo
    fs0 = [np.concatenate([feat[2 * b][:, 0:256], feat[2 * b + 1][:, 0:256], feat[2 * b][:, 256:512], feat[2 * b + 1][:, 256:512]], axis=1)
           for b in range(B)]
    hl, hc = L // 2, C // 2
    xs_l, fs_l, cd_l = [], [], []
    for i in range(8):
        b, h = i // 2, i % 2
        xs_l.append(np.concatenate([x[b, h * hl:(h + 1) * hl], ctx[b, h * hc:(h + 1) * hc]], axis=0))
        fs_l.append(np.concatenate([fs0[b][C + h * hl:C + (h + 1) * hl], fs0[b][h * hc:(h + 1) * hc]], axis=0))
        cd_l.append(np.stack([c[b], c_ctx]))
    xo = _stageB("l0_", inp, xs_l, fs_l, cd_l, hl // 128, hc // 128, 0)
    x1 = np.stack([np.concatenate([xo[2 * b][:hl], xo[2 * b + 1][:hl]], axis=0) for b in range(B)])
    ctx1 = np.stack([np.concatenate([xo[2 * b][hl:], xo[2 * b + 1][hl:]], axis=0) for b in range(B)])
    del xo, fs0, feat
    w_in = inp["l1_w_in"]
    cos, sin = rope_tables(L)
    am = attn_masks(C)
    maps = []
    for i in range(8):
        b, kvh = i // 2, i % 2
        cols = np.concatenate([np.arange(kvh * 256, (kvh + 1) * 256), 512 + np.arange(kvh * 64, (kvh + 1) * 64), 640 + np.arange(kvh * 64, (kvh + 1) * 64)])
        maps.append(dict(xs=np.concatenate([ctx1[b], x1[b]], axis=0), cond2=np.stack([c[b], c_ctx]), mod_w=inp["l1_mod_w"], mod_b=inp["l1_mod_b"],
                         norm1=inp["l1_norm1"], Wa=np.ascontiguousarray(w_in[:, cols]),
                         sink=np.ascontiguousarray(inp["l1_attn_sink"][kvh * 4:(kvh + 1) * 4]), cos_t=cos, sin_t=sin, amask=am,
                         ident_in=np.eye(128, dtype=np.float32)))
    nc = build_C(n_ctx, n_lat)
    ya = [r["yattn"] for r in _run(nc, maps)]
    nJ = n_lat
    maps = []
    xall = x1.reshape(B * L, D)
    for cg in range(8):
        cols = np.concatenate([768 + j * 512 + np.arange(cg * 64, (cg + 1) * 64) for j in range(3)])
        zT, win = hyena_consts(L, cg)
        w3cols = np.concatenate([n * 1024 + d * 512 + np.arange(cg * 64, (cg + 1) * 64) for n in range(2) for d in range(2)])
        maps.append(dict(xs=xall, condA=np.ascontiguousarray(c[0:2]), condB=np.ascontiguousarray(c[2:4]), mod_w=inp["l1_mod_w"], mod_b=inp["l1_mod_b"],
                         norm1=inp["l1_norm1"], Wh=np.ascontiguousarray(w_in[:, cols]),
                         hcw=np.ascontiguousarray(inp["l1_hy_conv_w"][:, cols - 768]).reshape(-1),
                         hcb=np.ascontiguousarray(inp["l1_hy_conv_b"][cols - 768]), zT=zT, w1=inp["l1_hy_w1"], b1=inp["l1_hy_b1"],
                         w2=inp["l1_hy_w2"], b2=inp["l1_hy_b2"], fr=inp["l1_hy_freq"], w3c=np.ascontiguousarray(inp["l1_hy_w3"][:, w3cols]),
                         win=win, ident_in=np.eye(128, dtype=np.float32)))
    nc = build_H1(nJ)
    rh = _run(nc, maps)
    us = [r["u"].reshape(B, L, 3, 64) for r in rh]
    hns = [r["hn"] for r in rh]
    del rh, maps
    z = [u[:, :, 0, :] for u in us]
    ncH = build_H2(nJ)
    for n in range(2):
        maps = []
        for cg in range(8):
            ka, hb0 = hy_kernel_array(hns[cg], n, L)
            maps.append(dict(zrev=hy_layout(z[cg], nJ, True), znat=hy_layout(z[cg], nJ, False), xg=hy_layout(us[cg][:, :, 1 + n, :], nJ, False),
                             ka=ka, bias=np.ascontiguousarray(inp["l1_hy_bias"][n, cg * 64:(cg + 1) * 64]), hb0=hb0))
        z = [hy_unlayout(r["zo"], nJ) for r in _run(ncH, maps)]
    yh = np.concatenate(z, axis=2)
    xs_l, fs_l, cd_l = [], [], []
    for i in range(8):
        b, h = i // 2, i % 2
        sl = slice(h * hl, (h + 1) * hl)
        xs_l.append(np.ascontiguousarray(x1[b, sl]))
        fs_l.append(np.concatenate([ya[2 * b][sl], ya[2 * b + 1][sl], yh[b, sl]], axis=1))
        cd_l.append(np.stack([c[b], c_ctx]))
    xo = _stageB("l1_", inp, xs_l, fs_l, cd_l, hl // 128, 0, 1)
    out = np.stack([np.concatenate([xo[2 * b], xo[2 * b + 1]], axis=0) for b in range(B)])
    return out.astype(np.float32)
```

```python
import numpy as np
from contextlib import ExitStack
import concourse.bass as bass
import concourse.mybir as mybir
from concourse.bass_utils import run_bass_kernel_spmd

F32 = mybir.dt.float32
BF16 = mybir.dt.bfloat16
ALU = mybir.AluOpType
AF = mybir.ActivationFunctionType
AX = mybir.AxisListType

SAME_ENGINE_SYNC = True
N_DMA_SEM = 8
D = 1024


class Buf:
    __slots__ = ("w", "r", "name")

    def __init__(self, name=""):
        self.w = None
        self.r = {}
        self.name = name


class Prog:
    def __init__(self, nc):
        self.nc = nc
        self.es = ExitStack()
        self.eng = {"pe": nc.tensor, "dve": nc.vector, "act": nc.scalar, "pool": nc.gpsimd, "sp": nc.sync}
        self.sem = {}
        self.cnt = {}
        for e in ("pe", "dve", "act", "pool"):
            self.sem[e] = self.es.enter_context(nc.semaphore("s_" + e))
            self.cnt[e] = 0
        self.dsem = []
        for i in range(N_DMA_SEM):
            k = "d%d" % i
            self.sem[k] = self.es.enter_context(nc.semaphore("s_" + k))
            self.cnt[k] = 0
            self.dsem.append(k)
        self.dnext = 0
        self.waited = {e: {} for e in self.eng}
        self.ninst = 0

    def sb(self, name, shape, dt=F32):
        return self.es.enter_context(self.nc.sbuf_tensor(name, list(shape), dt))

    def ps(self, name, shape, dt=F32):
        return self.es.enter_context(self.nc.psum_tensor(name, list(shape), dt))

    def dram(self, name, shape, dt=F32, kind="Internal"):
        return self.nc.dram_tensor(name, list(shape), dt, kind=kind).ap()

    def _wait(self, e, key, val):
        if key == e and (e == "pe" or not SAME_ENGINE_SYNC or e in getattr(self, "nosync", ())):
            return
        if self.waited[e].get(key, 0) >= val:
            return
        self.eng[e].wait_ge(self.sem[key], val)
        self.ninst += 1
        self.waited[e][key] = val

    def _deps(self, e, reads, writes):
        deps = {}
        for b in reads:
            if b.w is not None:
                k, v = b.w
                deps[k] = max(deps.get(k, 0), v)
        for b in writes:
            if b.w is not None:
                k, v = b.w
                deps[k] = max(deps.get(k, 0), v)
            for k, v in b.r.items():
                deps[k] = max(deps.get(k, 0), v)
        for k, v in deps.items():
            self._wait(e, k, v)

    def _mark(self, key, val, reads, writes):
        for b in reads:
            if b.r.get(key, 0) < val:
                b.r[key] = val
        for b in writes:
            b.w = (key, val)
            b.r = {}

    def op(self, e, fn, reads=(), writes=()):
        self._deps(e, reads, writes)
        ins = fn(self.eng[e])
        self.cnt[e] += 1
        ins.then_inc(self.sem[e], 1)
        self.ninst += 1
        self._mark(e, self.cnt[e], reads, writes)
        return ins

    def dma(self, q, out, in_, reads=(), writes=(), **kw):
        self._deps(q, reads, writes)
        k = self.dsem[self.dnext]
        self.dnext = (self.dnext + 1) % len(self.dsem)
        if self.cnt[k] > 0:
            self._wait(q, k, 16 * self.cnt[k])
        ins = self.eng[q].dma_start(out=out, in_=in_, **kw)
        self.cnt[k] += 1
        ins.then_inc(self.sem[k], 16)
        self.ninst += 1
        self._mark(k, 16 * self.cnt[k], reads, writes)
        return ins

    def finish(self, e="sp"):
        for k in self.dsem:
            if self.cnt[k] > 0:
                self._wait(e, k, 16 * self.cnt[k])
        for k in ("pe", "dve", "act", "pool"):
            if self.cnt[k] > 0 and k != e:
                self._wait(e, k, self.cnt[k])

    def barrier(self):
        for e in self.eng:
            for k in self.dsem:
                if self.cnt[k] > 0:
                    self._wait(e, k, 16 * self.cnt[k])
            for k in ("pe", "dve", "act", "pool"):
                if self.cnt[k] > 0 and k != e:
                    self._wait(e, k, self.cnt[k])

    def push_scope(self):
        self._outer = self.es
        self.es = ExitStack()

    def pop_scope(self):
        self.barrier()
        self.es.close()
        self.es = self._outer

    def close(self):
        self.es.close()


class Stream:
    def __init__(self, p, name, shape, nbuf, q):
        self.p = p
        self.q = q
        self.tiles = [p.sb("%s%d" % (name, i), shape) for i in range(nbuf)]
        self.bufs = [Buf() for _ in range(nbuf)]
        self.srcs = []
        self.issued = 0
        self.used = 0

    def plan(self, src):
        self.srcs.append(src)

    def _issue(self):
        k = self.issued
        if k >= len(self.srcs):
            return
        i = k % len(self.tiles)
        src, view = self.srcs[k]
        self.p.dma(self.q, view(self.tiles[i]), src, writes=[self.bufs[i]])
        self.issued += 1

    def get(self):
        k = self.used
        while self.issued <= min(k + len(self.tiles) - 1, len(self.srcs) - 1):
            if self.issued - k >= len(self.tiles):
                break
            self._issue()
        self.used += 1
        i = k % len(self.tiles)
        return self.tiles[i], self.bufs[i]


def bc_last(ap, n):
    sh = list(ap.shape)
    return ap.unsqueeze(len(sh)).to_broadcast(sh + [n])


def build_B(n_lat, n_ctx, final):
    NT = n_lat + n_ctx
    nc = bass.Bass("TRN2", target_bir_lowering=False)
    def ein(name, shape):
        return nc.dram_tensor(name, list(shape), F32, kind="ExternalInput").ap()
    xs = ein("xs", [NT * 128, D])
    fs = ein("fs", [NT * 128, D])
    cond2 = ein("cond2", [2, D])
    mod_w = ein("mod_w", [D, 6 * D])
    mod_b = ein("mod_b", [6 * D])
    norm2 = ein("norm2", [D])
    w_out = ein("w_out", [D, D])
    wq = ein("wq", [D, 2048])
    k1T = ein("k1T", [8, 128, 128])
    k2T = ein("k2T", [8, 128, 128])
    UT = ein("UT", [D, 16384])
    V = ein("V", [16384, D])
    fnorm = ein("fnorm", [D])
    ident_d = ein("ident_in", [128, 128])
    xo = nc.dram_tensor("xo", [NT * 128, D], F32, kind="ExternalOutput").ap()

    p = Prog(nc)
    ident = p.sb("ident", [128, 128]); b_id = Buf()
    p.dma("sp", ident[:], ident_d[:, :], writes=[b_id])

    ps_acc = p.ps("ps_acc", [128, 1024]); b_acc = Buf()
    ps_m = p.ps("ps_m", [128, 1024]); b_m = Buf()
    ps_a = [p.ps("ps_a%d" % i, [128, 512]) for i in range(2)]; b_a = [Buf(), Buf()]
    ps_t = [p.ps("ps_t%d" % i, [128, 512]) for i in range(2)]; b_t = [Buf(), Buf()]

    wst = Stream(p, "wst", [128, 8, 512], 2, "sp")
    vst = Stream(p, "vst", [128, 4, 1024], 2, "pool")
    kcv = lambda t: t[:]
    def wsrc(w, c0):
        return (w[:, c0:c0 + 512].rearrange("(kc p) n -> p kc n", p=128), kcv)

    csil = p.sb("csil", [128, 2, 8]); b_cs = Buf()
    p.dma("sp", csil[:], cond2.rearrange("s (kc p) -> p s kc", p=128), writes=[b_cs], allow_slow_non_contiguous=True)
    p.op("act", lambda e: e.activation(csil[:], csil[:], AF.Silu), reads=[b_cs], writes=[b_cs])
    CB = p.sb("CB", [128, 2, 8, 128]); b_cb = Buf()
    p.op("dve", lambda e: e.tensor_copy(CB[:], bc_last(csil[:], 128)), reads=[b_cs], writes=[b_cb])
    MOD = p.sb("MOD", [128, 2, 4, 1024]); b_mod = Buf()
    tmpb = p.sb("tmpb", [128, 1024]); b_tmpb = Buf()
    for j in range(4):
        p.dma("sp", tmpb[:], mod_b[(2 + j) * D:(3 + j) * D].partition_broadcast(128), writes=[b_tmpb])
        for nb in range(2):
            c0 = (2 + j) * D + nb * 512
            wt = p.sb("mw_%d_%d" % (j, nb), [128, 8, 512]) if False else None
            wst.plan(wsrc(mod_w, c0))
            blk, bb = wst.get()
            for s in range(2):
                for kc in range(8):
                    p.op("pe", lambda e, s=s, kc=kc: e.matmul(ps_a[s][:], lhsT=CB[:, s, kc, :], rhs=blk[:, kc, :],
                                                             start=(kc == 0), stop=(kc == 7)),
                         reads=[b_cb, bb], writes=[b_a[s]])
                p.op("dve", lambda e, s=s: e.tensor_tensor(MOD[:, s, j, nb * 512:(nb + 1) * 512], ps_a[s][:],
                                                           tmpb[:, nb * 512:(nb + 1) * 512], ALU.add),
                     reads=[b_a[s], b_tmpb], writes=[b_mod])
    n2b = p.sb("n2b", [128, 1024]); b_n2 = Buf()
    p.dma("sp", n2b[:], norm2.partition_broadcast(128), writes=[b_n2])
    for s in range(2):
        p.op("dve", lambda e, s=s: e.scalar_tensor_tensor(MOD[:, s, 2, :], in0=MOD[:, s, 2, :], scalar=1.0, in1=n2b[:],
                                                          op0=ALU.add, op1=ALU.mult),
             reads=[b_mod, b_n2], writes=[b_mod])
    if final:
        p.dma("sp", n2b[:], fnorm.partition_broadcast(128), reads=[b_mod], writes=[b_n2])
    k1s = p.sb("k1s", [128, 8, 128]); k2s = p.sb("k2s", [128, 8, 128]); b_k = Buf()
    p.dma("sp", k1s[:], k1T.rearrange("h k i -> k h i"), writes=[b_k])
    p.dma("sp", k2s[:], k2T.rearrange("h k i -> k h i"), writes=[b_k])

    A1 = p.sb("A1", [128, 1024]); b_A1 = Buf()
    A2 = p.sb("A2", [128, 1024]); b_A2 = Buf()
    A3 = p.sb("A3", [128, 1024]); b_A3 = Buf()
    A4 = p.sb("A4", [128, 2048]); b_A4 = Buf()
    A5 = p.sb("A5", [128, 2048]); b_A5 = Buf()
    s1 = p.sb("s1", [128, 1024]); b_s1 = Buf()
    s2 = p.sb("s2", [128, 1024]); b_s2 = Buf()
    e1 = p.sb("e1", [128, 1024]); b_e1 = Buf()
    e2 = p.sb("e2", [128, 1024]); b_e2 = Buf()
    wk = p.sb("wk", [128, 256]); b_wk = Buf()
    v1 = p.sb("v1", [128, 8, 16]); v2 = p.sb("v2", [128, 8, 16]); b_v = Buf()
    c24 = p.sb("c24", [128, 8, 24]); b_c = Buf()
    sm = p.sb("sm", [128, 64]); b_sm = Buf()
    Mw = [p.sb("Mw%d" % i, [128, 1024]) for i in range(2)]; b_Mw = [Buf(), Buf()]
    Wacc = [p.sb("Wacc%d" % i, [128, 512]) for i in range(2)]; b_W = [Buf(), Buf()]
    actt = [p.sb("actt%d" % i, [128, 512]) for i in range(2)]; b_act = [Buf(), Buf()]
    ATt = [p.sb("ATt%d" % i, [128, 512]) for i in range(2)]; b_AT = [Buf(), Buf()]

    for ti in range(NT):
        for nb in range(2):
            wst.plan(wsrc(w_out, nb * 512))
        for nb in range(4):
            wst.plan(wsrc(wq, nb * 512))
        for g in range(32):
            wst.plan(wsrc(UT, g * 512))
            vst.plan((V[g * 512:(g + 1) * 512, :].rearrange("(ib p) d -> p ib d", p=128), kcv))

    def transpose8(src, bsrc, dst, bdst, nblk=8):
        for c in range(nblk):
            p.op("pe", lambda e, c=c: e.transpose(ps_m[:, c * 128:(c + 1) * 128], src[:, c * 128:(c + 1) * 128], ident[:]),
                 reads=[bsrc, b_id], writes=[b_m])
        h = nblk * 64
        p.op("act", lambda e: e.copy(dst[:, 0:h], ps_m[:, 0:h]), reads=[b_m], writes=[bdst])
        p.op("dve", lambda e: e.tensor_copy(dst[:, h:2 * h], ps_m[:, h:2 * h]), reads=[b_m], writes=[bdst])

    for ti in range(NT):
        s = 0 if ti < n_lat else 1
        r0 = ti * 128
        p.dma("sp", A1[:], xs[r0:r0 + 128, :], writes=[b_A1])
        p.dma("sp", A2[:], fs[r0:r0 + 128, :], writes=[b_A2])
        transpose8(A2, b_A2, A3, b_A3)
        for nb in range(2):
            blk, bb = wst.get()
            for kc in range(8):
                p.op("pe", lambda e, kc=kc: e.matmul(ps_acc[:, nb * 512:(nb + 1) * 512], lhsT=A3[:, kc * 128:(kc + 1) * 128],
                                                     rhs=blk[:, kc, :], start=(kc == 0), stop=(kc == 7)),
                     reads=[b_A3, bb], writes=[b_acc])
        p.op("dve", lambda e: e.tensor_tensor(A2[:], ps_acc[:], MOD[:, s, 0, :], ALU.mult), reads=[b_acc, b_mod], writes=[b_A2])
        p.op("dve", lambda e: e.tensor_tensor(A1[:], A1[:], A2[:], ALU.add), reads=[b_A1, b_A2], writes=[b_A1])
        p.op("act", lambda e: e.activation(A2[:], A1[:], AF.Square, accum_out=sm[:, 0:1]), reads=[b_A1], writes=[b_A2, b_sm])
        p.op("dve", lambda e: e.tensor_scalar(sm[:, 1:2], sm[:, 0:1], 1.0 / D, 1e-6, ALU.mult, ALU.add), reads=[b_sm], writes=[b_sm])
        p.op("act", lambda e: e.sqrt(sm[:, 2:3], sm[:, 1:2]), reads=[b_sm], writes=[b_sm])
        p.op("dve", lambda e: e.reciprocal(sm[:, 3:4], sm[:, 2:3]), reads=[b_sm], writes=[b_sm])
        p.op("dve", lambda e: e.scalar_tensor_tensor(A2[:], in0=A1[:], scalar=sm[:, 3:4], in1=MOD[:, s, 2, :],
                                                     op0=ALU.mult, op1=ALU.mult), reads=[b_A1, b_sm, b_mod], writes=[b_A2])
        p.op("dve", lambda e: e.tensor_tensor(A2[:], A2[:], MOD[:, s, 1, :], ALU.add), reads=[b_A2, b_mod], writes=[b_A2])
        transpose8(A2, b_A2, A3, b_A3)
        for nb in range(4):
            blk, bb = wst.get()
            for kc in range(8):
                p.op("pe", lambda e, kc=kc: e.matmul(ps_a[nb % 2][:], lhsT=A3[:, kc * 128:(kc + 1) * 128], rhs=blk[:, kc, :],
                                                     start=(kc == 0), stop=(kc == 7)),
                     reads=[b_A3, bb], writes=[b_a[nb % 2]])
            p.op("act", lambda e: e.copy(A4[:, nb * 512:(nb + 1) * 512], ps_a[nb % 2][:]), reads=[b_a[nb % 2]], writes=[b_A4])
        for half in range(2):
            transpose8(A4[:, half * 1024:(half + 1) * 1024], b_A4, A5[:, half * 1024:(half + 1) * 1024], b_A5)
        for (ks, sd, bsd, part) in ((k1s, s1, b_s1, 0), (k2s, s2, b_s2, 1)):
            for h in range(8):
                c = 2 * h + part
                p.op("pe", lambda e, h=h, c=c: e.matmul(ps_acc[:, h * 128:(h + 1) * 128], lhsT=A5[:, c * 128:(c + 1) * 128],
                                                        rhs=ks[:, h, :], start=True, stop=True),
                     reads=[b_A5, b_k], writes=[b_acc])
            p.op("act", lambda e: e.copy(sd[:], ps_acc[:]), reads=[b_acc], writes=[bsd])
        for (sd, bsd, vv) in ((s1, b_s1, v1), (s2, b_s2, v2)):
            for h in range(8):
                sl = sd[:, h * 128:(h + 1) * 128]
                p.op("dve", lambda e: e.max(out=vv[:, h, 0:8], in_=sl), reads=[bsd], writes=[b_v])
                p.op("dve", lambda e: e.match_replace(out=wk[:, 0:128], in_to_replace=vv[:, h, 0:8], in_values=sl, imm_value=-1e30),
                     reads=[bsd, b_v], writes=[b_wk])
                p.op("dve", lambda e: e.max(out=vv[:, h, 8:16], in_=wk[:, 0:128]), reads=[b_wk], writes=[b_v])
        cand = A5[:, 0:2048].rearrange("p (h a b) -> p h a b", h=8, a=16)
        p.op("dve", lambda e: e.tensor_tensor(cand, v1[:].unsqueeze(3).to_broadcast([128, 8, 16, 16]),
                                              v2[:].unsqueeze(2).to_broadcast([128, 8, 16, 16]), ALU.add),
             reads=[b_v, b_A5], writes=[b_A5])
        for h in range(8):
            cs = A5[:, h * 256:(h + 1) * 256]
            p.op("dve", lambda e: e.max(out=c24[:, h, 0:8], in_=cs), reads=[b_A5], writes=[b_c])
            p.op("dve", lambda e: e.match_replace(out=wk[:], in_to_replace=c24[:, h, 0:8], in_values=cs, imm_value=-1e30),
                 reads=[b_A5, b_c], writes=[b_wk])
            p.op("dve", lambda e: e.max(out=c24[:, h, 8:16], in_=wk[:]), reads=[b_wk], writes=[b_c])
            p.op("dve", lambda e: e.match_replace(out=wk[:], in_to_replace=c24[:, h, 8:16], in_values=wk[:], imm_value=-1e30),
                 reads=[b_wk, b_c], writes=[b_wk])
            p.op("dve", lambda e: e.max(out=c24[:, h, 16:24], in_=wk[:]), reads=[b_wk], writes=[b_c])
        tau = sm[:, 8:16]; zs = sm[:, 16:24]; rz = sm[:, 24:32]
        p.op("dve", lambda e: e.tensor_tensor(tau, c24[:, :, 15], c24[:, :, 16], ALU.add), reads=[b_c], writes=[b_sm])
        p.op("dve", lambda e: e.tensor_scalar(tau, tau, 0.5, None, ALU.mult), reads=[b_sm], writes=[b_sm])
        ex = Mw[0][:, 0:128].rearrange("p (h a) -> p h a", h=8)
        p.op("dve", lambda e: e.tensor_tensor(ex, c24[:, :, 0:16], bc_last(c24[:, :, 0], 16), ALU.subtract),
             reads=[b_c], writes=[b_Mw[0]])
        p.op("act", lambda e: e.activation(ex, ex, AF.Exp), reads=[b_Mw[0]], writes=[b_Mw[0]])
        p.op("dve", lambda e: e.tensor_reduce(zs, ex, AX.X, ALU.add), reads=[b_Mw[0]], writes=[b_sm])
        p.op("dve", lambda e: e.reciprocal(rz, zs), reads=[b_sm], writes=[b_sm])
        s1v = s1[:].rearrange("p (h i) -> p h i", h=8); s2v = s2[:].rearrange("p (h i) -> p h i", h=8)
        e1v = e1[:].rearrange("p (h i) -> p h i", h=8); e2v = e2[:].rearrange("p (h i) -> p h i", h=8)
        p.op("dve", lambda e: e.tensor_tensor(e1v, s1v, bc_last(v1[:, :, 0], 128), ALU.subtract), reads=[b_s1, b_v], writes=[b_e1])
        p.op("act", lambda e: e.activation(e1[:], e1[:], AF.Exp), reads=[b_e1], writes=[b_e1])
        p.op("dve", lambda e: e.tensor_tensor(e1v, e1v, bc_last(rz, 128), ALU.mult), reads=[b_e1, b_sm], writes=[b_e1])
        p.op("dve", lambda e: e.tensor_tensor(e2v, s2v, bc_last(v2[:, :, 0], 128), ALU.subtract), reads=[b_s2, b_v], writes=[b_e2])
        p.op("act", lambda e: e.activation(e2[:], e2[:], AF.Exp), reads=[b_e2], writes=[b_e2])
        p.op("dve", lambda e: e.scalar_tensor_tensor(s1v, in0=s1v, scalar=-1.0, in1=bc_last(tau, 128), op0=ALU.mult, op1=ALU.add),
             reads=[b_s1, b_sm], writes=[b_s1])
        for g in range(32):
            gi = g % 2
            ublk, ub = wst.get()
            for kc in range(8):
                p.op("pe", lambda e, kc=kc: e.matmul(ps_a[gi][:], lhsT=A3[:, kc * 128:(kc + 1) * 128], rhs=ublk[:, kc, :],
                                                     start=(kc == 0), stop=(kc == 7)),
                     reads=[b_A3, ub], writes=[b_a[gi]])
            p.op("act", lambda e: e.activation(actt[gi][:], ps_a[gi][:], AF.Gelu_apprx_tanh), reads=[b_a[gi]], writes=[b_act[gi]])
            for ib in range(4):
                i = g * 4 + ib
                mi = i % 2
                M = Mw[mi][:].rearrange("p (h j) -> p h j", h=8)
                p.op("dve", lambda e: e.tensor_tensor(M, s2v, bc_last(s1v[:, :, i], 128), ALU.is_ge), reads=[b_s2, b_s1], writes=[b_Mw[mi]])
                p.op("dve", lambda e: e.tensor_tensor(M, M, e2v, ALU.mult), reads=[b_Mw[mi], b_e2], writes=[b_Mw[mi]])
                p.op("dve", lambda e: e.tensor_tensor(M, M, bc_last(e1v[:, :, i], 128), ALU.mult), reads=[b_Mw[mi], b_e1], writes=[b_Mw[mi]])
                p.op("dve", lambda e: e.tensor_reduce(Wacc[gi][:, ib * 128:(ib + 1) * 128], Mw[mi][:].rearrange("p (h j) -> p j h", h=8),
                                                      AX.X, ALU.add), reads=[b_Mw[mi]], writes=[b_W[gi]])
            p.op("dve", lambda e: e.tensor_tensor(actt[gi][:], actt[gi][:], Wacc[gi][:], ALU.mult), reads=[b_act[gi], b_W[gi]], writes=[b_act[gi]])
            for ib in range(4):
                p.op("pe", lambda e, ib=ib: e.transpose(ps_t[gi][:, ib * 128:(ib + 1) * 128], actt[gi][:, ib * 128:(ib + 1) * 128], ident[:]),
                     reads=[b_act[gi], b_id], writes=[b_t[gi]])
            p.op("act", lambda e: e.copy(ATt[gi][:], ps_t[gi][:]), reads=[b_t[gi]], writes=[b_AT[gi]])
            vblk, vb = vst.get()
            for ib in range(4):
                for nb in range(2):
                    p.op("pe", lambda e, ib=ib, nb=nb: e.matmul(ps_acc[:, nb * 512:(nb + 1) * 512], lhsT=ATt[gi][:, ib * 128:(ib + 1) * 128],
                                                                rhs=vblk[:, ib, nb * 512:(nb + 1) * 512],
                                                                start=(g == 0 and ib == 0), stop=(g == 31 and ib == 3)),
                         reads=[b_AT[gi], vb], writes=[b_acc])
        p.op("dve", lambda e: e.tensor_tensor(A2[:], ps_acc[:], MOD[:, s, 3, :], ALU.mult), reads=[b_acc, b_mod], writes=[b_A2])
        p.op("dve", lambda e: e.tensor_tensor(A1[:], A1[:], A2[:], ALU.add), reads=[b_A1, b_A2], writes=[b_A1])
        if final:
            p.op("act", lambda e: e.activation(A2[:], A1[:], AF.Square, accum_out=sm[:, 0:1]), reads=[b_A1], writes=[b_A2, b_sm])
            p.op("dve", lambda e: e.tensor_scalar(sm[:, 1:2], sm[:, 0:1], 1.0 / D, 1e-6, ALU.mult, ALU.add), reads=[b_sm], writes=[b_sm])
            p.op("act", lambda e: e.sqrt(sm[:, 2:3], sm[:, 1:2]), reads=[b_sm], writes=[b_sm])
            p.op("dve", lambda e: e.reciprocal(sm[:, 3:4], sm[:, 2:3]), reads=[b_sm], writes=[b_sm])
            p.op("dve", lambda e: e.scalar_tensor_tensor(A1[:], in0=A1[:], scalar=sm[:, 3:4], in1=n2b[:], op0=ALU.mult, op1=ALU.mult),
                 reads=[b_A1, b_sm, b_n2], writes=[b_A1])
        p.dma("sp", xo[r0:r0 + 128, :], A1[:], reads=[b_A1])
    p.finish("sp")
    p.close()
    return nc


class Tl:
    def __init__(self, t):
        self.t = t
        self.b = Buf()

    def __getitem__(self, k):
        return self.t[k]


def mk(p, name, shape, dt=F32):
    return Tl(p.sb(name, shape, dt))


def mkps(p, name, shape):
    return Tl(p.ps(name, shape))


def O(p, e, fn, outs, ins):
    return p.op(e, fn, reads=[i.b for i in ins], writes=[o.b for o in outs])


def adaln_chunks(p, cond2, mod_w, mod_b, chunks, MOD, psA, name):
    csil = mk(p, name + "csil", [128, 2, 8])
    p.dma("sp", csil[:], cond2.rearrange("s (kc p) -> p s kc", p=128), writes=[csil.b], allow_slow_non_contiguous=True)
    O(p, "act", lambda e: e.activation(csil[:], csil[:], AF.Silu), [csil], [csil])
    CB = mk(p, name + "CB", [128, 2, 8, 128])
    O(p, "dve", lambda e: e.tensor_copy(CB[:], bc_last(csil[:], 128)), [CB], [csil])
    tmpb = mk(p, name + "tmpb", [128, 1024])
    wb = [mk(p, name + "mwb%d" % i, [128, 8, 512]) for i in range(2)]
    k = 0
    for j, ch in enumerate(chunks):
        p.dma("sp", tmpb[:], mod_b[ch * D:(ch + 1) * D].partition_broadcast(128), reads=[tmpb.b], writes=[tmpb.b])
        for nb in range(2):
            c0 = ch * D + nb * 512
            blk = wb[k % 2]; k += 1
            p.dma("sp", blk[:], mod_w[:, c0:c0 + 512].rearrange("(kc p) n -> p kc n", p=128), writes=[blk.b])
            for s in range(2):
                for kc in range(8):
                    O(p, "pe", lambda e, s=s, kc=kc: e.matmul(psA[s][:], lhsT=CB[:, s, kc, :], rhs=blk[:, kc, :],
                                                             start=(kc == 0), stop=(kc == 7)), [psA[s]], [CB, blk])
                O(p, "dve", lambda e, s=s: e.tensor_tensor(MOD[:, s, j, nb * 512:(nb + 1) * 512], psA[s][:],
                                                           tmpb[:, nb * 512:(nb + 1) * 512], ALU.add), [MOD], [psA[s], tmpb])


def rms_mod(p, X, H, sm, G, SH, s):
    (Gt, gj), (St, sj) = G, SH
    O(p, "act", lambda e: e.activation(H[:], X[:], AF.Square, accum_out=sm[:, 0:1]), [H, sm], [X])
    O(p, "dve", lambda e: e.tensor_scalar(sm[:, 1:2], sm[:, 0:1], 1.0 / D, 1e-6, ALU.mult, ALU.add), [sm], [sm])
    O(p, "act", lambda e: e.sqrt(sm[:, 2:3], sm[:, 1:2]), [sm], [sm])
    O(p, "dve", lambda e: e.reciprocal(sm[:, 3:4], sm[:, 2:3]), [sm], [sm])
    O(p, "dve", lambda e: e.scalar_tensor_tensor(H[:], in0=X[:], scalar=sm[:, 3:4], in1=Gt[:, s, gj, :], op0=ALU.mult, op1=ALU.mult),
      [H], [X, sm, Gt])
    O(p, "dve", lambda e: e.tensor_tensor(H[:], H[:], St[:, s, sj, :], ALU.add), [H], [H, St])


def transpose_blocks(p, src, dst, psM, ident, nblk):
    for c in range(nblk):
        O(p, "pe", lambda e, c=c: e.transpose(psM[:, c * 128:(c + 1) * 128], src[:, c * 128:(c + 1) * 128], ident[:]), [psM], [src, ident])
    h = nblk * 64
    O(p, "act", lambda e: e.copy(dst[:, 0:h], psM[:, 0:h]), [dst], [psM])
    O(p, "dve", lambda e: e.tensor_copy(dst[:, h:2 * h], psM[:, h:2 * h]), [dst], [psM])


NW = 2064
NO1 = 21


def build_A1(n_ctx, n_lat):
    NT = n_ctx + n_lat
    nc = bass.Bass("TRN2", target_bir_lowering=False)
    def ein(name, shape):
        return nc.dram_tensor(name, list(shape), F32, kind="ExternalInput").ap()
    xs = ein("xs", [NT * 128, D])
    cond2 = ein("cond2", [2, D])
    mod_w = ein("mod_w", [D, 6 * D]); mod_b = ein("mod_b", [6 * D]); norm1 = ein("norm1", [D])
    Wg = ein("Wg", [D, NW])
    convw = ein("convw", [3 * 768]); mu = ein("mu", [2 * 1024])
    alog = ein("alog", [8]); dtb = ein("dtb", [8])
    w0 = ein("w0", [512]); a0 = ein("a0", [512]); k_k = ein("k_k", [256]); k_a = ein("k_a", [256]); r_k = ein("r_k", [256])
    w2 = ein("w2", [64, 512]); a2 = ein("a2", [64, 256]); g2 = ein("g2", [128, 256])
    ident_d = ein("ident_in", [128, 128])
    out = nc.dram_tensor("o1", [NT * 128, NO1 * 256], F32, kind="ExternalOutput").ap()
    p = Prog(nc)
    Pd = p.dram("Pd", [NT * 128 + 4, NW]); b_Pd = Buf()
    ident = mk(p, "ident", [128, 128])
    p.dma("sp", ident[:], ident_d[:, :], writes=[ident.b])
    psA = [mkps(p, "psA%d" % i, [128, 512]) for i in range(4)]
    psM = mkps(p, "psM", [128, 1024])
    psL = mkps(p, "psL", [128, 1024])
    MOD = mk(p, "MOD", [128, 2, 2, 1024])
    p.push_scope()
    adaln_chunks(p, cond2, mod_w, mod_b, [0, 1], MOD, psA, "ad")
    n1b = mk(p, "n1b", [128, 1024])
    p.dma("sp", n1b[:], norm1.partition_broadcast(128), writes=[n1b.b])
    for s in range(2):
        O(p, "dve", lambda e, s=s: e.scalar_tensor_tensor(MOD[:, s, 1, :], in0=MOD[:, s, 1, :], scalar=1.0, in1=n1b[:],
                                                          op0=ALU.add, op1=ALU.mult), [MOD], [MOD, n1b])
    Ws = mk(p, "Ws", [128, 8, NW])
    for kc in range(8):
        p.dma("sp" if kc % 2 == 0 else "pool", Ws[:, kc, :], Wg[kc * 128:(kc + 1) * 128, :], writes=[Ws.b])
    X = mk(p, "X", [128, 1024]); H = mk(p, "H", [128, 1024]); HT = mk(p, "HT", [128, 1024]); sm = mk(p, "sm", [128, 64])
    Pt = [mk(p, "Pt%d" % i, [128, NW]) for i in range(1)]
    zrow = mk(p, "zrow", [1, NW])
    O(p, "dve", lambda e: e.memset(zrow[:], 0.0), [zrow], [])
    rowof = lambda ti: (1 + ti * 128) if ti < n_ctx else (3 + ti * 128)
    for r in (0, n_ctx * 128 + 1, n_ctx * 128 + 2, NT * 128 + 3):
        p.dma("sp", Pd[r:r + 1, :], zrow[:], reads=[zrow.b], writes=[b_Pd])
    for ti in range(NT):
        s = 1 if ti < n_ctx else 0
        p.dma("sp", X[:], xs[ti * 128:(ti + 1) * 128, :], writes=[X.b])
        rms_mod(p, X, H, sm, (MOD, 1), (MOD, 0), s)
        transpose_blocks(p, H, HT, psM, ident, 8)
        PT = Pt[0]
        for nb in range(5):
            c0 = nb * 512; cw = min(512, NW - c0)
            ps = psA[nb % 4]
            for kc in range(8):
                O(p, "pe", lambda e, kc=kc: e.matmul(ps[:, 0:cw], lhsT=HT[:, kc * 128:(kc + 1) * 128], rhs=Ws[:, kc, c0:c0 + cw],
                                                     start=(kc == 0), stop=(kc == 7)), [ps], [HT, Ws])
            O(p, "act" if nb % 2 == 0 else "dve",
              (lambda e: e.copy(PT[:, c0:c0 + cw], ps[:, 0:cw])) if nb % 2 == 0 else (lambda e: e.tensor_copy(PT[:, c0:c0 + cw], ps[:, 0:cw])),
              [PT], [ps])
        r0 = rowof(ti)
        p.dma("sp", Pd[r0:r0 + 128, :], PT[:], reads=[PT.b], writes=[b_Pd])
    p.pop_scope()
    def bload(name, src, n):
        t = mk(p, name, [128, n])
        p.dma("sp", t[:], src.partition_broadcast(128), writes=[t.b])
        return t
    cw_t = bload("cw_t", convw, 3 * 768); mu_t = bload("mu_t", mu, 2048)
    al_t = bload("al_t", alog, 8); dtb_t = bload("dtb_t", dtb, 8)
    w0_t = bload("w0_t", w0, 512); a0_t = bload("a0_t", a0, 512)
    kk_t = bload("kk_t", k_k, 256); ka_t = bload("ka_t", k_a, 256); rk_t = bload("rk_t", r_k, 256)
    O(p, "act", lambda e: e.activation(al_t[:], al_t[:], AF.Exp), [al_t], [al_t])
    O(p, "dve", lambda e: e.tensor_scalar(al_t[:], al_t[:], -1.0, None, ALU.mult), [al_t], [al_t])
    lw = mk(p, "lw", [128, 768])
    p.dma("sp", lw[0:64, 0:512], w2[:, :], writes=[lw.b])
    p.dma("sp", lw[64:128, 0:256], a2[:, :], writes=[lw.b])
    p.dma("sp", lw[:, 512:768], g2[:, :], writes=[lw.b])
    Pc = mk(p, "Pc", [128, NW]); Pp = mk(p, "Pp", [128, 2048]); Pn = mk(p, "Pn", [128, 2048])
    T1 = mk(p, "T1", [128, 1024]); T2 = mk(p, "T2", [128, 1024]); Q = mk(p, "Q", [128, 768]); PR = mk(p, "PR", [128, 1024])
    OUT = [mk(p, "OUT%d" % i, [128, NO1 * 256]) for i in range(1)]
    L = mk(p, "L", [128, 256]); LT = mk(p, "LT", [128, 256])
    s8 = mk(p, "s8", [128, 64])
    def v3(ap):
        return ap.rearrange("p (h k) -> p h k", k=64)
    for ti in range(NT):
        r0 = rowof(ti)
        Ot = OUT[0]
        oc = lambda j: Ot[:, j * 256:(j + 1) * 256]
        p.dma("sp", Pc[:], Pd[r0:r0 + 128, :], reads=[b_Pd], writes=[Pc.b])
        p.dma("pool", Pp[:], Pd[r0 - 1:r0 + 127, 0:2048], reads=[b_Pd], writes=[Pp.b])
        p.dma("pool", Pn[:], Pd[r0 + 1:r0 + 129, 0:2048], reads=[b_Pd], writes=[Pn.b])
        O(p, "dve", lambda e: e.tensor_tensor(Q[:], Pp[:, 0:768], cw_t[:, 0:768], ALU.mult), [Q], [Pp, cw_t])
        O(p, "dve", lambda e: e.tensor_tensor(T1[:, 0:768], Pc[:, 0:768], cw_t[:, 768:1536], ALU.mult), [T1], [Pc, cw_t])
        O(p, "dve", lambda e: e.tensor_tensor(Q[:], Q[:], T1[:, 0:768], ALU.add), [Q], [Q, T1])
        O(p, "dve", lambda e: e.tensor_tensor(T1[:, 0:768], Pn[:, 0:768], cw_t[:, 1536:2304], ALU.mult), [T1], [Pn, cw_t])
        O(p, "dve", lambda e: e.tensor_tensor(Q[:], Q[:], T1[:, 0:768], ALU.add), [Q], [Q, T1])
        O(p, "act", lambda e: e.activation(Q[:], Q[:], AF.Silu), [Q], [Q])
        O(p, "act", lambda e: e.activation(T1[:, 0:512], Q[:, 0:512], AF.Square), [T1], [Q])
        O(p, "dve", lambda e: e.tensor_reduce(s8[:, 0:8], v3(T1[:, 0:512]), AX.X, ALU.add), [s8], [T1])
        O(p, "dve", lambda e: e.tensor_scalar(s8[:, 0:8], s8[:, 0:8], 1e-6, None, ALU.add), [s8], [s8])
        O(p, "act", lambda e: e.sqrt(s8[:, 0:8], s8[:, 0:8]), [s8], [s8])
        O(p, "dve", lambda e: e.reciprocal(s8[:, 0:8], s8[:, 0:8]), [s8], [s8])
        O(p, "dve", lambda e: e.scalar_tensor_tensor(v3(oc(1)), in0=v3(Q[:, 0:256]), scalar=0.125, in1=bc_last(s8[:, 0:4], 64),
                                                     op0=ALU.mult, op1=ALU.mult), [Ot], [Q, s8])
        O(p, "dve", lambda e: e.tensor_tensor(v3(T2[:, 0:256]), v3(Q[:, 256:512]), bc_last(s8[:, 4:8], 64), ALU.mult), [T2], [Q, s8])
        O(p, "dve", lambda e: e.tensor_scalar(oc(0), T2[:, 0:256], -1.0, None, ALU.mult), [Ot], [T2])
        O(p, "act", lambda e: e.copy(oc(2), Q[:, 512:768]), [Ot], [Q])
        O(p, "dve", lambda e: e.tensor_tensor(s8[:, 8:16], Pc[:, 2048:2056], dtb_t[:], ALU.add), [s8], [Pc, dtb_t])
        O(p, "act", lambda e: e.activation(s8[:, 8:16], s8[:, 8:16], AF.Exp), [s8], [s8])
        O(p, "act", lambda e: e.activation(s8[:, 8:16], s8[:, 8:16], AF.Ln, bias=1.0), [s8], [s8])
        O(p, "dve", lambda e: e.tensor_tensor(s8[:, 8:16], s8[:, 8:16], al_t[:], ALU.mult), [s8], [s8, al_t])
        O(p, "act", lambda e: e.activation(s8[:, 8:16], s8[:, 8:16], AF.Exp), [s8], [s8])
        O(p, "act", lambda e: e.activation(s8[:, 16:24], Pc[:, 2056:2064], AF.Sigmoid), [s8], [Pc])
        for d in range(2):
            O(p, "dve", lambda e, d=d: e.tensor_copy(v3(oc(3 + 3 * d)), bc_last(s8[:, 8 + 4 * d:12 + 4 * d], 64)), [Ot], [s8])
            O(p, "dve", lambda e, d=d: e.tensor_tensor(v3(oc(5 + 3 * d)), v3(T2[:, 0:256]), bc_last(s8[:, 16 + 4 * d:20 + 4 * d], 64), ALU.mult),
              [Ot], [T2, s8])
            O(p, "dve", lambda e, d=d: e.tensor_tensor(v3(oc(4 + 3 * d)), v3(oc(5 + 3 * d)), bc_last(s8[:, 8 + 4 * d:12 + 4 * d], 64), ALU.mult),
              [Ot], [Ot, s8])
        O(p, "act", lambda e: e.activation(oc(18), Pc[:, 768:1024], AF.Silu), [Ot], [Pc])
        O(p, "dve", lambda e: e.tensor_tensor(T1[:], Pp[:, 1024:2048], Pc[:, 1024:2048], ALU.subtract), [T1], [Pp, Pc])
        O(p, "dve", lambda e: e.tensor_tensor(T1[:], T1[:], mu_t[:, 0:1024], ALU.mult), [T1], [T1, mu_t])
        O(p, "dve", lambda e: e.tensor_tensor(PR[:], Pc[:, 1024:2048], T1[:], ALU.add), [PR], [Pc, T1])
        O(p, "dve", lambda e: e.tensor_tensor(T1[:], Pn[:, 1024:2048], Pc[:, 1024:2048], ALU.subtract), [T1], [Pn, Pc])
        O(p, "dve", lambda e: e.tensor_tensor(T1[:], T1[:], mu_t[:, 1024:2048], ALU.mult), [T1], [T1, mu_t])
        O(p, "dve", lambda e: e.tensor_tensor(PR[:], PR[:], T1[:], ALU.add), [PR], [PR, T1])
        O(p, "act", lambda e: e.copy(oc(10), PR[:, 0:256]), [Ot], [PR])
        O(p, "act", lambda e: e.copy(oc(11), PR[:, 512:768]), [Ot], [PR])
        O(p, "dve", lambda e: e.tensor_tensor(T2[:, 256:512], PR[:, 256:512], kk_t[:], ALU.mult), [T2], [PR, kk_t])
        O(p, "act", lambda e: e.activation(T1[:, 0:256], T2[:, 256:512], AF.Square), [T1], [T2])
        O(p, "dve", lambda e: e.tensor_reduce(s8[:, 24:28], v3(T1[:, 0:256]), AX.X, ALU.add), [s8], [T1])
        O(p, "dve", lambda e: e.tensor_scalar(s8[:, 24:28], s8[:, 24:28], 1e-6, None, ALU.add), [s8], [s8])
        O(p, "act", lambda e: e.sqrt(s8[:, 24:28], s8[:, 24:28]), [s8], [s8])
        O(p, "dve", lambda e: e.reciprocal(s8[:, 24:28], s8[:, 24:28]), [s8], [s8])
        O(p, "dve", lambda e: e.tensor_tensor(v3(T2[:, 256:512]), v3(T2[:, 256:512]), bc_last(s8[:, 24:28], 64), ALU.mult), [T2], [T2, s8])
        O(p, "dve", lambda e: e.tensor_scalar(oc(9), T2[:, 256:512], -1.0, None, ALU.mult), [Ot], [T2])
        O(p, "act", lambda e: e.activation(L[:, 0:64], PR[:, 768:832], AF.Tanh), [L], [PR])
        O(p, "act", lambda e: e.copy(L[:, 64:128], PR[:, 832:896]), [L], [PR])
        O(p, "act", lambda e: e.activation(L[:, 128:256], PR[:, 896:1024], AF.Sigmoid), [L], [PR])
        transpose_blocks(p, L, LT, psM, ident, 2)
        for d in range(2):
            O(p, "pe", lambda e, d=d: e.matmul(psL[:, d * 256:(d + 1) * 256], lhsT=LT[0:64, 0:128], rhs=lw[0:64, d * 256:(d + 1) * 256],
                                               start=True, stop=True), [psL], [LT, lw])
        O(p, "pe", lambda e: e.matmul(psL[:, 512:768], lhsT=LT[64:128, 0:128], rhs=lw[64:128, 0:256], start=True, stop=True), [psL], [LT, lw])
        O(p, "pe", lambda e: e.matmul(psL[:, 768:1024], lhsT=LT[:, 128:256], rhs=lw[:, 512:768], start=True, stop=True), [psL], [LT, lw])
        O(p, "act", lambda e: e.copy(oc(20), psL[:, 768:1024]), [Ot], [psL])
        O(p, "dve", lambda e: e.tensor_tensor(T1[:, 512:768], PR[:, 0:256], rk_t[:], ALU.mult), [T1], [PR, rk_t])
        for d in range(2):
            O(p, "dve", lambda e, d=d: e.tensor_tensor(T1[:, 0:256], psL[:, d * 256:(d + 1) * 256], w0_t[:, d * 256:(d + 1) * 256], ALU.add),
              [T1], [psL, w0_t])
            O(p, "act", lambda e: e.activation(T1[:, 0:256], T1[:, 0:256], AF.Exp, scale=-1.0), [T1], [T1])
            O(p, "dve", lambda e: e.tensor_scalar(T1[:, 0:256], T1[:, 0:256], 1.0, None, ALU.add), [T1], [T1])
            O(p, "dve", lambda e: e.reciprocal(T1[:, 0:256], T1[:, 0:256]), [T1], [T1])
            O(p, "act", lambda e, d=d: e.activation(oc(12 + 3 * d), T1[:, 0:256], AF.Exp, scale=-0.6065306597126334), [Ot], [T1])
            O(p, "dve", lambda e, d=d: e.tensor_tensor(T1[:, 256:512], psL[:, 512:768], a0_t[:, d * 256:(d + 1) * 256], ALU.add), [T1], [psL, a0_t])
            O(p, "act", lambda e: e.activation(T1[:, 256:512], T1[:, 256:512], AF.Sigmoid), [T1], [T1])
            O(p, "dve", lambda e, d=d: e.tensor_tensor(oc(13 + 3 * d), T2[:, 256:512], T1[:, 256:512], ALU.mult), [Ot], [T2, T1])
            O(p, "dve", lambda e: e.scalar_tensor_tensor(T1[:, 256:512], in0=T1[:, 256:512], scalar=-1.0, in1=ka_t[:], op0=ALU.add, op1=ALU.mult),
              [T1], [T1, ka_t])
            O(p, "dve", lambda e, d=d: e.scalar_tensor_tensor(oc(14 + 3 * d), in0=T1[:, 256:512], scalar=1.0, in1=PR[:, 256:512], op0=ALU.add, op1=ALU.mult),
              [Ot], [T1, PR])
            O(p, "dve", lambda e, d=d: e.tensor_tensor(T1[:, 768:1024], T1[:, 512:768], oc(14 + 3 * d), ALU.mult), [T1], [T1, Ot])
            O(p, "dve", lambda e, d=d: e.tensor_reduce(s8[:, 32 + 4 * d:36 + 4 * d], v3(T1[:, 768:1024]), AX.X, ALU.add), [s8], [T1])
        O(p, "dve", lambda e: e.tensor_tensor(s8[:, 32:36], s8[:, 32:36], s8[:, 36:40], ALU.add), [s8], [s8])
        O(p, "dve", lambda e: e.tensor_tensor(v3(oc(19)), v3(PR[:, 512:768]), bc_last(s8[:, 32:36], 64), ALU.mult), [Ot], [PR, s8])
        p.dma("sp", out[ti * 128:(ti + 1) * 128, :], Ot[:], reads=[Ot.b])
    p.finish("sp")
    p.close()
    return nc


TB = 4
TBV = 128


def build_A2(S_steps):
    nc = bass.Bass("TRN2", target_bir_lowering=False)
    names = ["sc_nk", "sc_w", "sc_bb", "sc_kt", "sc_r"]
    sc = [nc.dram_tensor(n, [2, S_steps, 512], F32, kind="ExternalInput").ap() for n in names]
    vt = nc.dram_tensor("vt", [128, S_steps, 8], F32, kind="ExternalInput").ap()
    yo = nc.dram_tensor("yo", [128, S_steps, 8], F32, kind="ExternalOutput").ap()
    p = Prog(nc)
    p.nosync = {"dve"}
    BR = [[mk(p, "br%d_%d" % (q, i), [128, TB, 512]) for i in range(2)] for q in range(5)]
    Vc = [mk(p, "vc%d" % i, [128, TBV, 8]) for i in range(2)]
    Yb = [mk(p, "yb%d" % i, [128, TBV, 8]) for i in range(2)]
    St = mk(p, "St", [128, 512]); tmp = mk(p, "tmp", [128, 512]); sa = mk(p, "sa", [128, 8])
    O(p, "dve", lambda e: e.memset(St[:], 0.0), [St], [])
    v3 = lambda ap: ap.rearrange("p (g k) -> p g k", k=64)
    nblk = S_steps // TB

    def load_blk(bi):
        for q in range(5):
            t = BR[q][bi % 2]
            for m in range(2):
                src = sc[q][m, bi * TB:(bi + 1) * TB, :].rearrange("s n -> (s n)").partition_broadcast(64)
                p.dma("sp" if m == 0 else "act", t[m * 64:(m + 1) * 64, :, :].rearrange("p s n -> p (s n)"), src, writes=[t.b])

    load_blk(0)
    for bi in range(nblk):
        if bi + 1 < nblk:
            load_blk(bi + 1)
        for j in range(TB):
            s = bi * TB + j
            vb = s // TBV
            if s % TBV == 0:
                p.dma("pool", Vc[vb % 2][:], vt[:, s:s + TBV, :], writes=[Vc[vb % 2].b])
            V_ = Vc[vb % 2]; Y_ = Yb[vb % 2]
            NK, W, BB, KT, R = [BR[q][bi % 2] for q in range(5)]
            O(p, "dve", lambda e: e.tensor_tensor(tmp[:], St[:], NK[:, j, :], ALU.mult), [tmp], [St, NK])
            O(p, "dve", lambda e: e.tensor_reduce(sa[:], v3(tmp[:]), AX.X, ALU.add), [sa], [tmp])
            O(p, "dve", lambda e: e.tensor_tensor(St[:], St[:], W[:, j, :], ALU.mult), [St], [St, W])
            O(p, "dve", lambda e: e.tensor_tensor(v3(tmp[:]), v3(BB[:, j, :]), bc_last(sa[:], 64), ALU.mult), [tmp], [BB, sa])
            O(p, "dve", lambda e: e.tensor_tensor(St[:], St[:], tmp[:], ALU.add), [St], [St, tmp])
            O(p, "dve", lambda e: e.tensor_tensor(v3(tmp[:]), v3(KT[:, j, :]), bc_last(V_[:, s % TBV, :], 64), ALU.mult), [tmp], [KT, V_])
            O(p, "dve", lambda e: e.tensor_tensor(St[:], St[:], tmp[:], ALU.add), [St], [St, tmp])
            O(p, "dve", lambda e: e.tensor_tensor(tmp[:], St[:], R[:, j, :], ALU.mult), [tmp], [St, R])
            O(p, "dve", lambda e: e.tensor_reduce(Y_[:, s % TBV, :], v3(tmp[:]), AX.X, ALU.add), [Y_], [tmp])
            if s % TBV == TBV - 1:
                p.dma("pool", yo[:, s - TBV + 1:s + 1, :], Y_[:], reads=[Y_.b])
    p.finish("sp")
    p.close()
    return nc


def build_A3(NT):
    nc = bass.Bass("TRN2", target_bir_lowering=False)
    def ein(name, shape):
        return nc.dram_tensor(name, list(shape), F32, kind="ExternalInput").ap()
    yin = ein("yin", [NT * 128, 4 * 256])
    aux = ein("aux", [NT * 128, 3 * 256])
    gnw = ein("gnw", [256]); lnw = ein("lnw", [256]); lnb = ein("lnb", [256])
    out = nc.dram_tensor("feat", [NT * 128, 512], F32, kind="ExternalOutput").ap()
    p = Prog(nc)
    def bload(name, src, n):
        t = mk(p, name, [128, n])
        p.dma("sp", t[:], src.partition_broadcast(128), writes=[t.b])
        return t
    gn_t = bload("gn_t", gnw, 256); lw_t = bload("lw_t", lnw, 256); lb_t = bload("lb_t", lnb, 256)
    Y = [mk(p, "Y%d" % i, [128, 1024]) for i in range(2)]; A = [mk(p, "A%d" % i, [128, 768]) for i in range(2)]
    Fo = [mk(p, "Fo%d" % i, [128, 512]) for i in range(2)]
    o = mk(p, "o", [128, 256]); t1 = mk(p, "t1", [128, 256]); s8 = mk(p, "s8", [128, 16])
    v3 = lambda ap: ap.rearrange("p (h k) -> p h k", k=64)
    for ti in range(NT):
        Yt = Y[ti % 2]; At = A[ti % 2]; F = Fo[ti % 2]
        p.dma("sp", Yt[:], yin[ti * 128:(ti + 1) * 128, :], writes=[Yt.b])
        p.dma("pool", At[:], aux[ti * 128:(ti + 1) * 128, :], writes=[At.b])
        O(p, "dve", lambda e: e.tensor_tensor(o[:], Yt[:, 0:256], Yt[:, 256:512], ALU.add), [o], [Yt])
        O(p, "act", lambda e: e.activation(t1[:], o[:], AF.Square), [t1], [o])
        O(p, "dve", lambda e: e.tensor_reduce(s8[:, 0:4], v3(t1[:]), AX.X, ALU.add), [s8], [t1])
        O(p, "dve", lambda e: e.tensor_scalar(s8[:, 0:4], s8[:, 0:4], 1.0 / 64, 1e-6, ALU.mult, ALU.add), [s8], [s8])
        O(p, "act", lambda e: e.sqrt(s8[:, 0:4], s8[:, 0:4]), [s8], [s8])
        O(p, "dve", lambda e: e.reciprocal(s8[:, 0:4], s8[:, 0:4]), [s8], [s8])
        O(p, "dve", lambda e: e.tensor_tensor(v3(o[:]), v3(o[:]), bc_last(s8[:, 0:4], 64), ALU.mult), [o], [o, s8])
        O(p, "dve", lambda e: e.tensor_tensor(o[:], o[:], gn_t[:], ALU.mult), [o], [o, gn_t])
        O(p, "dve", lambda e: e.tensor_tensor(F[:, 0:256], o[:], At[:, 0:256], ALU.mult), [F], [o, At])
        O(p, "dve", lambda e: e.tensor_tensor(o[:], Yt[:, 512:768], Yt[:, 768:1024], ALU.add), [o], [Yt])
        O(p, "dve", lambda e: e.tensor_reduce(s8[:, 4:8], v3(o[:]), AX.X, ALU.add), [s8], [o])
        O(p, "dve", lambda e: e.tensor_scalar(s8[:, 4:8], s8[:, 4:8], -1.0 / 64, None, ALU.mult), [s8], [s8])
        O(p, "dve", lambda e: e.tensor_tensor(v3(o[:]), v3(o[:]), bc_last(s8[:, 4:8], 64), ALU.add), [o], [o, s8])
        O(p, "act", lambda e: e.activation(t1[:], o[:], AF.Square), [t1], [o])
        O(p, "dve", lambda e: e.tensor_reduce(s8[:, 8:12], v3(t1[:]), AX.X, ALU.add), [s8], [t1])
        O(p, "dve", lambda e: e.tensor_scalar(s8[:, 8:12], s8[:, 8:12], 1.0 / 64, 64e-5, ALU.mult, ALU.add), [s8], [s8])
        O(p, "act", lambda e: e.sqrt(s8[:, 8:12], s8[:, 8:12]), [s8], [s8])
        O(p, "dve", lambda e: e.reciprocal(s8[:, 8:12], s8[:, 8:12]), [s8], [s8])
        O(p, "dve", lambda e: e.tensor_tensor(v3(o[:]), v3(o[:]), bc_last(s8[:, 8:12], 64), ALU.mult), [o], [o, s8])
        O(p, "dve", lambda e: e.tensor_tensor(o[:], o[:], lw_t[:], ALU.mult), [o], [o, lw_t])
        O(p, "dve", lambda e: e.tensor_tensor(o[:], o[:], lb_t[:], ALU.add), [o], [o, lb_t])
        O(p, "dve", lambda e: e.tensor_tensor(o[:], o[:], At[:, 256:512], ALU.add), [o], [o, At])
        O(p, "dve", lambda e: e.tensor_tensor(F[:, 256:512], o[:], At[:, 512:768], ALU.mult), [F], [o, At])
        p.dma("sp", out[ti * 128:(ti + 1) * 128, :], F[:], reads=[F.b])
    p.finish("sp")
    p.close()
    return nc


def l0_core_inputs(b, g, inp, x_seq, n_ctx, n_lat):
    hs = slice(g * 256, (g + 1) * 256)
    w_in = inp["l0_w_in"]
    o_qkv, o_z, o_ab, o_r = 0, 1536, 2048, 2080
    cols = np.concatenate([
        o_qkv + np.arange(g * 256, (g + 1) * 256), o_qkv + 512 + np.arange(g * 256, (g + 1) * 256),
        o_qkv + 1024 + np.arange(g * 256, (g + 1) * 256), o_z + np.arange(g * 256, (g + 1) * 256),
        o_r + np.arange(g * 256, (g + 1) * 256), o_r + 512 + np.arange(g * 256, (g + 1) * 256),
        o_r + 1024 + np.arange(g * 256, (g + 1) * 256), o_r + 1536 + np.arange(256),
        np.concatenate([o_ab + j * 8 + np.arange(g * 4, (g + 1) * 4) for j in range(4)])])
    qkv_cols = cols[0:768] - o_qkv
    r_cols = cols[1024:2048] - o_r
    return dict(
        xs=x_seq, cond2=np.stack([inp["c"][b], inp["c_ctx"]]), mod_w=inp["l0_mod_w"], mod_b=inp["l0_mod_b"], norm1=inp["l0_norm1"],
        Wg=np.ascontiguousarray(w_in[:, cols]),
        convw=np.ascontiguousarray(inp["l0_gdn_conv"][:, qkv_cols]).reshape(-1),
        mu=np.ascontiguousarray(inp["l0_rwkv_mu"][:, r_cols]).reshape(-1),
        alog=np.ascontiguousarray(inp["l0_gdn_A_log"][:, g * 4:(g + 1) * 4]).reshape(-1),
        dtb=np.ascontiguousarray(inp["l0_gdn_dt_bias"][:, g * 4:(g + 1) * 4]).reshape(-1),
        w0=np.ascontiguousarray(inp["l0_rwkv_w0"][:, hs]).reshape(-1), a0=np.ascontiguousarray(inp["l0_rwkv_a0"][:, hs]).reshape(-1),
        k_k=np.ascontiguousarray(inp["l0_rwkv_k_k"][hs]), k_a=np.ascontiguousarray(inp["l0_rwkv_k_a"][hs]),
        r_k=np.ascontiguousarray(inp["l0_rwkv_r_k"][g * 4:(g + 1) * 4]).reshape(-1),
        w2=np.ascontiguousarray(inp["l0_rwkv_w2"][:, :, hs].transpose(1, 0, 2)).reshape(64, 512),
        a2=np.ascontiguousarray(inp["l0_rwkv_a2"][:, hs]), g2=np.ascontiguousarray(inp["l0_rwkv_g2"][:, hs]),
        ident_in=np.eye(128, dtype=np.float32))


def _rev(a, nc_rows):
    return np.concatenate([a[:nc_rows][::-1], a[nc_rows:][::-1]], axis=0)


def l0_scan_inputs(o1, nc_rows):
    S = o1.shape[0]
    col = lambda j: o1[:, j * 256:(j + 1) * 256]
    res = {}
    spec = {"sc_nk": ((0, 0), (9, 9)), "sc_w": ((3, 6), (12, 15)), "sc_bb": ((4, 7), (13, 16)), "sc_kt": ((5, 8), (14, 17)), "sc_r": ((1, 1), (10, 10))}
    for name, ms in spec.items():
        arr = np.empty((2, S, 512), np.float32)
        for m, (cf, cb) in enumerate(ms):
            arr[m, :, 0:256] = col(cf)
            arr[m, :, 256:512] = _rev(col(cb), nc_rows)
        res[name] = arr
    vt = np.empty((128, S, 8), np.float32)
    for m, cv in enumerate((2, 11)):
        v = col(cv).reshape(S, 4, 64)
        vt[m * 64:(m + 1) * 64, :, 0:4] = v.transpose(2, 0, 1)
        vt[m * 64:(m + 1) * 64, :, 4:8] = _rev(v, nc_rows).transpose(2, 0, 1)
    res["vt"] = vt
    return res


def l0_post_inputs(yo, o1, nc_rows, inp, g):
    S = yo.shape[1]
    hs = slice(g * 256, (g + 1) * 256)
    yin = np.empty((S, 1024), np.float32)
    for m in range(2):
        y = yo[m * 64:(m + 1) * 64]
        f = y[:, :, 0:4].transpose(1, 2, 0).reshape(S, 256)
        bk = _rev(y[:, :, 4:8].transpose(1, 2, 0).reshape(S, 256), nc_rows)
        yin[:, m * 512:m * 512 + 256] = f
        yin[:, m * 512 + 256:m * 512 + 512] = bk
    return dict(yin=yin, aux=np.ascontiguousarray(o1[:, 18 * 256:21 * 256]),
                gnw=np.tile(inp["l0_gdn_norm"], 4), lnw=np.ascontiguousarray(inp["l0_rwkv_ln_w"][hs]),
                lnb=np.ascontiguousarray(inp["l0_rwkv_ln_b"][hs]))


def build_C(n_ctx, n_lat):
    NT = n_ctx + n_lat
    nc = bass.Bass("TRN2", target_bir_lowering=False)
    def ein(name, shape):
        return nc.dram_tensor(name, list(shape), F32, kind="ExternalInput").ap()
    xs = ein("xs", [NT * 128, D])
    cond2 = ein("cond2", [2, D])
    mod_w = ein("mod_w", [D, 6 * D]); mod_b = ein("mod_b", [6 * D]); norm1 = ein("norm1", [D])
    Wa = ein("Wa", [D, 384])
    sink = ein("sink", [4])
    cos_d = ein("cos_t", [n_lat * 128, 64]); sin_d = ein("sin_t", [n_lat * 128, 64])
    mask_d = ein("amask", [3, 128, 640])
    ident_d = ein("ident_in", [128, 128])
    out = nc.dram_tensor("yattn", [n_lat * 128, 256], F32, kind="ExternalOutput").ap()
    p = Prog(nc)
    ident = mk(p, "ident", [128, 128])
    p.dma("sp", ident[:], ident_d[:, :], writes=[ident.b])
    psA = [mkps(p, "psA%d" % i, [128, 512]) for i in range(4)]
    psM = mkps(p, "psM", [128, 1024])
    psO = mkps(p, "psO", [128, 512])
    Qall = mk(p, "Qall", [128, n_lat, 256]); KT = mk(p, "KT", [64, NT * 128]); Vall = mk(p, "Vall", [128, NT, 64])
    MOD = mk(p, "MOD", [128, 2, 2, 1024])
    p.push_scope()
    adaln_chunks(p, cond2, mod_w, mod_b, [0, 1], MOD, psA, "ad")
    n1b = mk(p, "n1b", [128, 1024])
    p.dma("sp", n1b[:], norm1.partition_broadcast(128), writes=[n1b.b])
    for s in range(2):
        O(p, "dve", lambda e, s=s: e.scalar_tensor_tensor(MOD[:, s, 1, :], in0=MOD[:, s, 1, :], scalar=1.0, in1=n1b[:],
                                                          op0=ALU.add, op1=ALU.mult), [MOD], [MOD, n1b])
    Ws = mk(p, "Ws", [128, 8, 384])
    p.dma("sp", Ws[:], Wa.rearrange("(kc p) n -> p kc n", p=128), writes=[Ws.b])
    X = mk(p, "X", [128, 1024]); H = mk(p, "H", [128, 1024]); HT = mk(p, "HT", [128, 1024]); sm = mk(p, "sm", [128, 64])
    Pq = mk(p, "Pq", [128, 384]); Pr = mk(p, "Pr", [128, 320]); CS = mk(p, "CS", [128, 128]); tq = mk(p, "tq", [128, 320])
    for ti in range(NT):
        s = 1 if ti < n_ctx else 0
        p.dma("sp", X[:], xs[ti * 128:(ti + 1) * 128, :], writes=[X.b])
        rms_mod(p, X, H, sm, (MOD, 1), (MOD, 0), s)
        transpose_blocks(p, H, HT, psM, ident, 8)
        for kc in range(8):
            O(p, "pe", lambda e, kc=kc: e.matmul(psA[0][:, 0:384], lhsT=HT[:, kc * 128:(kc + 1) * 128], rhs=Ws[:, kc, :],
                                                 start=(kc == 0), stop=(kc == 7)), [psA[0]], [HT, Ws])
        O(p, "act", lambda e: e.copy(Pq[:], psA[0][:, 0:384]), [Pq], [psA[0]])
        O(p, "act", lambda e: e.copy(Vall[:, ti, :], Pq[:, 320:384]), [Vall], [Pq])
        if s == 1:
            ksrc = Pq[:, 256:320]; kb = Pq
        else:
            li = ti - n_ctx
            p.dma("pool", CS[:, 0:64], cos_d[li * 128:(li + 1) * 128, :], writes=[CS.b])
            p.dma("pool", CS[:, 64:128], sin_d[li * 128:(li + 1) * 128, :], writes=[CS.b])
            x5 = Pq[:, 0:320].rearrange("p (h f c) -> p h f c", h=5, f=2)
            t5 = tq[:].rearrange("p (h f c) -> p h f c", h=5, f=2)
            O(p, "act", lambda e: e.copy(t5[:, :, :, 0:16], x5[:, :, :, 16:32]), [tq], [Pq])
            O(p, "act", lambda e: e.copy(t5[:, :, :, 16:32], x5[:, :, :, 0:16]), [tq], [Pq])
            v5 = lambda ap: ap.rearrange("p (h d) -> p h d", h=5)
            cosb = CS[:, 0:64].unsqueeze(1).to_broadcast([128, 5, 64]); sinb = CS[:, 64:128].unsqueeze(1).to_broadcast([128, 5, 64])
            O(p, "dve", lambda e: e.tensor_tensor(v5(Pr[:]), v5(Pq[:, 0:320]), cosb, ALU.mult), [Pr], [Pq, CS])
            O(p, "dve", lambda e: e.tensor_tensor(v5(tq[:]), v5(tq[:]), sinb, ALU.mult), [tq], [tq, CS])
            O(p, "dve", lambda e: e.tensor_tensor(Pr[:], Pr[:], tq[:], ALU.add), [Pr], [Pr, tq])
            O(p, "act", lambda e, li=li: e.copy(Qall[:, li, :], Pr[:, 0:256]), [Qall], [Pr])
            ksrc = Pr[:, 256:320]; kb = Pr
        O(p, "pe", lambda e: e.transpose(psO[0:64, 0:128], ksrc, ident[:]), [psO], [kb, ident])
        O(p, "act", lambda e, ti=ti: e.copy(KT[:, ti * 128:(ti + 1) * 128], psO[0:64, 0:128]), [KT], [psO])
    p.pop_scope()
    masks = mk(p, "masks", [128, 3, 640])
    p.dma("sp", masks[:], mask_d.rearrange("m p k -> p m k"), writes=[masks.b])
    snk = mk(p, "snk", [128, 4])
    p.dma("sp", snk[:], sink.partition_broadcast(128), writes=[snk.b])
    qT = mk(p, "qT", [64, 512]); Sc = [mk(p, "Sc%d" % i, [128, 640]) for i in range(2)]; PT = [mk(p, "PT%d" % i, [128, 640]) for i in range(2)]
    s4 = mk(p, "s4", [128, 16]); Ot = [mk(p, "Ot%d" % i, [128, 256]) for i in range(2)]
    nck = n_ctx
    for li in range(n_lat):
        first, last = (li == 0), (li == n_lat - 1)
        lo = li - 1 if not first else li
        hi = li + 1 if not last else li
        chunks = list(range(nck)) + [nck + t for t in range(lo, hi + 1)]
        nk = len(chunks) * 128
        mi = 1 if first else (2 if last else 0)
        kranges = [(0, nck * 128), ((nck + lo) * 128, (nck + hi + 1) * 128)]
        for h in range(4):
            O(p, "pe", lambda e, h=h: e.transpose(psO[0:64, h * 128:(h + 1) * 128], Qall[:, li, h * 64:(h + 1) * 64], ident[:]), [psO], [Qall, ident])
        O(p, "act", lambda e: e.copy(qT[:], psO[0:64, :]), [qT], [psO])
        O_ = Ot[li % 2]
        for h in range(4):
            ps = psA[2 * (h % 2)]; ps2 = psA[2 * (h % 2) + 1]
            S_ = Sc[h % 2]; P_ = PT[h % 2]
            c0 = 0
            for (a, b_) in kranges:
                w = b_ - a
                tgt = ps if c0 + w <= 512 else None
                if c0 == 0:
                    O(p, "pe", lambda e, a=a, w=w: e.matmul(ps[:, 0:w], lhsT=qT[:, h * 128:(h + 1) * 128], rhs=KT[:, a:a + w], start=True, stop=True),
                      [ps], [qT, KT])
                    O(p, "dve", lambda e, w=w: e.scalar_tensor_tensor(S_[:, 0:w], in0=ps[:, 0:w], scalar=0.125, in1=masks[:, mi, 0:w],
                                                                      op0=ALU.mult, op1=ALU.add), [S_], [ps, masks])
                else:
                    O(p, "pe", lambda e, a=a, w=w: e.matmul(ps2[:, 0:w], lhsT=qT[:, h * 128:(h + 1) * 128], rhs=KT[:, a:a + w], start=True, stop=True),
                      [ps2], [qT, KT])
                    O(p, "dve", lambda e, w=w, c0=c0: e.scalar_tensor_tensor(S_[:, c0:c0 + w], in0=ps2[:, 0:w], scalar=0.125,
                                                                             in1=masks[:, mi, c0:c0 + w], op0=ALU.mult, op1=ALU.add), [S_], [ps2, masks])
                c0 += w
            O(p, "dve", lambda e: e.tensor_reduce(s4[:, 0:1], S_[:, 0:nk], AX.X, ALU.max), [s4], [S_])
            O(p, "dve", lambda e, h=h: e.tensor_tensor(s4[:, 0:1], s4[:, 0:1], snk[:, h:h + 1], ALU.max), [s4], [s4, snk])
            O(p, "dve", lambda e: e.tensor_scalar(s4[:, 1:2], s4[:, 0:1], -1.0, None, ALU.mult), [s4], [s4])
            O(p, "act", lambda e: e.activation(S_[:, 0:nk], S_[:, 0:nk], AF.Exp, bias=s4[:, 1:2], accum_out=s4[:, 2:3]), [S_, s4], [S_, s4])
            O(p, "act", lambda e, h=h: e.activation(s4[:, 3:4], snk[:, h:h + 1], AF.Exp, bias=s4[:, 1:2]), [s4], [snk, s4])
            O(p, "dve", lambda e: e.tensor_tensor(s4[:, 4:5], s4[:, 2:3], s4[:, 3:4], ALU.add), [s4], [s4])
            O(p, "dve", lambda e: e.reciprocal(s4[:, 5:6], s4[:, 4:5]), [s4], [s4])
            ncks = len(chunks)
            for c in range(ncks):
                O(p, "pe", lambda e, c=c: e.transpose(psM[:, c * 128:(c + 1) * 128], S_[:, c * 128:(c + 1) * 128], ident[:]), [psM], [S_, ident])
            O(p, "act", lambda e: e.copy(P_[:, 0:nk], psM[:, 0:nk]), [P_], [psM])
            for c, tix in enumerate(chunks):
                O(p, "pe", lambda e, c=c, tix=tix: e.matmul(psO[:, 0:64], lhsT=P_[:, c * 128:(c + 1) * 128], rhs=Vall[:, tix, :],
                                                            start=(c == 0), stop=(c == ncks - 1)), [psO], [P_, Vall])
            O(p, "dve", lambda e, h=h: e.tensor_scalar(O_[:, h * 64:(h + 1) * 64], psO[:, 0:64], s4[:, 5:6], None, ALU.mult), [O_], [psO, s4])
        p.dma("sp", out[li * 128:(li + 1) * 128, :], O_[:], reads=[O_.b])
    p.finish("sp")
    p.close()
    return nc


def rope_tables(L):
    t = np.arange(L)
    row = (t // 64).astype(np.float32); col = (t % 64).astype(np.float32)
    inv = (10000.0 ** (-np.arange(16, dtype=np.float32) / 16)).astype(np.float32)
    ar = row[:, None] * inv[None]; ac = col[:, None] * inv[None]
    cos = np.concatenate([np.cos(ar), np.cos(ar), np.cos(ac), np.cos(ac)], axis=1).astype(np.float32)
    sin = np.concatenate([-np.sin(ar), np.sin(ar), -np.sin(ac), np.sin(ac)], axis=1).astype(np.float32)
    return cos, sin


def attn_masks(n_ctx_rows=256):
    q = np.arange(128)[:, None]; j = np.arange(384)[None]
    loc = np.where((j >= q) & (j <= q + 256), 0.0, -1e30).astype(np.float32)
    m = np.zeros((3, 128, 640), np.float32)
    m[0, :, n_ctx_rows:n_ctx_rows + 384] = loc
    m[1, :, n_ctx_rows:n_ctx_rows + 256] = loc[:, 128:384]
    m[2, :, n_ctx_rows:n_ctx_rows + 256] = loc[:, 0:256]
    return m


TWO_PI = 6.283185307179586
MAGIC = 12582912.0


def build_H1(nJ):
    L = nJ * 128
    nc = bass.Bass("TRN2", target_bir_lowering=False)
    def ein(name, shape):
        return nc.dram_tensor(name, list(shape), F32, kind="ExternalInput").ap()
    xs = ein("xs", [4 * L, D])
    condA = ein("condA", [2, D]); condB = ein("condB", [2, D])
    mod_w = ein("mod_w", [D, 6 * D]); mod_b = ein("mod_b", [6 * D]); norm1 = ein("norm1", [D])
    Wh = ein("Wh", [D, 192]); hcw = ein("hcw", [3 * 192]); hcb = ein("hcb", [192])
    zT = ein("zT", [33, L]); w1 = ein("w1", [33, 64]); b1 = ein("b1", [64]); w2 = ein("w2", [64, 64]); b2 = ein("b2", [64])
    fr = ein("fr", [64]); w3c = ein("w3c", [64, 256]); win = ein("win", [L, 64])
    ident_d = ein("ident_in", [128, 128])
    u_out = nc.dram_tensor("u", [4 * L, 192], F32, kind="ExternalOutput").ap()
    hn_out = nc.dram_tensor("hn", [L, 256], F32, kind="ExternalOutput").ap()
    p = Prog(nc)
    Pd = p.dram("Pd", [4 * (L + 2), 192]); b_Pd = Buf()
    ident = mk(p, "ident", [128, 128])
    p.dma("sp", ident[:], ident_d[:, :], writes=[ident.b])
    psA = [mkps(p, "psA%d" % i, [128, 512]) for i in range(4)]
    psM = mkps(p, "psM", [128, 1024])
    psL = mkps(p, "psL", [128, 512])
    MODS = [mk(p, "MODa", [128, 2, 2, 1024]), mk(p, "MODb", [128, 2, 2, 1024])]
    p.push_scope()
    n1b = mk(p, "n1b", [128, 1024])
    p.dma("sp", n1b[:], norm1.partition_broadcast(128), writes=[n1b.b])
    for i, cd in enumerate((condA, condB)):
        p.push_scope2 = None
        adaln_chunks(p, cd, mod_w, mod_b, [0, 1], MODS[i], psA, "ad%d" % i)
        for s in range(2):
            O(p, "dve", lambda e, s=s, i=i: e.scalar_tensor_tensor(MODS[i][:, s, 1, :], in0=MODS[i][:, s, 1, :], scalar=1.0, in1=n1b[:],
                                                              op0=ALU.add, op1=ALU.mult), [MODS[i]], [MODS[i], n1b])
    p.pop_scope()
    p.push_scope()
    Ws = mk(p, "Ws", [128, 8, 192])
    p.dma("sp", Ws[:], Wh.rearrange("(kc p) n -> p kc n", p=128), writes=[Ws.b])
    X = mk(p, "X", [128, 1024]); H = mk(p, "H", [128, 1024]); HT = mk(p, "HT", [128, 1024]); sm = mk(p, "sm", [128, 64])
    Pt = [mk(p, "Pt%d" % i, [128, 192]) for i in range(2)]
    zrow = mk(p, "zrow", [1, 192])
    O(p, "dve", lambda e: e.memset(zrow[:], 0.0), [zrow], [])
    for b in range(4):
        for r in (b * (L + 2), b * (L + 2) + L + 1):
            p.dma("sp", Pd[r:r + 1, :], zrow[:], reads=[zrow.b], writes=[b_Pd])
    for b in range(4):
        for ti in range(nJ):
            gi = b * nJ + ti
            p.dma("sp", X[:], xs[gi * 128:(gi + 1) * 128, :], writes=[X.b])
            rms_mod(p, X, H, sm, (MODS[b // 2], 1), (MODS[b // 2], 0), b % 2)
            transpose_blocks(p, H, HT, psM, ident, 8)
            PT = Pt[gi % 2]
            for kc in range(8):
                O(p, "pe", lambda e, kc=kc: e.matmul(psA[0][:, 0:192], lhsT=HT[:, kc * 128:(kc + 1) * 128], rhs=Ws[:, kc, :],
                                                     start=(kc == 0), stop=(kc == 7)), [psA[0]], [HT, Ws])
            O(p, "act", lambda e: e.copy(PT[:], psA[0][:, 0:192]), [PT], [psA[0]])
            r0 = b * (L + 2) + 1 + ti * 128
            p.dma("sp", Pd[r0:r0 + 128, :], PT[:], reads=[PT.b], writes=[b_Pd])
    p.pop_scope()
    def bload(name, src, n):
        t = mk(p, name, [128, n])
        p.dma("sp", t[:], src.partition_broadcast(128), writes=[t.b])
        return t
    cw_t = bload("cw_t", hcw, 576); cb_t = bload("cb_t", hcb, 192)
    Pc = [mk(p, "Pc%d" % i, [128, 3, 192]) for i in range(2)]; U = [mk(p, "U%d" % i, [128, 192]) for i in range(2)]; t1 = mk(p, "t1", [128, 192])
    for b in range(4):
        for ti in range(nJ):
            gi = b * nJ + ti
            r0 = b * (L + 2) + 1 + ti * 128
            P3 = Pc[gi % 2]; Ut = U[gi % 2]
            for k in range(3):
                p.dma("sp" if k == 1 else "pool", P3[:, k, :], Pd[r0 - 1 + k:r0 + 127 + k, :], reads=[b_Pd], writes=[P3.b])
            O(p, "dve", lambda e: e.tensor_tensor(Ut[:], P3[:, 0, :], cw_t[:, 0:192], ALU.mult), [Ut], [P3, cw_t])
            O(p, "dve", lambda e: e.tensor_tensor(t1[:], P3[:, 1, :], cw_t[:, 192:384], ALU.mult), [t1], [P3, cw_t])
            O(p, "dve", lambda e: e.tensor_tensor(Ut[:], Ut[:], t1[:], ALU.add), [Ut], [Ut, t1])
            O(p, "dve", lambda e: e.tensor_tensor(t1[:], P3[:, 2, :], cw_t[:, 384:576], ALU.mult), [t1], [P3, cw_t])
            O(p, "dve", lambda e: e.tensor_tensor(Ut[:], Ut[:], t1[:], ALU.add), [Ut], [Ut, t1])
            O(p, "dve", lambda e: e.tensor_tensor(Ut[:], Ut[:], cb_t[:], ALU.add), [Ut], [Ut, cb_t])
            p.dma("sp", u_out[gi * 128:(gi + 1) * 128, :], Ut[:], reads=[Ut.b])
    zTs = mk(p, "zTs", [33, L]); p.dma("sp", zTs[:], zT[:, :], writes=[zTs.b])
    w1s = mk(p, "w1s", [33, 64]); p.dma("sp", w1s[:], w1[:, :], writes=[w1s.b])
    w2s = mk(p, "w2s", [64, 64]); p.dma("sp", w2s[:], w2[:, :], writes=[w2s.b])
    w3s = mk(p, "w3s", [64, 256]); p.dma("sp", w3s[:], w3c[:, :], writes=[w3s.b])
    b1_t = bload("b1_t", b1, 64); b2_t = bload("b2_t", b2, 64); fr_t = bload("fr_t", fr, 64)
    ones = mk(p, "ones", [128, 128]); O(p, "dve", lambda e: e.memset(ones[:], 1.0), [ones], [])
    hraw = mk(p, "hraw", [128, nJ, 256]); habs = mk(p, "habs", [128, 256])
    a = mk(p, "a", [128, 64]); k_ = mk(p, "k_", [128, 64]); hT = mk(p, "hT", [64, 128]); wn = [mk(p, "wn%d" % i, [128, 64]) for i in range(2)]

    def sin_layer(ps, bt):
        O(p, "dve", lambda e: e.tensor_tensor(a[:], ps[:, 0:64], bt[:], ALU.add), [a], [ps, bt])
        O(p, "dve", lambda e: e.tensor_tensor(a[:], a[:], fr_t[:], ALU.mult), [a], [a, fr_t])
        O(p, "dve", lambda e: e.tensor_scalar(k_[:], a[:], 1.0 / TWO_PI, MAGIC, ALU.mult, ALU.add), [k_], [a])
        O(p, "dve", lambda e: e.tensor_scalar(k_[:], k_[:], -MAGIC, None, ALU.add), [k_], [k_])
        O(p, "dve", lambda e: e.scalar_tensor_tensor(a[:], in0=k_[:], scalar=-TWO_PI, in1=a[:], op0=ALU.mult, op1=ALU.add), [a], [k_, a])
        O(p, "act", lambda e: e.activation(a[:], a[:], AF.Sin), [a], [a])
        O(p, "pe", lambda e: e.transpose(psM[0:64, 0:128], a[:], ident[:]), [psM], [a, ident])
        O(p, "act", lambda e: e.copy(hT[:], psM[0:64, 0:128]), [hT], [psM])

    for ti in range(nJ):
        W_ = wn[ti % 2]
        p.dma("pool", W_[:], win[ti * 128:(ti + 1) * 128, :], writes=[W_.b])
        O(p, "pe", lambda e: e.matmul(psA[1][:, 0:64], lhsT=zTs[:, ti * 128:(ti + 1) * 128], rhs=w1s[:], start=True, stop=True), [psA[1]], [zTs, w1s])
        sin_layer(psA[1], b1_t)
        O(p, "pe", lambda e: e.matmul(psA[2][:, 0:64], lhsT=hT[:], rhs=w2s[:], start=True, stop=True), [psA[2]], [hT, w2s])
        sin_layer(psA[2], b2_t)
        O(p, "pe", lambda e: e.matmul(psA[3][:, 0:256], lhsT=hT[:], rhs=w3s[:], start=True, stop=True), [psA[3]], [hT, w3s])
        O(p, "dve", lambda e: e.tensor_tensor(hraw[:, ti, :].rearrange("p (g c) -> p g c", c=64), psA[3][:, 0:256].rearrange("p (g c) -> p g c", c=64),
                                              W_[:].unsqueeze(1).to_broadcast([128, 4, 64]), ALU.mult), [hraw], [psA[3], W_])
        O(p, "act", lambda e: e.activation(habs[:], hraw[:, ti, :], AF.Abs), [habs], [hraw])
        O(p, "pe", lambda e: e.matmul(psL[:, 0:256], lhsT=ones[:], rhs=habs[:], start=(ti == 0), stop=(ti == nJ - 1)), [psL], [ones, habs])
    l1 = mk(p, "l1", [128, 2, 64])
    pl = psL[:, 0:256].rearrange("p (n f c) -> p n f c", n=2, f=2)
    O(p, "dve", lambda e: e.tensor_copy(l1[:], pl[:, :, 0, :]), [l1], [psL])
    O(p, "dve", lambda e: e.tensor_tensor(l1[:], l1[:], pl[:, :, 1, :], ALU.add), [l1], [l1, psL])
    O(p, "dve", lambda e: e.reciprocal(l1[:], l1[:]), [l1], [l1])
    ho = [mk(p, "ho%d" % i, [128, 256]) for i in range(2)]
    for ti in range(nJ):
        Ho = ho[ti % 2]
        O(p, "dve", lambda e: e.tensor_tensor(Ho[:].rearrange("p (n f c) -> p n f c", n=2, f=2), hraw[:, ti, :].rearrange("p (n f c) -> p n f c", n=2, f=2),
                                              l1[:].unsqueeze(2).to_broadcast([128, 2, 2, 64]), ALU.mult), [Ho], [hraw, l1])
        p.dma("sp", hn_out[ti * 128:(ti + 1) * 128, :], Ho[:], reads=[Ho.b])
    p.finish("sp")
    p.close()
    return nc


def build_H2(nJ):
    L = nJ * 128
    NB = nJ * 4
    nc = bass.Bass("TRN2", target_bir_lowering=False)
    def ein(name, shape):
        return nc.dram_tensor(name, list(shape), F32, kind="ExternalInput").ap()
    zrev = ein("zrev", [128, 64, NB]); znat = ein("znat", [128, 64, NB]); xg = ein("xg", [128, 64, NB])
    ka = ein("ka", [64, 2 * L]); bias = ein("bias", [64]); hb0 = ein("hb0", [64])
    zo = nc.dram_tensor("zo", [128, 64, NB], F32, kind="ExternalOutput").ap()
    p = Prog(nc)
    be = mk(p, "be", [128, 64]); hb = mk(p, "hb", [128, 64])
    p.dma("sp", be[:], bias.partition_broadcast(128), writes=[be.b])
    p.dma("sp", hb[:], hb0.partition_broadcast(128), writes=[hb.b])
    O(p, "dve", lambda e: e.tensor_tensor(be[:], be[:], hb[:], ALU.add), [be], [be, hb])
    CH = 8
    ZR = [mk(p, "ZR%d" % i, [128, CH, NB]) for i in range(2)]; ZN = [mk(p, "ZN%d" % i, [128, CH, NB]) for i in range(2)]
    XG = [mk(p, "XG%d" % i, [128, CH, NB]) for i in range(2)]; ZO = [mk(p, "ZO%d" % i, [128, CH, NB]) for i in range(2)]
    GD = 16
    ds_all = [0] + [d for d in range(-(nJ - 1), nJ) if d != 0]
    dlist = list(range(-(nJ - 1), nJ))
    groups = [dlist[i:i + GD] for i in range(0, len(dlist), GD)]
    groups.sort(key=lambda g: 0 if 0 in g else 1)
    Kt = [mk(p, "Kt%d" % i, [128, GD, 128]) for i in range(3)]
    psY = [mkps(p, "psY%d" % i, [128, 512]) for i in range(2)]
    kidx = 0
    for c0 in range(0, 64, CH):
        ci = (c0 // CH) % 2
        p.dma("pool", ZR[ci][:], zrev[:, c0:c0 + CH, :], writes=[ZR[ci].b])
        p.dma("pool", ZN[ci][:], znat[:, c0:c0 + CH, :], writes=[ZN[ci].b])
        p.dma("pool", XG[ci][:], xg[:, c0:c0 + CH, :], writes=[XG[ci].b])
        for cc in range(CH):
            c = c0 + cc
            Y = psY[c % 2]
            first = True
            nmm = 2 * nJ - 1
            done = 0
            for g in groups:
                K_ = Kt[kidx % 3]; kidx += 1
                src = bass.AP(ka.tensor, c * 2 * L + L + g[0] * 128 - 127, [[1, 128], [128, len(g)], [1, 128]])
                p.dma("sp", K_[:, 0:len(g), :], src, writes=[K_.b])
                order = ([0] + [d for d in g if d != 0]) if 0 in g else g
                for d in order:
                    J0, J1 = max(0, -d), min(nJ, nJ - d)
                    done += 1
                    O(p, "pe", lambda e, d=d, J0=J0, J1=J1, gi=d - g[0], first=first, last=(done == nmm):
                      e.matmul(Y[:, (J0 + d) * 4:(J1 + d) * 4], lhsT=K_[:, gi, :], rhs=ZR[ci][:, cc, J0 * 4:J1 * 4], start=first, stop=last),
                      [Y], [K_, ZR[ci]])
                    first = False
            O(p, "dve", lambda e, c=c, cc=cc: e.scalar_tensor_tensor(ZO[ci][:, cc, :], in0=ZN[ci][:, cc, :], scalar=be[:, c:c + 1], in1=Y[:, 0:NB],
                                                                    op0=ALU.mult, op1=ALU.add), [ZO[ci]], [ZN[ci], be, Y])
            O(p, "dve", lambda e, cc=cc: e.tensor_tensor(ZO[ci][:, cc, :], ZO[ci][:, cc, :], XG[ci][:, cc, :], ALU.mult), [ZO[ci]], [ZO[ci], XG[ci]])
        p.dma("sp", zo[:, c0:c0 + CH, :], ZO[ci][:], reads=[ZO[ci].b])
    p.finish("sp")
    p.close()
    return nc


def hyena_consts(L, cg):
    t = np.arange(L, dtype=np.float32)
    t_norm = np.linspace(0.0, 1.0, L, dtype=np.float32)[:, None]
    bands = np.linspace(1e-4, 15, 16, dtype=np.float32)
    ang = (np.float32(2.0 * np.pi / L) * t[:, None] * bands[None, :]).astype(np.float32)
    z = np.concatenate([t_norm, np.cos(ang), np.sin(ang)], axis=-1).astype(np.float32)
    deltas = np.abs(np.linspace(np.log(1e-2) / 1.5, np.log(1e-2) / 0.3, 512, dtype=np.float32))
    window = np.exp(-t_norm * deltas[None, :]).astype(np.float32)
    return np.ascontiguousarray(z.T), np.ascontiguousarray(window[:, cg * 64:(cg + 1) * 64])


def hy_layout(a, nJ, rev):
    x = a.reshape(4, nJ, 128, 64)
    if rev:
        x = x[:, :, ::-1, :]
    return np.ascontiguousarray(x.transpose(2, 3, 1, 0).reshape(128, 64, nJ * 4))


def hy_unlayout(zo, nJ):
    return np.ascontiguousarray(zo.reshape(128, 64, nJ, 4).transpose(3, 2, 0, 1).reshape(4, nJ * 128, 64))


def hy_kernel_array(hn, n, L):
    h = hn.reshape(L, 2, 2, 64)
    ka = np.zeros((64, 2 * L), np.float32)
    ka[:, L:2 * L] = h[:, n, 0, :].T
    ka[:, 1:L] = h[1:, n, 1, :][::-1].T
    return ka, np.ascontiguousarray(h[0, n, 1, :])


def _run(nc, in_maps):
    res = run_bass_kernel_spmd(nc, in_maps, core_ids=list(range(8)))
    return res.results


def _stageB(pf, inp, xs_list, fs_list, cond_list, n_lat, n_ctx, final):
    nc = build_B(n_lat, n_ctx, final)
    common = dict(mod_w=inp[pf + "mod_w"], mod_b=inp[pf + "mod_b"], norm2=inp[pf + "norm2"], w_out=inp[pf + "w_out"], wq=inp[pf + "peer_wq"],
                  k1T=np.ascontiguousarray(inp[pf + "peer_k1"].transpose(0, 2, 1)), k2T=np.ascontiguousarray(inp[pf + "peer_k2"].transpose(0, 2, 1)),
                  UT=np.ascontiguousarray(inp[pf + "peer_u"].T), V=inp[pf + "peer_v"], fnorm=inp["final_norm"],
                  ident_in=np.eye(128, dtype=np.float32))
    maps = [dict(common, xs=xs_list[i], fs=fs_list[i], cond2=cond_list[i]) for i in range(8)]
    return [r["xo"] for r in _run(nc, maps)]


def kernel(**inp):
    inp = {k: np.ascontiguousarray(np.asarray(v, dtype=np.float32)) for k, v in inp.items()}
    x, c, ctx, c_ctx = inp["x"], inp["c"], inp["ctx"], inp["c_ctx"]
    B, L, _ = x.shape
    C = ctx.shape[1]
    n_ctx, n_lat = C // 128, L // 128
    NT = n_ctx + n_lat
    xseq = [np.concatenate([ctx[b], x[b]], axis=0) for b in range(B)]
    nc = build_A1(n_ctx, n_lat)
    r1 = _run(nc, [l0_core_inputs(i // 2, i % 2, inp, xseq[i // 2], n_ctx, n_lat) for i in range(8)])
    o1 = [r["o1"] for r in r1]
    del r1
    nc = build_A2(NT * 128)
    r2 = _run(nc, [l0_scan_inputs(o1[i], C) for i in range(8)])
    yo = [r["yo"] for r in r2]
    del r2
    nc = build_A3(NT)
    r3 = _run(nc, [l0_post_inputs(yo[i], o1[i], C, inp, i % 2) for i in range(8)])
    feat = [r["feat"] for r in r3]
    del r3, o1, yo
    fs0 = [np.concatenate([feat[2 * b][:, 0:256], feat[2 * b + 1][:, 0:256], feat[2 * b][:, 256:512], feat[2 * b + 1][:, 256:512]], axis=1)
           for b in range(B)]
    hl, hc = L // 2, C // 2
    xs_l, fs_l, cd_l = [], [], []
    for i in range(8):
        b, h = i // 2, i % 2
        xs_l.append(np.concatenate([x[b, h * hl:(h + 1) * hl], ctx[b, h * hc:(h + 1) * hc]], axis=0))
        fs_l.append(np.concatenate([fs0[b][C + h * hl:C + (h + 1) * hl], fs0[b][h * hc:(h + 1) * hc]], axis=0))
        cd_l.append(np.stack([c[b], c_ctx]))
    xo = _stageB("l0_", inp, xs_l, fs_l, cd_l, hl // 128, hc // 128, 0)
    x1 = np.stack([np.concatenate([xo[2 * b][:hl], xo[2 * b + 1][:hl]], axis=0) for b in range(B)])
    ctx1 = np.stack([np.concatenate([xo[2 * b][hl:], xo[2 * b + 1][hl:]], axis=0) for b in range(B)])
    del xo, fs0, feat
    w_in = inp["l1_w_in"]
    cos, sin = rope_tables(L)
    am = attn_masks(C)
    maps = []
    for i in range(8):
        b, kvh = i // 2, i % 2
        cols = np.concatenate([np.arange(kvh * 256, (kvh + 1) * 256), 512 + np.arange(kvh * 64, (kvh + 1) * 64), 640 + np.arange(kvh * 64, (kvh + 1) * 64)])
        maps.append(dict(xs=np.concatenate([ctx1[b], x1[b]], axis=0), cond2=np.stack([c[b], c_ctx]), mod_w=inp["l1_mod_w"], mod_b=inp["l1_mod_b"],
                         norm1=inp["l1_norm1"], Wa=np.ascontiguousarray(w_in[:, cols]),
                         sink=np.ascontiguousarray(inp["l1_attn_sink"][kvh * 4:(kvh + 1) * 4]), cos_t=cos, sin_t=sin, amask=am,
                         ident_in=np.eye(128, dtype=np.float32)))
    nc = build_C(n_ctx, n_lat)
    ya = [r["yattn"] for r in _run(nc, maps)]
    nJ = n_lat
    maps = []
    xall = x1.reshape(B * L, D)
    for cg in range(8):
        cols = np.concatenate([768 + j * 512 + np.arange(cg * 64, (cg + 1) * 64) for j in range(3)])
        zT, win = hyena_consts(L, cg)
        w3cols = np.concatenate([n * 1024 + d * 512 + np.arange(cg * 64, (cg + 1) * 64) for n in range(2) for d in range(2)])
        maps.append(dict(xs=xall, condA=np.ascontiguousarray(c[0:2]), condB=np.ascontiguousarray(c[2:4]), mod_w=inp["l1_mod_w"], mod_b=inp["l1_mod_b"],
                         norm1=inp["l1_norm1"], Wh=np.ascontiguousarray(w_in[:, cols]),
                         hcw=np.ascontiguousarray(inp["l1_hy_conv_w"][:, cols - 768]).reshape(-1),
                         hcb=np.ascontiguousarray(inp["l1_hy_conv_b"][cols - 768]), zT=zT, w1=inp["l1_hy_w1"], b1=inp["l1_hy_b1"],
                         w2=inp["l1_hy_w2"], b2=inp["l1_hy_b2"], fr=inp["l1_hy_freq"], w3c=np.ascontiguousarray(inp["l1_hy_w3"][:, w3cols]),
                         win=win, ident_in=np.eye(128, dtype=np.float32)))
    nc = build_H1(nJ)
    rh = _run(nc, maps)
    us = [r["u"].reshape(B, L, 3, 64) for r in rh]
    hns = [r["hn"] for r in rh]
    del rh, maps
    z = [u[:, :, 0, :] for u in us]
    ncH = build_H2(nJ)
    for n in range(2):
        maps = []
        for cg in range(8):
            ka, hb0 = hy_kernel_array(hns[cg], n, L)
            maps.append(dict(zrev=hy_layout(z[cg], nJ, True), znat=hy_layout(z[cg], nJ, False), xg=hy_layout(us[cg][:, :, 1 + n, :], nJ, False),
                             ka=ka, bias=np.ascontiguousarray(inp["l1_hy_bias"][n, cg * 64:(cg + 1) * 64]), hb0=hb0))
        z = [hy_unlayout(r["zo"], nJ) for r in _run(ncH, maps)]
    yh = np.concatenate(z, axis=2)
    xs_l, fs_l, cd_l = [], [], []
    for i in range(8):
        b, h = i // 2, i % 2
        sl = slice(h * hl, (h + 1) * hl)
        xs_l.append(np.ascontiguousarray(x1[b, sl]))
        fs_l.append(np.concatenate([ya[2 * b][sl], ya[2 * b + 1][sl], yh[b, sl]], axis=1))
        cd_l.append(np.stack([c[b], c_ctx]))
    xo = _stageB("l1_", inp, xs_l, fs_l, cd_l, hl // 128, 0, 1)
    out = np.stack([np.concatenate([xo[2 * b], xo[2 * b + 1]], axis=0) for b in range(B)])
    return out.astype(np.float32)
```
